# Optimizing a Trainium2 kernel written in Bass

```python
import math
import jax, jax.numpy as jnp
from jax import lax
import numpy as np

D_MODEL = 1024
BATCH = 1
SEQ = 16384
DEPTH = 1

N_HEADS = 8
HEAD_DIM = 64
N_KV = 2
HPG = N_HEADS // N_KV
ATTN_WIDTH = N_HEADS * HEAD_DIM
SSM_GROUP = 16
SSM_GROUPS = 32
SSM_WIDTH = SSM_GROUP * SSM_GROUPS
SSM_STATE = 64
MIX_WIDTH = ATTN_WIDTH + SSM_WIDTH
KV_WIDTH = N_KV * HEAD_DIM
IN_WIDTH = ATTN_WIDTH + 6 * KV_WIDTH + 3 * N_HEADS + SSM_WIDTH
CMP_LEN = 32
CMP_STRIDE = 16
CMP_HIDDEN = 128
SEL_BLOCK = 64
N_SEL = 16
WINDOW = 512
Q_BLOCK = 128
ROPE_THETA = 500000.0
ROPE_DIM = HEAD_DIM // 4
PEER_HEADS = 8
PEER_NKEYS = 128
PEER_EXPERTS = PEER_NKEYS * PEER_NKEYS
PEER_QDIM = 256
PEER_TOPK = 16
PEER_CHUNK = 128
NORM_EPS = 1e-6
NEG_INF = -1e30
FORCE_SCORE = 1e9

kernel_name = "hymba_nsa_s5_peer_block"


def rms_norm(x, g):
    xf = x.astype(jnp.float32)
    y = xf * lax.rsqrt(jnp.mean(xf * xf, axis=-1, keepdims=True) + NORM_EPS)
    return (y * g.astype(jnp.float32)).astype(x.dtype)


def rope(x, pos):
    half = ROPE_DIM // 2
    inv = ROPE_THETA ** (-jnp.arange(half, dtype=jnp.float32) * 2.0 / ROPE_DIM)
    ang = pos.astype(jnp.float32)[:, None] * inv[None, :]
    cos = jnp.cos(ang)[None, :, None, :]
    sin = jnp.sin(ang)[None, :, None, :]
    xr = x[..., :ROPE_DIM].astype(jnp.float32)
    x1, x2 = xr[..., :half], xr[..., half:]
    rot = jnp.concatenate([x1 * cos - x2 * sin, x2 * cos + x1 * sin], axis=-1).astype(x.dtype)
    return jnp.concatenate([rot, x[..., ROPE_DIM:]], axis=-1)


def masked_softmax(s, mask):
    s = jnp.where(mask, s, NEG_INF)
    m = jnp.max(s, axis=-1, keepdims=True)
    e = jnp.where(mask, jnp.exp(s - m), 0.0)
    return e / jnp.maximum(jnp.sum(e, axis=-1, keepdims=True), 1e-30)


def compress(kv, pe, w1, w2):
    b, s = kv.shape[0], kv.shape[1]
    n_cmp = (s - CMP_LEN) // CMP_STRIDE + 1
    idx = jnp.arange(n_cmp)[:, None] * CMP_STRIDE + jnp.arange(CMP_LEN)[None, :]
    blk = kv[:, idx] + pe[None, None, :, None, :]
    blk = blk.transpose(0, 1, 3, 2, 4).reshape(b, n_cmp, N_KV, CMP_LEN * HEAD_DIM)
    return jax.nn.gelu(blk @ w1) @ w2


def nsa_attention(q, kc_raw, vc_raw, ks, vs, kw, vw, gates, pe_k, w1k, w2k, pe_v, w1v, w2v):
    b, s = q.shape[0], q.shape[1]
    pos = jnp.arange(s)
    n_blk = s // Q_BLOCK
    n_selb = s // SEL_BLOCK
    n_sel = min(N_SEL, n_selb)
    q = rope(q, pos) * (HEAD_DIM ** -0.5)
    kc = compress(kc_raw, pe_k, w1k, w2k)
    vc = compress(vc_raw, pe_v, w1v, w2v)
    n_cmp = kc.shape[1]
    cmp_start = jnp.arange(n_cmp) * CMP_STRIDE
    cmp_end = cmp_start + CMP_LEN - 1
    kc = rope(kc, cmp_end).transpose(0, 2, 1, 3)
    vc = vc.transpose(0, 2, 1, 3)
    sel_ids = jnp.arange(n_selb)
    sel_start = sel_ids * SEL_BLOCK
    overlap = ((cmp_start[:, None] < sel_start[None, :] + SEL_BLOCK)
               & (cmp_start[:, None] + CMP_LEN > sel_start[None, :])).astype(jnp.float32)
    ks_blk = rope(ks, pos).reshape(b, n_selb, SEL_BLOCK, N_KV, HEAD_DIM).transpose(0, 3, 1, 2, 4)
    vs_blk = vs.reshape(b, n_selb, SEL_BLOCK, N_KV, HEAD_DIM).transpose(0, 3, 1, 2, 4)
    kw_pad = jnp.pad(rope(kw, pos), ((0, 0), (WINDOW, 0), (0, 0), (0, 0))).transpose(0, 2, 1, 3)
    vw_pad = jnp.pad(vw, ((0, 0), (WINDOW, 0), (0, 0), (0, 0))).transpose(0, 2, 1, 3)
    q_blocks = q.reshape(b, n_blk, Q_BLOCK, N_KV, HPG, HEAD_DIM).transpose(1, 0, 3, 4, 2, 5)
    g_blocks = jax.nn.sigmoid(gates.astype(jnp.float32)).reshape(
        b, n_blk, Q_BLOCK, N_KV, HPG, 3).transpose(1, 0, 3, 4, 2, 5)
    b_ix = jnp.arange(b)[:, None, None, None]
    g_ix = jnp.arange(N_KV)[None, :, None, None]

    def block(args):
        blk, qb, gb = args
        t = blk * Q_BLOCK + jnp.arange(Q_BLOCK)
        s_c = jnp.einsum('bghqd,bgnd->bghqn', qb, kc).astype(jnp.float32)
        p_c = masked_softmax(s_c, cmp_end[None, :] <= t[:, None])
        o_c = jnp.einsum('bghqn,bgnd->bghqd', p_c.astype(vc.dtype), vc)
        imp = jnp.einsum('bghqn,nm->bgqm', p_c, overlap)
        cur = (t // SEL_BLOCK)[:, None]
        valid = sel_start[None, :] <= t[:, None]
        forced = (sel_ids[None, :] == 0) | (sel_ids[None, :] == cur) | (sel_ids[None, :] == cur - 1)
        imp = jnp.where(forced, FORCE_SCORE, jnp.where(valid, imp, NEG_INF))
        _, sidx = lax.top_k(imp, n_sel)
        k_sel = ks_blk[b_ix, g_ix, sidx]
        v_sel = vs_blk[b_ix, g_ix, sidx]
        s_s = jnp.einsum('bghqd,bgqnkd->bghqnk', qb, k_sel).astype(jnp.float32)
        kpos = sidx[..., None] * SEL_BLOCK + jnp.arange(SEL_BLOCK)
        m_s = (kpos <= t[:, None, None])[:, :, None]
        shp = s_s.shape
        p_s = masked_softmax(s_s.reshape(shp[0], shp[1], shp[2], shp[3], -1),
                             m_s.reshape(shp[0], shp[1], 1, shp[3], -1)).reshape(shp)
        o_s = jnp.einsum('bghqnk,bgqnkd->bghqd', p_s.astype(v_sel.dtype), v_sel)
        start = blk * Q_BLOCK
        k_w = lax.dynamic_slice_in_dim(kw_pad, start, Q_BLOCK + WINDOW, axis=2)
        v_w = lax.dynamic_slice_in_dim(vw_pad, start, Q_BLOCK + WINDOW, axis=2)
        kpos_w = start - WINDOW + jnp.arange(Q_BLOCK + WINDOW)
        m_w = ((kpos_w[None, :] >= 0) & (kpos_w[None, :] <= t[:, None])
               & (t[:, None] - kpos_w[None, :] < WINDOW))
        s_w = jnp.einsum('bghqd,bgkd->bghqk', qb, k_w).astype(jnp.float32)
        p_w = masked_softmax(s_w, m_w)
        o_w = jnp.einsum('bghqk,bgkd->bghqd', p_w.astype(v_w.dtype), v_w)
        gb = gb.astype(qb.dtype)
        return gb[..., 0:1] * o_c + gb[..., 1:2] * o_s + gb[..., 2:3] * o_w

    out = lax.map(block, (jnp.arange(n_blk), q_blocks, g_blocks))
    return out.transpose(1, 0, 4, 2, 3, 5).reshape(b, s, ATTN_WIDTH)


def s5_ssm(u, lam_re, lam_im, log_dt, b_re, b_im, c_re, c_im, d_skip, w_glu):
    b, s = u.shape[0], u.shape[1]
    f32 = jnp.float32
    lam = lax.complex(lam_re.astype(f32), lam_im.astype(f32))
    dt = jnp.exp(log_dt.astype(f32))[:, None]
    lam_bar = jnp.exp(lam * dt)
    bmat = lax.complex(b_re.astype(f32), b_im.astype(f32))
    b_bar = ((lam_bar - 1.0) / lam)[..., None] * bmat
    cmat = lax.complex(c_re.astype(f32), c_im.astype(f32))
    uf = u.astype(f32)
    ug = uf.reshape(b, s, SSM_GROUPS, SSM_GROUP).astype(jnp.complex64)
    bu = jnp.einsum('gpc,bsgc->bsgp', b_bar, ug)
    a = jnp.broadcast_to(lam_bar, bu.shape)

    def combine(e1, e2):
        a1, b1 = e1
        a2, b2 = e2
        return (a1 * a2, a2 * b1 + b2)

    _, states = lax.associative_scan(combine, (a, bu), axis=1)
    y = jnp.real(jnp.einsum('gcp,bsgp->bsgc', cmat, states)).reshape(b, s, SSM_WIDTH)
    y = jax.nn.gelu(y + d_skip.astype(f32) * uf).astype(u.dtype)
    ab = y @ w_glu
    return ab[..., :SSM_WIDTH] * jax.nn.sigmoid(ab[..., SSM_WIDTH:])


def peer_ffn(x, w_q, sub1, sub2, u_tab, v_tab):
    b, s, d = x.shape
    n_ch = s // PEER_CHUNK
    half = PEER_QDIM // 2
    xc = x.reshape(b, n_ch, PEER_CHUNK, d).transpose(1, 0, 2, 3)

    def chunk(xb):
        q = (xb @ w_q).reshape(b, PEER_CHUNK, PEER_HEADS, PEER_QDIM)
        s1 = jnp.einsum('bchd,kd->bchk', q[..., :half], sub1).astype(jnp.float32)
        s2 = jnp.einsum('bchd,kd->bchk', q[..., half:], sub2).astype(jnp.float32)
        v1, i1 = lax.top_k(s1, PEER_TOPK)
        v2, i2 = lax.top_k(s2, PEER_TOPK)
        cand = (v1[..., :, None] + v2[..., None, :]).reshape(b, PEER_CHUNK, PEER_HEADS, PEER_TOPK * PEER_TOPK)
        cidx = (i1[..., :, None] * PEER_NKEYS + i2[..., None, :]).reshape(b, PEER_CHUNK, PEER_HEADS, PEER_TOPK * PEER_TOPK)
        top, sel = lax.top_k(cand, PEER_TOPK)
        eidx = jnp.take_along_axis(cidx, sel, axis=-1)
        gate = jax.nn.softmax(top, axis=-1)
        u_sel = u_tab[eidx]
        v_sel = v_tab[eidx]
        hid = jnp.einsum('bcd,bchkd->bchk', xb, u_sel).astype(jnp.float32)
        act = (jax.nn.gelu(hid) * gate).astype(xb.dtype)
        return jnp.einsum('bchk,bchkd->bcd', act, v_sel)

    out = lax.map(chunk, xc)
    return out.transpose(1, 0, 2, 3).reshape(b, s, d)


def setup_inputs(seed: int = 0) -> dict:
    key = jax.random.key(seed)
    ks = jax.random.split(key, 25)
    f32 = jnp.float32

    def nrm(k, shape, scale):
        return jax.random.normal(k, shape, f32) * scale

    return {
        'x': nrm(ks[0], (BATCH, SEQ, D_MODEL), 1.0),
        'attn_norm': 1.0 + nrm(ks[1], (DEPTH, D_MODEL), 0.01),
        'w_in': nrm(ks[2], (DEPTH, D_MODEL, IN_WIDTH), D_MODEL ** -0.5),
        'cmp_k_pe': nrm(ks[3], (DEPTH, CMP_LEN, HEAD_DIM), 0.1),
        'cmp_k_w1': nrm(ks[4], (DEPTH, CMP_LEN * HEAD_DIM, CMP_HIDDEN), (CMP_LEN * HEAD_DIM) ** -0.5),
        'cmp_k_w2': nrm(ks[5], (DEPTH, CMP_HIDDEN, HEAD_DIM), CMP_HIDDEN ** -0.5),
        'cmp_v_pe': nrm(ks[6], (DEPTH, CMP_LEN, HEAD_DIM), 0.1),
        'cmp_v_w1': nrm(ks[7], (DEPTH, CMP_LEN * HEAD_DIM, CMP_HIDDEN), (CMP_LEN * HEAD_DIM) ** -0.5),
        'cmp_v_w2': nrm(ks[8], (DEPTH, CMP_HIDDEN, HEAD_DIM), CMP_HIDDEN ** -0.5),
        'ssm_lam_re': -0.5 + nrm(ks[9], (DEPTH, SSM_GROUPS, SSM_STATE), 0.01),
        'ssm_lam_im': jnp.broadcast_to(math.pi * jnp.arange(SSM_STATE, dtype=f32), (DEPTH, SSM_GROUPS, SSM_STATE)),
        'ssm_log_dt': jax.random.uniform(ks[10], (DEPTH, SSM_GROUPS), f32, math.log(1e-3), math.log(1e-1)),
        'ssm_b_re': nrm(ks[11], (DEPTH, SSM_GROUPS, SSM_STATE, SSM_GROUP), (2 * SSM_GROUP) ** -0.5),
        'ssm_b_im': nrm(ks[12], (DEPTH, SSM_GROUPS, SSM_STATE, SSM_GROUP), (2 * SSM_GROUP) ** -0.5),
        'ssm_c_re': nrm(ks[13], (DEPTH, SSM_GROUPS, SSM_GROUP, SSM_STATE), 0.5),
        'ssm_c_im': nrm(ks[14], (DEPTH, SSM_GROUPS, SSM_GROUP, SSM_STATE), 0.5),
        'ssm_d': nrm(ks[15], (DEPTH, SSM_WIDTH), 1.0),
        'ssm_w_glu': nrm(ks[16], (DEPTH, SSM_WIDTH, 2 * SSM_WIDTH), SSM_WIDTH ** -0.5),
        'w_out': nrm(ks[17], (DEPTH, MIX_WIDTH, D_MODEL), MIX_WIDTH ** -0.5),
        'ffn_norm': 1.0 + nrm(ks[18], (DEPTH, D_MODEL), 0.01),
        'peer_w_q': nrm(ks[19], (DEPTH, D_MODEL, PEER_HEADS * PEER_QDIM), D_MODEL ** -0.5),
        'peer_subkeys_1': nrm(ks[20], (DEPTH, PEER_NKEYS, PEER_QDIM // 2), (PEER_QDIM // 2) ** -0.5),
        'peer_subkeys_2': nrm(ks[21], (DEPTH, PEER_NKEYS, PEER_QDIM // 2), (PEER_QDIM // 2) ** -0.5),
        'peer_u': nrm(ks[22], (DEPTH, PEER_EXPERTS, D_MODEL), D_MODEL ** -0.5),
        'peer_v': nrm(ks[23], (DEPTH, PEER_EXPERTS, D_MODEL), PEER_HEADS ** -0.5),
        'final_norm': 1.0 + nrm(ks[24], (D_MODEL,), 0.01),
    }


def reference(x, attn_norm, w_in, cmp_k_pe, cmp_k_w1, cmp_k_w2, cmp_v_pe, cmp_v_w1, cmp_v_w2,
              ssm_lam_re, ssm_lam_im, ssm_log_dt, ssm_b_re, ssm_b_im, ssm_c_re, ssm_c_im, ssm_d,
              ssm_w_glu, w_out, ffn_norm, peer_w_q, peer_subkeys_1, peer_subkeys_2, peer_u, peer_v,
              final_norm):
    b, s, _ = x.shape
    sizes = [ATTN_WIDTH] + [KV_WIDTH] * 6 + [3 * N_HEADS]
    cuts = [int(c) for c in np.cumsum(sizes)]
    h = x
    for l in range(DEPTH):
        z = rms_norm(h, attn_norm[l])
        proj = z @ w_in[l]
        q, kc, vc, ksl, vsl, kwn, vwn, gates, u = jnp.split(proj, cuts, axis=-1)
        kv_shape = (b, s, N_KV, HEAD_DIM)
        a_out = nsa_attention(q.reshape(b, s, N_HEADS, HEAD_DIM),
                              kc.reshape(kv_shape), vc.reshape(kv_shape),
                              ksl.reshape(kv_shape), vsl.reshape(kv_shape),
                              kwn.reshape(kv_shape), vwn.reshape(kv_shape), gates,
                              cmp_k_pe[l], cmp_k_w1[l], cmp_k_w2[l],
                              cmp_v_pe[l], cmp_v_w1[l], cmp_v_w2[l])
        s_out = s5_ssm(u, ssm_lam_re[l], ssm_lam_im[l], ssm_log_dt[l], ssm_b_re[l], ssm_b_im[l],
                       ssm_c_re[l], ssm_c_im[l], ssm_d[l], ssm_w_glu[l])
        h = h + jnp.concatenate([a_out, s_out], axis=-1) @ w_out[l]
        h = h + peer_ffn(rms_norm(h, ffn_norm[l]), peer_w_q[l], peer_subkeys_1[l],
                         peer_subkeys_2[l], peer_u[l], peer_v[l])
    return rms_norm(h, final_norm)
```

```python
import math
import numpy as np
import concourse.bass as bass
import concourse.mybir as mybir
from concourse.bass_utils import run_bass_kernel_spmd
from contextlib import ExitStack

F32 = mybir.dt.float32
BF16 = mybir.dt.bfloat16
I32 = mybir.dt.int32
U32 = mybir.dt.uint32
AF = mybir.ActivationFunctionType
ALU = mybir.AluOpType
AX = mybir.AxisListType

NCORES = 8
SEQ = 16384
D = 1024
NT = 32
NOWN = 4
EPS = 1e-6
NGB = 16
PI = math.pi


class Sync:
    def __init__(self, nc, es):
        self.nc = nc
        self.engs = {"pe": nc.tensor, "act": nc.scalar, "dve": nc.vector,
                     "pool": nc.gpsimd, "sp": nc.sync}
        self.sem = {k: es.enter_context(nc.semaphore("sem_" + k)) for k in self.engs}
        self.cnt = {k: 0 for k in self.engs}
        self.dsem = [es.enter_context(nc.semaphore(f"dsem{i}")) for i in range(32)]
        self.dcnt = [0] * len(self.dsem)
        self.qsl = {"sp": list(range(0, 24)), "act": list(range(0, 24)), "pool": list(range(24, 32))}
        self.drr = {"sp": 0, "act": 0, "pool": 0}
        self.waited = {k: {} for k in self.engs}
        self.lastw = {}
        self.reads = {}

    def _wait(self, e, ev):
        if ev is None:
            return
        sem, val, src = ev
        if src == e and e == "pe":
            return
        key = id(sem)
        if self.waited[e].get(key, 0) >= val:
            return
        self.engs[e].wait_ge(sem, val)
        self.waited[e][key] = val

    def deps(self, e, reads, writes):
        for b in reads:
            self._wait(e, self.lastw.get(b))
        for b in writes:
            self._wait(e, self.lastw.get(b))
            for ev in self.reads.get(b, []):
                self._wait(e, ev)

    def commit(self, ev, reads, writes):
        for b in reads:
            self.reads.setdefault(b, []).append(ev)
        for b in writes:
            self.lastw[b] = ev
            self.reads[b] = []

    def op(self, e, inst_fn, reads=(), writes=()):
        self.deps(e, reads, writes)
        inst = inst_fn(self.engs[e])
        self.cnt[e] += 1
        inst.then_inc(self.sem[e], 1)
        ev = (self.sem[e], self.cnt[e], e)
        self.commit(ev, reads, writes)
        return ev

    def dma(self, e, inst_fn, reads=(), writes=()):
        self.deps(e, reads, writes)
        sl = self.qsl[e]
        k = sl[self.drr[e] % len(sl)]
        self.drr[e] += 1
        inst = inst_fn(self.engs[e])
        self.dcnt[k] += 16
        inst.then_inc(self.dsem[k], 16)
        ev = (self.dsem[k], self.dcnt[k], "dma")
        self.commit(ev, reads, writes)
        return ev

    def barrier(self):
        for e in self.engs:
            for e2 in self.engs:
                if e2 != e and self.cnt[e2]:
                    self._wait(e, (self.sem[e2], self.cnt[e2], e2))
            for k in range(len(self.dsem)):
                if self.dcnt[k]:
                    self._wait(e, (self.dsem[k], self.dcnt[k], "dma"))
        self.lastw = {}
        self.reads = {}

    def finish(self):
        for k in range(len(self.dsem)):
            if self.dcnt[k]:
                self._wait("sp", (self.dsem[k], self.dcnt[k], "dma"))
        for e2 in self.engs:
            if e2 != "sp" and self.cnt[e2]:
                self._wait("sp", (self.sem[e2], self.cnt[e2], e2))


HD = 64
ROPE_THETA = 500000.0


def _rope_tables_T(pos):
    pos = np.asarray(pos, np.float32)
    inv = (ROPE_THETA ** (-np.arange(8, dtype=np.float32) * 2.0 / 16.0)).astype(np.float32)
    ang = pos[None, :] * inv[:, None]
    c, s = np.cos(ang).astype(np.float32), np.sin(ang).astype(np.float32)
    C = np.ones((64, len(pos)), np.float32)
    S = np.zeros((64, len(pos)), np.float32)
    C[0:8] = c
    C[8:16] = c
    S[0:8] = -s
    S[8:16] = s
    return np.concatenate([C, C], 0), np.concatenate([S, S], 0)


def _swap_cols(cols):
    cols = np.asarray(cols).reshape(-1, 64).copy()
    out = cols.copy()
    out[:, 0:8] = cols[:, 8:16]
    out[:, 8:16] = cols[:, 0:8]
    return out.reshape(-1)


Q0, KC0, VC0, KS0, VS0, KW0, VW0, GT0, U0 = 0, 512, 640, 768, 896, 1024, 1152, 1280, 1304
NWA = 1152
NWB = 2944


def _prep(inputs):
    f = lambda k: np.ascontiguousarray(np.asarray(inputs[k], dtype=np.float32))
    x = f("x").reshape(SEQ, D)
    w_in = f("w_in")[0]
    ar = np.arange
    ks_c, kc_c, vc_c, vs_c = KS0 + ar(128), KC0 + ar(128), VC0 + ar(128), VS0 + ar(128)
    kw_c, vw_c, u_c = KW0 + ar(128), VW0 + ar(128), U0 + ar(512)
    colsA = np.concatenate([ks_c, _swap_cols(ks_c), kc_c, vc_c, vs_c, u_c])
    q_c = np.concatenate([np.concatenate([Q0 + 64 * hl + ar(64), Q0 + 64 * (4 + hl) + ar(64)])
                          for hl in range(4)])
    g_c = np.concatenate([GT0 + ar(24), np.full(104, GT0)])
    colsB = np.concatenate([q_c, _swap_cols(q_c), ks_c, _swap_cols(ks_c), kw_c, _swap_cols(kw_c),
                            u_c, vs_c, vw_c, g_c, u_c])
    assert len(colsA) == NWA and len(colsB) == NWB
    com = {}
    com["x_all"] = x.reshape(NT, 512, D)
    com["wA"] = np.ascontiguousarray(w_in[:, colsA])
    com["wB"] = np.ascontiguousarray(w_in[:, colsB])
    com["gA"] = np.ascontiguousarray(f("attn_norm")[0].reshape(8, 128).T)
    com["ident"] = np.eye(128, dtype=np.float32)
    C, S = _rope_tables_T(np.arange(SEQ))
    com["ropeC"] = np.ascontiguousarray(C.reshape(128, NT, 512).transpose(1, 0, 2))
    com["ropeS"] = np.ascontiguousarray(S.reshape(128, NT, 512).transpose(1, 0, 2))
    cmp_end = np.arange(1024) * 16 + 31
    Cc, Sc = _rope_tables_T(cmp_end)
    com["cmpC"], com["cmpS"] = Cc, Sc
    for kv in ("k", "v"):
        w1 = f(f"cmp_{kv}_w1")[0].reshape(32, 64, 128).transpose(1, 0, 2)
        com[f"w1{kv}"] = np.ascontiguousarray(np.concatenate([w1, w1], 0))
        com[f"pe{kv}T"] = np.ascontiguousarray(np.concatenate([f(f"cmp_{kv}_pe")[0].T] * 2, 0))
        w2 = f(f"cmp_{kv}_w2")[0]
        z = np.zeros_like(w2)
        com[f"w2{kv}"] = np.ascontiguousarray(np.stack(
            [np.concatenate([w2, z], 1), np.concatenate([z, w2], 1)], 1))
        if kv == "k":
            w2s = w2[:, _swap_cols(np.arange(64))]
            com["w2ksw"] = np.ascontiguousarray(np.stack(
                [np.concatenate([w2s, z], 1), np.concatenate([z, w2s], 1)], 1))
    def fm(a):
        a = a.reshape((16, 2) + a.shape[1:])
        a = np.moveaxis(a, 0, 2)
        return np.ascontiguousarray(a.reshape((128, 16) + a.shape[3:]))
    lam_re, lam_im = f("ssm_lam_re")[0], f("ssm_lam_im")[0]
    log_dt = f("ssm_log_dt")[0]
    com["lamre_f"], com["lamim_f"] = fm(lam_re), fm(lam_im)
    com["logdt_f"] = fm(np.repeat(log_dt[:, None], 64, 1))
    com["bre_f"], com["bim_f"] = fm(f("ssm_b_re")[0]), fm(f("ssm_b_im")[0])
    com["cre_f"] = fm(f("ssm_c_re")[0].transpose(0, 2, 1))
    com["cim_f"] = fm(f("ssm_c_im")[0].transpose(0, 2, 1))
    def rowb(a):
        return np.ascontiguousarray(np.broadcast_to(fm(a).transpose(1, 0)[None], (128, 16, 128)))
    com["lamre_r"], com["lamim_r"] = rowb(lam_re), rowb(lam_im)
    com["logdt_r"] = rowb(np.repeat(log_dt[:, None], 64, 1))
    com["tcol"] = np.ascontiguousarray(np.broadcast_to((127 - np.arange(128, dtype=np.float32))[:, None], (128, 1)))
    com["trow"] = np.ascontiguousarray(np.broadcast_to(np.arange(1, 129, dtype=np.float32)[None], (128, 128)))
    com["final_norm"] = f("final_norm").reshape(1, D)
    import ml_dtypes
    bf = lambda a: np.ascontiguousarray(np.asarray(a, np.float32).astype(ml_dtypes.bfloat16))
    cc = np.arange(8192)
    com["Fbase"] = bf((cc[None, :] // 64) == np.arange(128)[:, None])
    n_all = np.arange(1024)
    m_all = np.arange(256)
    ov = ((16 * n_all[:, None] < 64 * m_all[None, :] + 64) & (16 * n_all[:, None] + 32 > 64 * m_all[None, :]))
    ov[1023] = False
    com["OV"] = bf(ov.reshape(8, 128, 256).transpose(1, 0, 2))
    kk_, tt_ = np.arange(128)[:, None], np.arange(128)[None, :]
    com["mWM0"] = bf(kk_ > tt_)
    com["mTRI"] = bf(kk_ <= tt_)
    com["mLMA"] = bf((kk_ >= 64) & (tt_ < 64))
    com["blk64"] = np.ascontiguousarray(np.broadcast_to((64.0 * np.arange(256, dtype=np.float32))[None], (128, 256)))
    com["cmpend"] = np.ascontiguousarray((16.0 * n_all + 31.0).astype(np.float32).reshape(8, 128).T)
    com["dskip"] = np.ascontiguousarray(np.broadcast_to(f("ssm_d")[0][None], (128, 512)))
    com["wglu"] = f("ssm_w_glu")[0]
    com["wout"] = f("w_out")[0]
    com["ffn_norm"] = f("ffn_norm")[0].reshape(1, D)
    com["wq"] = f("peer_w_q")[0]
    com["sub1T"] = np.ascontiguousarray(f("peer_subkeys_1")[0].T)
    com["sub2T"] = np.ascontiguousarray(f("peer_subkeys_2")[0].T)
    com["peer_u"] = f("peer_u")[0]
    com["peer_v"] = f("peer_v")[0]
    per = []
    for c in range(NCORES):
        d = {}
        tiles = [8 * j + c for j in range(NOWN)]
        xl = np.zeros((NOWN, 1024, D), np.float32)
        for j, T in enumerate(tiles):
            lo = 512 * T - 512
            if lo >= 0:
                xl[j] = x[lo:lo + 1024]
            else:
                xl[j, 512:] = x[0:512]
        d["x_loc"] = xl
        oh = np.zeros((128, NOWN, NT), np.float32)
        for j, T in enumerate(tiles):
            oh[:, j, T] = 1.0
        d["onehot"] = oh
        rCl = np.zeros((NOWN, 2, 128, 512), np.float32)
        rSl = np.zeros((NOWN, 2, 128, 512), np.float32)
        tpB = np.zeros((NOWN, 128, 512), np.float32)
        tpt = np.zeros((128, 16), np.float32)
        vW = np.zeros((128, 16, 5), np.float32)
        vA = np.zeros((128, 16), np.float32)
        for j, T in enumerate(tiles):
            pos = 512 * T - 512 + np.arange(1024)
            Cl, Sl = _rope_tables_T(np.maximum(pos, 0))
            rCl[j] = Cl.reshape(128, 2, 512).transpose(1, 0, 2)
            rSl[j] = Sl.reshape(128, 2, 512).transpose(1, 0, 2)
            tpB[j] = np.broadcast_to((512 * T + np.arange(512)).astype(np.float32)[None], (128, 512))
            for qb in range(4):
                t0 = 512 * T + 128 * qb
                tpt[:, 4 * j + qb] = t0 + np.arange(128)
                for c5 in range(5):
                    vW[:, 4 * j + qb, c5] = 1.0 if t0 - 512 + 128 * c5 >= 0 else 0.0
                vA[:, 4 * j + qb] = 1.0 if t0 - 128 >= 0 else 0.0
        d.update(ropeCl=rCl, ropeSl=rSl, tposB=tpB, tpos_tm=tpt, tposm128=tpt - 128.0, validW=vW, validA=vA)
        per.append(d)
    return com, per


DBG = {}


def build_nc(debug=None):
    nc = bass.Bass("TRN2", target_bir_lowering=False, num_swdge_queues=DBG.get("swq", 4))
    dins = {}

    def din(name, shape, dt=F32):
        dins[name] = nc.dram_tensor(name, list(shape), dt, kind="ExternalInput").ap()
        return dins[name]

    x_all = din("x_all", [NT, 512, D])
    x_loc = din("x_loc", [NOWN, 1024, D])
    wA = din("wA", [D, NWA])
    wB = din("wB", [D, NWB])
    gA = din("gA", [128, 8])
    ident = din("ident", [128, 128])
    ropeC = din("ropeC", [NT, 128, 512])
    ropeS = din("ropeS", [NT, 128, 512])
    cmpC = din("cmpC", [128, 1024])
    cmpS = din("cmpS", [128, 1024])
    w1d = {kv: din(f"w1{kv}", [128, 32, 128]) for kv in "kv"}
    peTd = {kv: din(f"pe{kv}T", [128, 32]) for kv in "kv"}
    w2d = {"k": din("w2k", [128, 2, 128]), "v": din("w2v", [128, 2, 128]), "ksw": din("w2ksw", [128, 2, 128])}
    ssm_f = {n: din(n, [128, 16]) for n in ("lamre_f", "lamim_f", "logdt_f")}
    ssm_b = {n: din(n, [128, 16, 16]) for n in ("bre_f", "bim_f", "cre_f", "cim_f")}
    ssm_r = {n: din(n, [128, 16, 128]) for n in ("lamre_r", "lamim_r", "logdt_r")}
    tcol = din("tcol", [128, 1])
    trow = din("trow", [128, 128])
    onehot = din("onehot", [128, NOWN, NT])
    fnorm = din("final_norm", [1, D])
    Fbase_d = din("Fbase", [128, 8192], BF16)
    OV_d = din("OV", [128, 8, 256], BF16)
    mWM0_d, mTRI_d, mLMA_d = din("mWM0", [128, 128], BF16), din("mTRI", [128, 128], BF16), din("mLMA", [128, 128], BF16)
    blk64_d = din("blk64", [128, 256])
    cmpend_d = din("cmpend", [128, 8])
    dskip_d = din("dskip", [128, 512])
    wglu_d = din("wglu", [512, 1024])
    wout_d = din("wout", [1024, 1024])
    ropeCl = din("ropeCl", [NOWN, 2, 128, 512])
    ropeSl = din("ropeSl", [NOWN, 2, 128, 512])
    tposB_d = din("tposB", [NOWN, 128, 512])
    tpos_d = din("tpos_tm", [128, 16])
    tposm_d = din("tposm128", [128, 16])
    validW_d = din("validW", [128, 16, 5])
    validA_d = din("validA", [128, 16])
    h1d = nc.dram_tensor("h1scr", [16, 128, D], F32, kind="Internal").ap()
    uvb = nc.dram_tensor("uvtab_bf", [16384, 2 * D], BF16, kind="Internal").ap()
    wBb = nc.dram_tensor("wB_bf", [NWB // 128, 128, 8, 128], BF16, kind="Internal").ap()
    gffn_d = din("ffn_norm", [1, D])
    wq_d = din("wq", [D, 2048])
    sub1T_d = din("sub1T", [128, 128])
    sub2T_d = din("sub2T", [128, 128])
    utab = din("peer_u", [16384, D])
    vtab = din("peer_v", [16384, D])
    y = nc.dram_tensor("y", [NOWN, 512, D], F32, kind="ExternalOutput").ap()
    dbg_out = {}

    def dout(name, shape):
        dbg_out[name] = nc.dram_tensor("dbg_" + name, list(shape), F32, kind="ExternalOutput").ap()
        return dbg_out[name]

    with ExitStack() as es:
        S = Sync(nc, es)
        _nctr = [0]

        def sb(name, shape, dt, st=es):
            _nctr[0] += 1
            return st.enter_context(nc.sbuf_tensor(f"s{_nctr[0]}_{name}", list(shape), dt))
        pbank = [es.enter_context(nc.psum_tensor(f"pb{i}", [128, 512], F32)) for i in range(8)]

        def DMA(out, in_, r=(), w=(), q="sp"):
            return S.dma(q, lambda e: e.dma_start(out=out, in_=in_), reads=r, writes=w)

        def V(fn, r=(), w=(), e="dve"):
            return S.op(e, fn, reads=r, writes=w)

        def CP(out, in_, r=(), w=(), e="dve"):
            if e == "act":
                return S.op(e, lambda en: en.copy(out=out, in_=in_), reads=r, writes=w)
            return S.op(e, lambda en: en.tensor_copy(out=out, in_=in_), reads=r, writes=w)

        def MM(out, lhsT, rhs, start, stop, r=(), w=()):
            return S.op("pe", lambda e: e.matmul(out=out, lhsT=lhsT, rhs=rhs, start=start, stop=stop, skip_group_check=True),
                        reads=r, writes=w)

        identf = sb("identf", [128, 128], F32)
        identb = sb("identb", [128, 128], BF16)
        gAs = sb("gAs", [128, 8], F32)
        sctx = ExitStack()
        ksT = sb("ksT", [128, SEQ], BF16, sctx)
        vsaug = sb("vsaug", [128, 128, 2, 65], BF16, sctx)
        kcT = sb("kcT", [128, 1024], BF16, sctx)
        vcT = sb("vcT", [128, 1024], BF16, sctx)
        Xtile = sb("Xtile", [128, NT, 32], F32, sctx)
        kap = sb("kap", [128, 2, 16], F32, sctx)
        Bbar = sb("Bbar", [128, 2, 16, 16], F32, sctx)
        af = sb("af", [128, 16], F32, sctx)
        thf = sb("thf", [128, 16], F32, sctx)
        DMA(identf[:], ident, w=["identf"])
        DMA(gAs[:], gA, w=["gAs"])
        V(lambda e: e.tensor_copy(out=identb[:], in_=identf[:]), r=["identf"], w=["identb"])
        V(lambda e: e.memset(vsaug[:], 1.0), w=["vsaug"], e="pool")
        V(lambda e: e.memset(kcT[:], 0.0), w=["kcT"], e="pool")
        V(lambda e: e.memset(vcT[:], 0.0), w=["vcT"], e="pool")

        with ExitStack() as sa:
            sba = lambda name, shape, dt: sb(name, shape, dt, sa)
            Wa = sba("Wa", [128, 8, NWA], BF16)
            W1 = {kv: sba(f"W1{kv}", [128, 32, 128], BF16) for kv in "kv"}
            biasc = {kv: sba(f"biasc{kv}", [128, 1], F32) for kv in "kv"}
            W2 = {nm: sba(f"W2{nm}", [128, 2, 128], BF16) for nm in ("k", "ksw", "v")}
            L128 = sba("L128", [128, 2, 16], F32)
            Bs1 = sba("Bs1", [128, 16, 2, 32], F32)
            Bs2 = sba("Bs2", [128, 16, 2, 32], F32)
            Gt = sba("Gt", [128, 2, 16, 128], BF16)
            sp_ = sa.enter_context(ExitStack())
            sba_persist = sba
            sba = lambda name, shape, dt: sb(name, shape, dt, sp_)
            wst = [sba(f"wst{i}", [128, 8, 128], F32) for i in range(2)]
            for ci, c0 in enumerate(range(0, NWA, 128)):
                w_ = wst[ci % 2]
                DMA(w_[:], wA[:, c0:c0 + 128].rearrange("(c p) n -> p c n", p=128), w=[f"wst{ci % 2}"])
                for dc in range(8):
                    V(lambda e: e.tensor_scalar(out=Wa[:, dc, c0:c0 + 128], in0=w_[:, dc, :],
                                                scalar1=gAs[:, dc:dc + 1], scalar2=None, op0=ALU.mult),
                      r=[f"wst{ci % 2}", "gAs"], w=["Wa"], e=("dve" if dc % 2 else "pool"))
            wbo = [sba(f"wbo{i}", [128, 8, 128], BF16) for i in range(2)]
            for ci, c0 in enumerate(range(0, NWB, 128)):
                w_ = wst[ci % 2]
                o_ = wbo[ci % 2]
                DMA(w_[:], wB[:, c0:c0 + 128].rearrange("(c p) n -> p c n", p=128), w=[f"wst{ci % 2}"])
                for dc in range(8):
                    V(lambda e: e.tensor_scalar(out=o_[:, dc, :], in0=w_[:, dc, :], scalar1=gAs[:, dc:dc + 1], scalar2=None, op0=ALU.mult),
                      r=[f"wst{ci % 2}", "gAs"], w=[f"wbo{ci % 2}"], e=("dve" if dc % 2 else "pool"))
                DMA(wBb[ci], o_[:], r=[f"wbo{ci % 2}"], w=["wBb"])
            w1st = sba("w1st", [128, 16, 128], F32)
            w2st = sba("w2st", [128, 2, 128], F32)
            pest = sba("pest", [128, 32], F32)
            peb = sba("peb", [128, 32], BF16)
            for kv in "kv":
                for hf in range(2):
                    DMA(w1st[:], w1d[kv][:, 16 * hf:16 * hf + 16, :], w=["w1st"])
                    V(lambda e: e.tensor_copy(out=W1[kv][:, 16 * hf:16 * hf + 16, :], in_=w1st[:]), r=["w1st"], w=[f"W1{kv}"])
                DMA(pest[:], peTd[kv], w=["pest"])
                V(lambda e: e.tensor_copy(out=peb[:], in_=pest[:]), r=["pest"], w=["peb"])
                for l in range(32):
                    MM(pbank[5][:, 0:1], W1[kv][0:64, l, :], peb[0:64, l:l + 1], l == 0, l == 31,
                       r=[f"W1{kv}", "peb"], w=["pb5"])
                V(lambda e: e.tensor_copy(out=biasc[kv][:], in_=pbank[5][:, 0:1]), r=["pb5"], w=[f"biasc{kv}"])
            for nm in ("k", "ksw", "v"):
                DMA(w2st[:], w2d[nm], w=["w2st"])
                V(lambda e: e.tensor_copy(out=W2[nm][:], in_=w2st[:]), r=["w2st"], w=[f"W2{nm}"])

            sf = {n: sba(n, [128, 16], F32) for n in ssm_f}
            for n in ssm_f:
                DMA(sf[n][:], ssm_f[n], w=[n])
            bf_ = {n: sba(n, [128, 16, 16], F32) for n in ("bre_f", "bim_f")}
            for n in bf_:
                DMA(bf_[n][:], ssm_b[n], w=[n])
            dtf = sba("dtf", [128, 16], F32)
            V(lambda e: e.activation(out=dtf[:], in_=sf["logdt_f"][:], func=AF.Exp), r=["logdt_f"], w=["dtf"], e="act")
            V(lambda e: e.tensor_tensor(out=af[:], in0=sf["lamre_f"][:], in1=dtf[:], op=ALU.mult), r=["lamre_f", "dtf"], w=["af"])
            V(lambda e: e.tensor_tensor(out=thf[:], in0=sf["lamim_f"][:], in1=dtf[:], op=ALU.mult), r=["lamim_f", "dtf"], w=["thf"])

            tmpi = sba("tmpi", [128, 1024], I32)
            tmpa = sba("tmpa", [128, 1024], F32)
            tmpb = sba("tmpb", [128, 1024], F32)

            def sincos(out_ap, ang_ap, n, shift, key_out, key_ang):
                A_, B_, I_ = tmpa[:, 0:n], tmpb[:, 0:n], tmpi[:, 0:n]
                V(lambda e: e.tensor_scalar(out=A_, in0=ang_ap, scalar1=shift, scalar2=1.0 / (2 * PI),
                                            op0=ALU.add, op1=ALU.mult), r=[key_ang], w=["tmpa"])
                V(lambda e: e.tensor_copy(out=I_, in_=A_), r=["tmpa"], w=["tmpi"])
                V(lambda e: e.tensor_copy(out=B_, in_=I_), r=["tmpi"], w=["tmpb"])
                V(lambda e: e.tensor_tensor(out=A_, in0=A_, in1=B_, op=ALU.subtract), r=["tmpa", "tmpb"], w=["tmpa"])
                V(lambda e: e.tensor_scalar(out=B_, in0=A_, scalar1=0.5, scalar2=None, op0=ALU.is_gt),
                  r=["tmpa"], w=["tmpb"])
                V(lambda e: e.tensor_tensor(out=A_, in0=A_, in1=B_, op=ALU.subtract), r=["tmpa", "tmpb"], w=["tmpa"])
                V(lambda e: e.tensor_scalar(out=B_, in0=A_, scalar1=-0.5, scalar2=None, op0=ALU.is_lt),
                  r=["tmpa"], w=["tmpb"])
                V(lambda e: e.tensor_tensor(out=A_, in0=A_, in1=B_, op=ALU.add), r=["tmpa", "tmpb"], w=["tmpa"])
                V(lambda e: e.activation(out=out_ap, in_=A_, func=AF.Sin, scale=2 * PI), r=["tmpa"], w=[key_out], e="act")

            mag128 = sba("mag128", [128, 16], F32)
            ang128 = sba("ang128", [128, 16], F32)
            V(lambda e: e.activation(out=mag128[:], in_=af[:], func=AF.Exp, scale=128.0), r=["af"], w=["mag128"], e="act")
            V(lambda e: e.tensor_scalar(out=ang128[:], in0=thf[:], scalar1=128.0, scalar2=None, op0=ALU.mult), r=["thf"], w=["ang128"])
            sincos(L128[:, 0, :], ang128[:], 16, PI / 2, "L128c", "ang128")
            sincos(L128[:, 1, :], ang128[:], 16, 0.0, "L128s", "ang128")
            V(lambda e: e.tensor_tensor(out=L128[:, 0, :], in0=L128[:, 0, :], in1=mag128[:], op=ALU.mult), r=["L128c", "mag128"], w=["L128c"])
            V(lambda e: e.tensor_tensor(out=L128[:, 1, :], in0=L128[:, 1, :], in1=mag128[:], op=ALU.mult), r=["L128s", "mag128"], w=["L128s"])
            L1 = sba("L1", [128, 2, 16], F32)
            mag1 = sba("mag1", [128, 16], F32)
            V(lambda e: e.activation(out=mag1[:], in_=af[:], func=AF.Exp), r=["af"], w=["mag1"], e="act")
            sincos(L1[:, 0, :], thf[:], 16, PI / 2, "L1c", "thf")
            sincos(L1[:, 1, :], thf[:], 16, 0.0, "L1s", "thf")
            V(lambda e: e.tensor_tensor(out=L1[:, 0, :], in0=L1[:, 0, :], in1=mag1[:], op=ALU.mult), r=["L1c", "mag1"], w=["L1c"])
            V(lambda e: e.tensor_tensor(out=L1[:, 1, :], in0=L1[:, 1, :], in1=mag1[:], op=ALU.mult), r=["L1s", "mag1"], w=["L1s"])
            t1 = sba("t1", [128, 16], F32)
            t2 = sba("t2", [128, 16], F32)
            den = sba("den", [128, 16], F32)
            lr, li = sf["lamre_f"], sf["lamim_f"]
            V(lambda e: e.tensor_tensor(out=den[:], in0=lr[:], in1=lr[:], op=ALU.mult), r=["lamre_f"], w=["den"])
            V(lambda e: e.tensor_tensor(out=t1[:], in0=li[:], in1=li[:], op=ALU.mult), r=["lamim_f"], w=["t1"])
            V(lambda e: e.tensor_tensor(out=den[:], in0=den[:], in1=t1[:], op=ALU.add), r=["den", "t1"], w=["den"])
            V(lambda e: e.reciprocal(out=den[:], in_=den[:]), r=["den"], w=["den"])
            V(lambda e: e.tensor_scalar(out=t1[:], in0=L1[:, 0, :], scalar1=-1.0, scalar2=None, op0=ALU.add), r=["L1c"], w=["t1"])
            V(lambda e: e.tensor_tensor(out=kap[:, 0, :], in0=t1[:], in1=lr[:], op=ALU.mult), r=["t1", "lamre_f"], w=["kapr"])
            V(lambda e: e.tensor_tensor(out=t2[:], in0=L1[:, 1, :], in1=li[:], op=ALU.mult), r=["L1s", "lamim_f"], w=["t2"])
            V(lambda e: e.tensor_tensor(out=kap[:, 0, :], in0=kap[:, 0, :], in1=t2[:], op=ALU.add), r=["kapr", "t2"], w=["kapr"])
            V(lambda e: e.tensor_tensor(out=kap[:, 0, :], in0=kap[:, 0, :], in1=den[:], op=ALU.mult), r=["kapr", "den"], w=["kapr"])
            V(lambda e: e.tensor_tensor(out=kap[:, 1, :], in0=L1[:, 1, :], in1=lr[:], op=ALU.mult), r=["L1s", "lamre_f"], w=["kapi"])
            V(lambda e: e.tensor_tensor(out=t2[:], in0=t1[:], in1=li[:], op=ALU.mult), r=["t1", "lamim_f"], w=["t2"])
            V(lambda e: e.tensor_tensor(out=kap[:, 1, :], in0=kap[:, 1, :], in1=t2[:], op=ALU.subtract), r=["kapi", "t2"], w=["kapi"])
            V(lambda e: e.tensor_tensor(out=kap[:, 1, :], in0=kap[:, 1, :], in1=den[:], op=ALU.mult), r=["kapi", "den"], w=["kapi"])
            tb1 = sba("tb1", [128, 16, 16], F32)
            kr_b = kap[:, 0, :].unsqueeze(2).to_broadcast([128, 16, 16])
            ki_b = kap[:, 1, :].unsqueeze(2).to_broadcast([128, 16, 16])
            V(lambda e: e.tensor_tensor(out=Bbar[:, 0], in0=bf_["bre_f"][:], in1=kr_b, op=ALU.mult), r=["bre_f", "kapr"], w=["Bbr"])
            V(lambda e: e.tensor_tensor(out=tb1[:], in0=bf_["bim_f"][:], in1=ki_b, op=ALU.mult), r=["bim_f", "kapi"], w=["tb1"])
            V(lambda e: e.tensor_tensor(out=Bbar[:, 0], in0=Bbar[:, 0], in1=tb1[:], op=ALU.subtract), r=["Bbr", "tb1"], w=["Bbr"])
            V(lambda e: e.tensor_tensor(out=Bbar[:, 1], in0=bf_["bim_f"][:], in1=kr_b, op=ALU.mult), r=["bim_f", "kapr"], w=["Bbi"])
            V(lambda e: e.tensor_tensor(out=tb1[:], in0=bf_["bre_f"][:], in1=ki_b, op=ALU.mult), r=["bre_f", "kapi"], w=["tb1"])
            V(lambda e: e.tensor_tensor(out=Bbar[:, 1], in0=Bbar[:, 1], in1=tb1[:], op=ALU.add), r=["Bbi", "tb1"], w=["Bbi"])
            V(lambda e: e.memset(Bs1[:], 0.0), w=["Bs1"], e="pool")
            V(lambda e: e.memset(Bs2[:], 0.0), w=["Bs2"], e="pool")
            for glo in range(2):
                ps_ = slice(64 * glo, 64 * glo + 64)
                cs_ = slice(16 * glo, 16 * glo + 16)
                V(lambda e: e.tensor_copy(out=Bs1[ps_, :, 0, cs_], in_=Bbar[ps_, 0]), r=["Bbr"], w=["Bs1"])
                V(lambda e: e.tensor_scalar(out=Bs1[ps_, :, 1, cs_], in0=Bbar[ps_, 1], scalar1=-1.0, scalar2=None, op0=ALU.mult), r=["Bbi"], w=["Bs1"])
                V(lambda e: e.tensor_copy(out=Bs2[ps_, :, 0, cs_], in_=Bbar[ps_, 1]), r=["Bbi"], w=["Bs2"])
                V(lambda e: e.tensor_copy(out=Bs2[ps_, :, 1, cs_], in_=Bbar[ps_, 0]), r=["Bbr"], w=["Bs2"])
            rrb = sba("rrb", [128, 8, 128], F32)
            tcs = sba("tcs", [128, 1], F32)
            DMA(tcs[:], tcol, w=["tcs"])
            dtr = sba("dtr", [128, 1024], F32)
            ar_ = sba("ar_", [128, 1024], F32)
            magr = sba("magr", [128, 1024], F32)
            trg = sba("trg", [128, 1024], F32)
            rrf = rrb[:].rearrange("p a b -> p (a b)")
            for kh in range(2):
                ksl = slice(8 * kh, 8 * kh + 8)
                DMA(rrb[:], ssm_r["logdt_r"][:, ksl, :], w=["rrb"])
                V(lambda e: e.activation(out=dtr[:], in_=rrf, func=AF.Exp), r=["rrb"], w=["dtr"], e="act")
                DMA(rrb[:], ssm_r["lamre_r"][:, ksl, :], w=["rrb"])
                V(lambda e: e.tensor_tensor(out=ar_[:], in0=rrf, in1=dtr[:], op=ALU.mult), r=["rrb", "dtr"], w=["ar_"])
                V(lambda e: e.activation(out=magr[:], in_=ar_[:], func=AF.Exp, scale=tcs[:, 0:1]), r=["ar_", "tcs"], w=["magr"], e="act")
                DMA(rrb[:], ssm_r["lamim_r"][:, ksl, :], w=["rrb"])
                V(lambda e: e.tensor_tensor(out=ar_[:], in0=rrf, in1=dtr[:], op=ALU.mult), r=["rrb", "dtr", "magr"], w=["ar_"])
                V(lambda e: e.tensor_scalar(out=ar_[:], in0=ar_[:], scalar1=tcs[:, 0:1], scalar2=None, op0=ALU.mult), r=["ar_", "tcs"], w=["ar_"])
                for ri, sh in ((0, PI / 2), (1, 0.0)):
                    sincos(trg[:], ar_[:], 1024, sh, "trg", "ar_")
                    V(lambda e: e.tensor_tensor(out=Gt[:, ri, ksl, :].rearrange("p a b -> p (a b)"), in0=trg[:], in1=magr[:], op=ALU.mult),
                      r=["trg", "magr"], w=["Gt"])
            S.barrier()
            sp_.close()
            sba = sba_persist

            xt = [sba(f"xt{i}", [128, 4, D], F32) for i in range(2)]
            xs = sba("xs", [128, 4, D], BF16)
            ss = sba("ss", [128, 4], F32)
            rstd = sba("rstd", [128, 4], F32)
            zT = sba("zT", [128, 8, 512], BF16)
            rC = [sba(f"rC{i}", [128, 512], F32) for i in range(2)]
            rS = [sba(f"rS{i}", [128, 512], F32) for i in range(2)]
            rtmp = sba("rtmp", [128, 512], F32)
            craw = {kv: [sba(f"craw{kv}{i}", [128, 528], BF16) for i in range(2)] for kv in "kv"}
            utm = sba("utm", [128, 512], BF16)
            hid = sba("hid", [128, 4, 32], BF16)
            Eblk = sba("Eblk", [128, 32], F32)
            cC = sba("cC", [128, 32], F32)
            cS = sba("cS", [128, 32], F32)
            Xc = sba("Xc", [128, 32], F32)
            Xn = sba("Xn", [128, 32], F32)
            ta = sba("ta", [128, 16], F32)
            V(lambda e: e.memset(Xc[:], 0.0), w=["Xc"])
            Lr, Li = L128[:, 0, :], L128[:, 1, :]
            lk = ["L128c", "L128s"]
            Msb = sba("Msb", [128, 1024], F32)
            cmb1 = sba("cmb1", [128, 1024], F32)
            cmb2 = sba("cmb2", [128, 1024], F32)
            for kv in "kv":
                for i in range(2):
                    V(lambda e: e.memset(craw[kv][i][:], 0.0), w=[f"craw{kv}{i}"], e="pool")

            def compress(Tc, nblk):
                pi_ = Tc % 2
                ph = pbank[5]
                for a, kv in enumerate("kv"):
                    for g in range(2):
                        col = 128 + 32 * (2 * a + g)
                        for l in range(32):
                            MM(ph[:, col:col + nblk], W1[kv][64 * g:64 * g + 64, l, :],
                               craw[kv][pi_][64 * g:64 * g + 64, l:l + 16 * (nblk - 1) + 1:16],
                               l == 0, l == 31, r=[f"W1{kv}", f"craw{kv}{pi_}", "hidall"], w=["pb5"])
                        if DBG.get("cstage", 9) < 1:
                            continue
                        V(lambda e: e.activation(out=hid[:, 2 * a + g, 0:nblk], in_=ph[:, col:col + nblk],
                                                 func=AF.Gelu_apprx_tanh, bias=biasc[kv][:, 0:1], scale=1.0),
                          r=["pb5", f"biasc{kv}"], w=[f"hid{a}{g}", "hidall"], e="act")
                n0 = 32 * Tc
                if DBG.get("cstage", 9) < 2:
                    return
                DMA(cC[:], cmpC[:, n0:n0 + 32], w=["cC"], q="pool")
                DMA(cS[:], cmpS[:, n0:n0 + 32], w=["cS"], q="pool")
                for j, nm in enumerate(("k", "ksw", "v")):
                    a = 0 if nm != "v" else 1
                    col = 256 + 32 * j
                    for g in range(2):
                        MM(ph[:, col:col + nblk], W2[nm][:, g, :], hid[:, 2 * a + g, 0:nblk], g == 0, g == 1,
                           r=[f"W2{nm}", f"hid{a}{g}"], w=["pb5"])
                if DBG.get("cstage", 9) < 3:
                    return
                V(lambda e: e.tensor_tensor(out=rtmp[:, 0:nblk], in0=ph[:, 256:256 + nblk], in1=cC[:, 0:nblk], op=ALU.mult),
                  r=["pb5", "cC"], w=["rtmp"])
                V(lambda e: e.tensor_tensor(out=rtmp[:, 32:32 + nblk], in0=ph[:, 288:288 + nblk], in1=cS[:, 0:nblk], op=ALU.mult),
                  r=["pb5", "cS"], w=["rtmp"])
                V(lambda e: e.tensor_tensor(out=kcT[:, n0:n0 + nblk], in0=rtmp[:, 0:nblk], in1=rtmp[:, 32:32 + nblk], op=ALU.add),
                  r=["rtmp"], w=["kcT"])
                V(lambda e: e.tensor_copy(out=vcT[:, n0:n0 + nblk], in_=ph[:, 320:320 + nblk]), r=["pb5"], w=["vcT"])

            for T in range(DBG.get("ntiles", NT)):
                p_ = T % 2
                x_ = xt[p_]
                DMA(x_[:], x_all[T].rearrange("(a p) d -> p a d", p=128), w=[f"xt{p_}"])
                DMA(rC[p_][:], ropeC[T], w=[f"rC{p_}"], q="pool")
                DMA(rS[p_][:], ropeS[T], w=[f"rS{p_}"], q="pool")
                for a in range(4):
                    V(lambda e: e.activation(out=xs[:, a, :], in_=x_[:, a, :], func=AF.Square, accum_out=ss[:, a:a + 1]),
                      r=[f"xt{p_}"], w=[f"xs{a}", "ss"], e="act")
                V(lambda e: e.activation(out=rstd[:], in_=ss[:], func=AF.Sqrt, bias=EPS, scale=1.0 / D),
                  r=["ss"], w=["rstd"], e="act")
                V(lambda e: e.reciprocal(out=rstd[:], in_=rstd[:]), r=["rstd"], w=["rstd"])
                for a in range(4):
                    V(lambda e: e.tensor_scalar(out=xs[:, a, :], in0=x_[:, a, :], scalar1=rstd[:, a:a + 1], scalar2=None, op0=ALU.mult),
                      r=[f"xt{p_}", "rstd"], w=[f"xs{a}"], e=("dve" if a % 2 else "pool"))
                for k2 in range(4):
                    pbb = pbank[k2][:].bitcast(BF16)
                    for dd in range(2):
                        dc = 2 * k2 + dd
                        for a in range(4):
                            S.op("pe", lambda e: e.transpose(out=pbb[:, dd * 512 + a * 128: dd * 512 + a * 128 + 128],
                                                             in_=xs[:, a, dc * 128:(dc + 1) * 128], identity=identb[:]),
                                 reads=[f"xs{a}", "identb"], writes=[f"pb{k2}"])
                    CP(zT[:, 2 * k2:2 * k2 + 2, :].rearrange("p a b -> p (a b)"), pbb,
                       r=[f"pb{k2}"], w=[f"zT{k2}"], e=("act" if k2 % 2 else "dve"))
                zk = [f"zT{k2}" for k2 in range(4)]
                for blk in range(4):
                    for dc in range(8):
                        MM(pbank[blk][:], Wa[:, dc, blk * 128:(blk + 1) * 128], zT[:, dc, :], dc == 0, dc == 7,
                           r=["Wa"] + zk, w=[f"pb{blk}"])
                V(lambda e: e.tensor_tensor(out=rtmp[:], in0=pbank[1][:], in1=rS[p_][:], op=ALU.mult), r=["pb1", f"rS{p_}"], w=["rtmp"])
                V(lambda e: e.tensor_tensor(out=rC[p_][:], in0=pbank[0][:], in1=rC[p_][:], op=ALU.mult), r=["pb0", f"rC{p_}"], w=[f"rC{p_}"])
                V(lambda e: e.tensor_tensor(out=ksT[:, 512 * T:512 * T + 512], in0=rC[p_][:], in1=rtmp[:], op=ALU.add),
                  r=[f"rC{p_}", "rtmp"], w=["ksT"])
                for a, kv in enumerate("kv"):
                    V(lambda e: e.activation(out=craw[kv][p_][:, 0:512], in_=pbank[2 + a][:], func=AF.Copy),
                      r=[f"pb{2 + a}"], w=[f"craw{kv}{p_}"], e="act")
                    if T > 0:
                        V(lambda e: e.tensor_copy(out=craw[kv][1 - p_][:, 512:528], in_=craw[kv][p_][:, 0:16]),
                          r=[f"craw{kv}{p_}"], w=[f"craw{kv}{1 - p_}"], e="pool")
                for a in range(0 if not DBG.get("nossm") else 4, 4):
                    b = 4 * T + a
                    for dc in range(8):
                        MM(pbank[5][:, 0:128], zT[:, dc, a * 128:(a + 1) * 128], Wa[:, dc, 512:640], dc == 0, dc == 7,
                           r=["Wa"] + zk, w=["pb5"])
                    CP(vsaug[:, b, :, 0:64], pbank[5][:, 0:128].rearrange("p (g d) -> p g d", g=2),
                       r=["pb5"], w=["vsaug"], e="act")
                    for dc in range(8):
                        MM(pbank[4][:], zT[:, dc, a * 128:(a + 1) * 128], Wa[:, dc, 640:1152], dc == 0, dc == 7,
                           r=["Wa"] + zk, w=["pb4"])
                    V(lambda e: e.tensor_copy(out=utm[:], in_=pbank[4][:]), r=["pb4"], w=["utm"])
                    for k in range(16):
                        for ri in range(2):
                            bank = pbank[6 + k // 8]
                            c0 = (k % 8) * 64 + ri * 32
                            MM(bank[:, c0:c0 + 32], Gt[:, ri, k, :], utm[:, 32 * k:32 * k + 32], True, True,
                               r=["Gt", "utm"], w=[f"pb{6 + k // 8}"])
                    for hh in range(2):
                        CP(Msb[:, hh * 512:(hh + 1) * 512], pbank[6 + hh][:], r=[f"pb{6 + hh}"], w=[f"Msb{hh}"], e="act")
                        V(lambda e: e.tensor_tensor(out=cmb1[:, hh * 512:(hh + 1) * 512], in0=Msb[:, hh * 512:(hh + 1) * 512],
                                                    in1=Bs1[:, 8 * hh:8 * hh + 8].rearrange("p a b c -> p (a b c)"), op=ALU.mult),
                          r=[f"Msb{hh}", "Bs1"], w=["cmb1"])
                        V(lambda e: e.tensor_tensor(out=cmb2[:, hh * 512:(hh + 1) * 512], in0=Msb[:, hh * 512:(hh + 1) * 512],
                                                    in1=Bs2[:, 8 * hh:8 * hh + 8].rearrange("p a b c -> p (a b c)"), op=ALU.mult),
                          r=[f"Msb{hh}", "Bs2"], w=["cmb2"], e="pool")
                    V(lambda e: e.tensor_reduce(out=Eblk[:, 0:16], in_=cmb1[:].rearrange("p (k x) -> p k x", k=16), axis=AX.X, op=ALU.add),
                      r=["cmb1"], w=["Eblk"])
                    V(lambda e: e.tensor_reduce(out=Eblk[:, 16:32], in_=cmb2[:].rearrange("p (k x) -> p k x", k=16), axis=AX.X, op=ALU.add),
                      r=["cmb2"], w=["Eblk"])
                    if b % 4 == 0:
                        V(lambda e: e.tensor_copy(out=Xtile[:, b // 4, :], in_=Xc[:]), r=["Xc"], w=["Xtile"])
                    V(lambda e: e.tensor_tensor(out=Xn[:, 0:16], in0=Xc[:, 0:16], in1=Lr, op=ALU.mult), r=["Xc"] + lk, w=["Xn"])
                    V(lambda e: e.tensor_tensor(out=ta[:], in0=Xc[:, 16:32], in1=Li, op=ALU.mult), r=["Xc"] + lk, w=["ta"])
                    V(lambda e: e.tensor_tensor(out=Xn[:, 0:16], in0=Xn[:, 0:16], in1=ta[:], op=ALU.subtract), r=["Xn", "ta"], w=["Xn"])
                    V(lambda e: e.tensor_tensor(out=Xn[:, 16:32], in0=Xc[:, 0:16], in1=Li, op=ALU.mult), r=["Xc"] + lk, w=["Xn"])
                    V(lambda e: e.tensor_tensor(out=ta[:], in0=Xc[:, 16:32], in1=Lr, op=ALU.mult), r=["Xc"] + lk, w=["ta"])
                    V(lambda e: e.tensor_tensor(out=Xn[:, 16:32], in0=Xn[:, 16:32], in1=ta[:], op=ALU.add), r=["Xn", "ta"], w=["Xn"])
                    V(lambda e: e.tensor_tensor(out=Xc[:], in0=Xn[:], in1=Eblk[:], op=ALU.add), r=["Xn", "Eblk"], w=["Xc"])
                if T > 0 and not DBG.get("nocompress"):
                    compress(T - 1, 32)
            if not DBG.get("nocompress"):
                compress(NT - 1, 31)

            if debug == "A":
                def doutt(name, shape, dt):
                    dbg_out[name] = nc.dram_tensor("dbg_" + name, list(shape), dt, kind="ExternalOutput").ap()
                    return dbg_out[name]
                DMA(doutt("ksT", [128, SEQ], BF16), ksT[:], r=["ksT"])
                DMA(doutt("vs", [128, 128 * 130], BF16), vsaug[:].rearrange("p a g d -> p (a g d)"), r=["vsaug"])
                DMA(doutt("kcT", [128, 1024], BF16), kcT[:], r=["kcT"])
                DMA(doutt("vcT", [128, 1024], BF16), vcT[:], r=["vcT"])
                DMA(doutt("Xtile", [128, NT * 32], F32), Xtile[:].rearrange("p a b -> p (a b)"), r=["Xtile"])
            S.barrier()


        with ExitStack() as sB:
            sbb = lambda name, shape, dt, st=sB: sb(name, shape, dt, st)
            mWM0, mTRI, mLMA = sbb("mWM0", [128, 128], BF16), sbb("mTRI", [128, 128], BF16), sbb("mLMA", [128, 128], BF16)
            blk64 = sbb("blk64", [128, 256], F32)
            cmpend = sbb("cmpend", [128, 8], F32)
            tpos = sbb("tpos", [128, 16], F32)
            tposm = sbb("tposm", [128, 16], F32)
            validW = sbb("validW", [128, 16, 5], F32)
            validA = sbb("validA", [128, 16], F32)
            vcaug = sbb("vcaug", [128, 8, 2, 65], BF16)
            qTz = sbb("qTz", [128, 2, 4, 512], BF16)
            kslocT = sbb("kslocT", [128, 640], BF16)
            kwT = sbb("kwT", [128, 1024], BF16)
            vslaug = sbb("vslaug", [128, 5, 2, 65], BF16)
            vwaug = sbb("vwaug", [128, 8, 2, 65], BF16)
            uT = sbb("uT", [128, 4, 512], BF16)
            utm = sbb("utmB", [128, 4, 512], BF16)
            gsig = sbb("gsig", [128, 4, 24], F32)
            aout = sbb("aout", [128, 4, 512], BF16)
            sout = sbb("sout", [128, 4, 512], BF16)
            tposBt = sbb("tposBt", [128, 512], F32)
            for (t_, d_, k_) in ((mWM0, mWM0_d, "mWM0"), (mTRI, mTRI_d, "mTRI"),
                                 (mLMA, mLMA_d, "mLMA"), (blk64, blk64_d, "blk64"), (cmpend, cmpend_d, "cmpend"),
                                 (tpos, tpos_d, "tpos"), (tposm, tposm_d, "tposm"), (validW, validW_d, "validW"),
                                 (validA, validA_d, "validA")):
                DMA(t_[:], d_, w=[k_])
            V(lambda e: e.memset(vcaug[:], 1.0), w=["vcaug"], e="pool")
            V(lambda e: e.memset(vslaug[:], 1.0), w=["vslaug"], e="pool")
            V(lambda e: e.memset(vwaug[:], 1.0), w=["vwaug"], e="pool")
            V(lambda e: e.memset(qTz[:], 0.0), w=["qTz"], e="pool")
            for m in range(8):
                pbb = pbank[7][:].bitcast(BF16)
                S.op("pe", lambda e: e.transpose(out=pbb[:, 0:128], in_=vcT[:, 128 * m:128 * m + 128], identity=identb[:]),
                     reads=["vcT", "identb"], writes=["pb7"])
                CP(vcaug[:, m, :, 0:64], pbb[:, 0:128].rearrange("p (g d) -> p g d", g=2), r=["pb7"], w=["vcaug"])

            cvin = [sbb(f"cvin{i}", [128, D], F32) for i in range(2)]
            cvout = [sbb(f"cvout{i}", [128, D], BF16) for i in range(2)]
            bg = []
            for ti, (src_t, dst_t) in enumerate(((utab, uvb[:, 0:D]), (vtab, uvb[:, D:2 * D]))):
                def op_in(ch, src_t=src_t):
                    i2 = ch % 2
                    return lambda: DMA(cvin[i2][:], src_t[128 * ch:128 * ch + 128, :], w=[f"cvin{i2}"])

                def op_cast(ch):
                    i2 = ch % 2
                    return lambda: CP(cvout[i2][:], cvin[i2][:], r=[f"cvin{i2}"], w=[f"cvout{i2}"], e="pool")

                def op_out(ch, dst_t=dst_t):
                    i2 = ch % 2
                    return lambda: DMA(dst_t[128 * ch:128 * ch + 128, :], cvout[i2][:], r=[f"cvout{i2}"], w=["tabbf"])
                bg.append(op_in(0))
                bg.append(op_in(1))
                for ch in range(128):
                    bg.append(op_cast(ch))
                    bg.append(op_out(ch))
                    if ch + 2 < 128:
                        bg.append(op_in(ch + 2))

            def bg_step(n=1):
                for _ in range(n):
                    if bg:
                        bg.pop(0)()

            wstB = [sbb(f"wstB{i}", [128, 8, 128], F32) for i in range(2)]
            wblB = [sbb(f"wblB{i}", [128, 8, 128], BF16) for i in range(3)]
            wctr = [0]

            def load_wblk(c0):
                i = wctr[0]
                wctr[0] += 1
                bl_ = wblB[i % 3]
                DMA(bl_[:], wBb[c0 // 128], r=["wBb"], w=[f"wblB{i % 3}"])
                return bl_, f"wblB{i % 3}"

            for jt in range(DBG.get("ntilesB", NOWN)):
                with ExitStack() as s1:
                    sb1 = lambda name, shape, dt: sb(name, shape, dt, s1)
                    xb = [sb1(f"xb{i}", [128, D], F32) for i in range(2)]
                    xsb = [sb1(f"xsb{i}", [128, D], BF16) for i in range(2)]
                    ssb = sb1("ssb", [128, 8], F32)
                    rsb = sb1("rsb", [128, 8], F32)
                    zT = sb1("zTB", [128, 8, 512], BF16)
                    rCt = sb1("rCt", [128, 512], F32)
                    rSt = sb1("rSt", [128, 512], F32)
                    rt1 = sb1("rt1", [128, 512], F32)
                    rt2 = sb1("rt2", [128, 512], F32)
                    DMA(tposBt[:], tposB_d[jt], w=["tposBt"])
                    for st in range(2):
                        DMA(rCt[:], ropeCl[jt, st], w=["rCt"], q="pool")
                        DMA(rSt[:], ropeSl[jt, st], w=["rSt"], q="pool")
                        for a in range(4):
                            i2 = a % 2
                            DMA(xb[i2][:], x_loc[jt, st * 512 + a * 128: st * 512 + a * 128 + 128, :], w=[f"xb{i2}"])
                            cidx = 4 * st + a
                            V(lambda e: e.activation(out=xsb[i2][:], in_=xb[i2][:], func=AF.Square, accum_out=ssb[:, cidx:cidx + 1]),
                              r=[f"xb{i2}"], w=[f"xsb{i2}", "ssb"], e="act")
                            V(lambda e: e.activation(out=rsb[:, cidx:cidx + 1], in_=ssb[:, cidx:cidx + 1], func=AF.Sqrt, bias=EPS, scale=1.0 / D),
                              r=["ssb"], w=["rsb"], e="act")
                            V(lambda e: e.reciprocal(out=rsb[:, cidx:cidx + 1], in_=rsb[:, cidx:cidx + 1]), r=["rsb"], w=["rsb"])
                            V(lambda e: e.tensor_scalar(out=xsb[i2][:], in0=xb[i2][:], scalar1=rsb[:, cidx:cidx + 1], scalar2=None, op0=ALU.mult),
                              r=[f"xb{i2}", "rsb"], w=[f"xsb{i2}"])
                            pbb = pbank[a % 2][:].bitcast(BF16)
                            for dc in range(8):
                                S.op("pe", lambda e: e.transpose(out=pbb[:, dc * 128:(dc + 1) * 128], in_=xsb[i2][:, dc * 128:(dc + 1) * 128],
                                                                 identity=identb[:]), reads=[f"xsb{i2}", "identb"], writes=[f"pb{a % 2}"])
                            CP(zT[:, :, a * 128:(a + 1) * 128], pbb.rearrange("p (c t) -> p c t", c=8), r=[f"pb{a % 2}"], w=["zTB"],
                               e=("act" if a % 2 else "dve"))

                        def fm_block(c0, bank):
                            wb_, wk_ = load_wblk(c0)
                            for dc in range(8):
                                MM(pbank[bank][:], wb_[:, dc, :], zT[:, dc, :], dc == 0, dc == 7, r=[wk_, "zTB"], w=[f"pb{bank}"])

                        def rope_evac(bA, bS, outs):
                            V(lambda e: e.tensor_tensor(out=rt1[:], in0=pbank[bA][:], in1=rCt[:], op=ALU.mult), r=[f"pb{bA}", "rCt"], w=["rt1"])
                            V(lambda e: e.tensor_tensor(out=rt2[:], in0=pbank[bS][:], in1=rSt[:], op=ALU.mult), r=[f"pb{bS}", "rSt"], w=["rt2"])
                            for (o_, ps_, k_, cs_) in outs:
                                V(lambda e: e.tensor_tensor(out=o_, in0=rt1[ps_, cs_], in1=rt2[ps_, cs_], op=ALU.add), r=["rt1", "rt2"], w=[k_], e="pool")

                        if st == 1:
                            for hl in range(4):
                                fm_block(128 * hl, 2)
                                fm_block(512 + 128 * hl, 3)
                                rope_evac(2, 3, [(qTz[0:64, 0, hl, :], slice(0, 64), "qTz", slice(0, 512)), (qTz[64:128, 1, hl, :], slice(64, 128), "qTz", slice(0, 512))])
                        fm_block(1024, 2)
                        fm_block(1152, 3)
                        if st == 0:
                            rope_evac(2, 3, [(kslocT[:, 0:128], slice(0, 128), "kslocT", slice(384, 512))])
                        else:
                            rope_evac(2, 3, [(kslocT[:, 128:640], slice(0, 128), "kslocT", slice(0, 512))])
                        fm_block(1280, 2)
                        fm_block(1408, 3)
                        rope_evac(2, 3, [(kwT[:, st * 512:(st + 1) * 512], slice(0, 128), "kwT", slice(0, 512))])
                        if st == 1:
                            for ub in range(4):
                                fm_block(1536 + 128 * ub, 2)
                                CP(uT[:, ub, :], pbank[2][:], r=["pb2"], w=["uT"], e="act")
                        chunks = [("vw", 2176)] + ([("vs", 2048)] if True else [])
                        if st == 1:
                            chunks += [("gt", 2304)] + [(f"u{i}", 2432 + 128 * i) for i in range(4)]
                        for (nm, c0) in chunks:
                            wb_, wk_ = load_wblk(c0)
                            for a in range(4):
                                if nm == "vs" and st == 0 and a != 3:
                                    continue
                                for dc in range(8):
                                    MM(pbank[4][:, a * 128:(a + 1) * 128], zT[:, dc, a * 128:(a + 1) * 128], wb_[:, dc, :], dc == 0, dc == 7,
                                       r=[wk_, "zTB"], w=["pb4"])
                            for a in range(4):
                                src = pbank[4][:, a * 128:(a + 1) * 128]
                                if nm == "vw":
                                    CP(vwaug[:, 4 * st + a, :, 0:64], src.rearrange("p (g d) -> p g d", g=2), r=["pb4"], w=["vwaug"], e="act")
                                elif nm == "vs":
                                    if st == 0 and a != 3:
                                        continue
                                    CP(vslaug[:, (0 if st == 0 else 1 + a), :, 0:64], src.rearrange("p (g d) -> p g d", g=2), r=["pb4"], w=["vslaug"], e="act")
                                elif nm == "gt":
                                    V(lambda e: e.activation(out=gsig[:, a, :], in_=src[:, 0:24], func=AF.Sigmoid), r=["pb4"], w=["gsig"], e="act")
                                else:
                                    ui = int(nm[1])
                                    CP(utm[:, a, 128 * ui:128 * ui + 128], src, r=["pb4"], w=["utmB"], e="act")
                S.barrier()

                if DBG.get("stopB1"):
                    continue
                with ExitStack() as s2:
                    sb2 = lambda name, shape, dt: sb(name, shape, dt, s2)
                    Fb = sb2("Fb", [128, 8192], BF16)
                    OV = sb2("OV", [128, 8, 256], BF16)
                    DMA(Fb[:], Fbase_d, w=["Fb"])
                    DMA(OV[:], OV_d, w=["OV"])
                    Eb = [sb2(f"Eb{i}", [128, 512], BF16) for i in range(3)]
                    cmall = sb2("cmall", [128, 8, 128], BF16)
                    selT = sb2("selT", [128, 2, 128], BF16)
                    negsel = sb2("negsel", [128, 256], BF16)
                    impq = sb2("impq", [128, 256], F32)
                    w1_ = sb2("w1_", [128, 256], F32)
                    w2_ = sb2("w2_", [128, 256], F32)
                    w3_ = sb2("w3_", [128, 256], F32)
                    valid_ = sb2("valid_", [128, 256], F32)
                    local_ = sb2("local_", [128, 256], F32)
                    m8a = sb2("m8a", [128, 8], F32)
                    m8b = sb2("m8b", [128, 8], F32)
                    rz = sb2("rz", [128, 3, 4], F32)
                    coef = sb2("coef", [128, 3, 4], F32)
                    atmp = sb2("atmp", [128, 64], F32)
                    ectr = [0]

                    def next_E():
                        i = ectr[0] % 3
                        ectr[0] += 1
                        return Eb[i], f"Eb{i}"

                    sctr = [0]

                    def next_S():
                        i = (0, 1, 7)[sctr[0] % 3]
                        sctr[0] += 1
                        return pbank[i], f"pb{i}"

                    def acc_view(bank):
                        return pbank[bank][:].rearrange("p (h x) -> p h x", h=4)

                    for qb in range(4):
                        col = 4 * jt + qb
                        tsl = slice(128 * qb, 128 * qb + 128)
                        for m in range(8):
                            V(lambda e: e.tensor_scalar(out=cmall[:, m, :], in0=tposBt[:, tsl], scalar1=cmpend[:, m:m + 1], scalar2=None, op0=ALU.is_ge),
                              r=["tposBt", "cmpend"], w=["cmall"], e="pool")
                        V(lambda e: e.tensor_scalar(out=valid_[:], in0=blk64[:], scalar1=tpos[:, col:col + 1], scalar2=None, op0=ALU.is_le),
                          r=["blk64", "tpos"], w=["valid_"])
                        V(lambda e: e.tensor_scalar(out=local_[:], in0=blk64[:], scalar1=tposm[:, col:col + 1], scalar2=None, op0=ALU.is_gt),
                          r=["blk64", "tposm"], w=["local_"])
                        V(lambda e: e.tensor_tensor(out=local_[:], in0=local_[:], in1=valid_[:], op=ALU.mult), r=["local_", "valid_"], w=["local_"])
                        for g in range(2):
                            qrhs = qTz[:, g, :, tsl]
                            def run_pass(items):
                                def issue_S(it):
                                    sbk, skey = next_S()
                                    it["s"](sbk, skey)
                                    return sbk, skey
                                pend = [issue_S(items[0])]
                                if len(items) > 1:
                                    pend.append(issue_S(items[1]))
                                for i, it in enumerate(items):
                                    if i + 2 < len(items):
                                        pend.append(issue_S(items[i + 2]))
                                    sbk, skey = pend.pop(0)
                                    E_, ek = next_E()
                                    V(lambda e: e.activation(out=E_[:], in_=sbk[:], func=AF.Exp, scale=0.125), r=[skey], w=[ek], e="act")
                                    if it.get("post"):
                                        it["post"](E_, ek)
                                    it["pv"](E_, ek)
                                    if it.get("bg"):
                                        bg_step(1)

                            def mk_cmp(m):
                                def s_(sbk, skey):
                                    MM(sbk[:], kcT[:, 128 * m:128 * m + 128], qrhs, True, True, r=["kcT", "qTz"], w=[skey])

                                def post(E_, ek):
                                    E3 = E_[:].rearrange("p (h t) -> p h t", h=4)
                                    V(lambda e: e.tensor_tensor(out=E3, in0=E3, in1=cmall[:, m, :].unsqueeze(1).to_broadcast([128, 4, 128]), op=ALU.mult),
                                      r=[ek, "cmall"], w=[ek])

                                def pv(E_, ek):
                                    for hl in range(4):
                                        MM(pbank[2][:, hl * 128:hl * 128 + 65], E_[:, hl * 128:(hl + 1) * 128], vcaug[:, m, g, :], m == 0 and hl == 0, m == 7,
                                           r=[ek, "vcaug"], w=["pb2"])
                                    for hl in range(4):
                                        bk = 3 + hl // 2
                                        MM(pbank[bk][:, (hl % 2) * 256:(hl % 2) * 256 + 256], E_[:, hl * 128:(hl + 1) * 128], OV[:, m, :], m == 0 and hl % 2 == 0, m == 7,
                                           r=[ek, "OV"], w=[f"pb{bk}"])
                                return dict(s=s_, post=post, pv=pv)

                            run_pass([mk_cmp(m) for m in range(8)])
                            V(lambda e: e.tensor_scalar(out=rz[:, 0, :], in0=acc_view(2)[:, :, 64], scalar1=1e-30, scalar2=None, op0=ALU.max),
                              r=["pb2"], w=["rz0"])
                            V(lambda e: e.reciprocal(out=rz[:, 0, :], in_=rz[:, 0, :]), r=["rz0"], w=["rz0"])
                            for hl in range(4):
                                bk = 3 + hl // 2
                                src = pbank[bk][:, (hl % 2) * 256:(hl % 2) * 256 + 256]
                                if hl == 0:
                                    V(lambda e: e.tensor_scalar(out=impq[:], in0=src, scalar1=rz[:, 0, 0:1], scalar2=None, op0=ALU.mult),
                                      r=[f"pb{bk}", "rz0"], w=["impq"])
                                else:
                                    V(lambda e: e.scalar_tensor_tensor(out=impq[:], in0=src, scalar=rz[:, 0, hl:hl + 1], in1=impq[:],
                                                                       op0=ALU.mult, op1=ALU.add), r=[f"pb{bk}", "rz0", "impq"], w=["impq"])
                            V(lambda e: e.tensor_scalar(out=w1_[:], in0=valid_[:], scalar1=-1.0, scalar2=1e30, op0=ALU.add, op1=ALU.mult), r=["valid_"], w=["w1_"])
                            V(lambda e: e.tensor_tensor(out=w2_[:], in0=impq[:], in1=valid_[:], op=ALU.mult), r=["impq", "valid_"], w=["w2_"])
                            V(lambda e: e.tensor_tensor(out=w2_[:], in0=w2_[:], in1=w1_[:], op=ALU.add), r=["w2_", "w1_"], w=["w2_"])
                            V(lambda e: e.tensor_scalar(out=w1_[:], in0=local_[:], scalar1=1e9, scalar2=None, op0=ALU.mult), r=["local_"], w=["w1_"])
                            V(lambda e: e.memset(w1_[:, 0:1], 1e9), r=[], w=["w1_"])
                            V(lambda e: e.tensor_tensor(out=w2_[:], in0=w2_[:], in1=w1_[:], op=ALU.max), r=["w2_", "w1_"], w=["w2_"])
                            V(lambda e: e.max(out=m8a[:], in_=w2_[:]), r=["w2_"], w=["m8a"])
                            V(lambda e: e.match_replace(out=w3_[:], in_to_replace=m8a[:], in_values=w2_[:], imm_value=-3e38), r=["w2_", "m8a"], w=["w3_"])
                            V(lambda e: e.max(out=m8b[:], in_=w3_[:]), r=["w3_"], w=["m8b"])
                            V(lambda e: e.tensor_scalar(out=w3_[:], in0=w2_[:], scalar1=m8b[:, 7:8], scalar2=None, op0=ALU.is_ge), r=["w2_", "m8b"], w=["w3_"])
                            V(lambda e: e.tensor_tensor(out=w3_[:], in0=w3_[:], in1=valid_[:], op=ALU.mult), r=["w3_", "valid_"], w=["w3_"])
                            V(lambda e: e.tensor_scalar(out=w1_[:], in0=local_[:], scalar1=-1.0, scalar2=-1.0, op0=ALU.add, op1=ALU.mult), r=["local_"], w=["w1_"])
                            V(lambda e: e.tensor_tensor(out=w3_[:], in0=w3_[:], in1=w1_[:], op=ALU.mult), r=["w3_", "w1_"], w=["w3_"])
                            V(lambda e: e.tensor_copy(out=negsel[:], in_=w3_[:]), r=["w3_"], w=["negsel"])
                            pbb7 = pbank[7][:].bitcast(BF16)
                            for hf in range(2):
                                S.op("pe", lambda e: e.transpose(out=pbb7[:, hf * 128:(hf + 1) * 128], in_=negsel[:, hf * 128:(hf + 1) * 128], identity=identb[:]),
                                     reads=["negsel", "identb"], writes=["pb7"])
                            for hf in range(2):
                                CP(selT[:, hf, :], pbb7[:, hf * 128:(hf + 1) * 128], r=["pb7"], w=["selT"])
                            J = 4 * (8 * jt + 7) + qb + 1
                            J = min(J, DBG.get("maxJ", 1000))

                            def mk_win(c5):
                                c0 = 128 * qb + 128 * c5

                                def s_(sbk, skey):
                                    MM(sbk[:], kwT[:, c0:c0 + 128], qrhs, True, True, r=["kwT", "qTz"], w=[skey])

                                def post(E_, ek):
                                    if c5 in (0, 4):
                                        msk, mk = (mWM0, "mWM0") if c5 == 0 else (mTRI, "mTRI")
                                        E3 = E_[:].rearrange("p (h t) -> p h t", h=4)
                                        V(lambda e: e.tensor_tensor(out=E3, in0=E3, in1=msk[:].unsqueeze(1).to_broadcast([128, 4, 128]), op=ALU.mult), r=[ek, mk], w=[ek])
                                    V(lambda e: e.tensor_scalar(out=E_[:], in0=E_[:], scalar1=validW[:, col, c5:c5 + 1], scalar2=None, op0=ALU.mult),
                                      r=[ek, "validW"], w=[ek], e="pool")

                                def pv(E_, ek):
                                    for hl in range(4):
                                        MM(pbank[6][:, hl * 128:hl * 128 + 65], E_[:, hl * 128:(hl + 1) * 128], vwaug[:, qb + c5, g, :], c5 == 0 and hl == 0, c5 == 4,
                                           r=[ek, "vwaug"], w=["pb6"])
                                return dict(s=s_, post=post, pv=pv)

                            def mk_loc(ci):
                                c0, msk, mk = ((128 * qb, mLMA, "mLMA"), (128 * qb + 128, mTRI, "mTRI"))[ci]

                                def s_(sbk, skey):
                                    MM(sbk[:], kslocT[:, c0:c0 + 128], qrhs, True, True, r=["kslocT", "qTz"], w=[skey])

                                def post(E_, ek):
                                    E3 = E_[:].rearrange("p (h t) -> p h t", h=4)
                                    V(lambda e: e.tensor_tensor(out=E3, in0=E3, in1=msk[:].unsqueeze(1).to_broadcast([128, 4, 128]), op=ALU.mult), r=[ek, mk], w=[ek])
                                    if ci == 0:
                                        V(lambda e: e.tensor_scalar(out=E_[:], in0=E_[:], scalar1=validA[:, col:col + 1], scalar2=None, op0=ALU.mult),
                                          r=[ek, "validA"], w=[ek], e="pool")

                                def pv(E_, ek):
                                    for hl in range(4):
                                        MM(pbank[5][:, hl * 128:hl * 128 + 65], E_[:, hl * 128:(hl + 1) * 128], vslaug[:, qb + ci, g, :], ci == 0 and hl == 0, False,
                                           r=[ek, "vslaug"], w=["pb5"])
                                return dict(s=s_, post=post, pv=pv)

                            def mk_sel(j):
                                mb = 3 + j % 2

                                def s_(sbk, skey):
                                    MM(sbk[:], ksT[:, 128 * j:128 * j + 128], qrhs, True, True, r=["ksT", "qTz"], w=[skey])

                                def post(E_, ek):
                                    MM(pbank[mb][:, 0:128], Fb[:, 128 * (j % 64):128 * (j % 64) + 128], selT[:, j // 64, :], True, True, r=["Fb", "selT"], w=[f"pb{mb}"])
                                    E3 = E_[:].rearrange("p (h t) -> p h t", h=4)
                                    V(lambda e: e.tensor_tensor(out=E3, in0=E3, in1=pbank[mb][:, 0:128].unsqueeze(1).to_broadcast([128, 4, 128]), op=ALU.mult),
                                      r=[ek, f"pb{mb}"], w=[ek])

                                def pv(E_, ek):
                                    for hl in range(4):
                                        MM(pbank[5][:, hl * 128:hl * 128 + 65], E_[:, hl * 128:(hl + 1) * 128], vsaug[:, j, g, :], False, j == J - 1,
                                           r=[ek, "vsaug"], w=["pb5"])
                                return dict(s=s_, post=post, pv=pv, bg=True)

                            run_pass([mk_win(c5) for c5 in range(5)] + [mk_loc(ci) for ci in range(2)] + [mk_sel(j) for j in range(J)])
                            for jb, bank in ((1, 5), (2, 6)):
                                V(lambda e: e.tensor_scalar(out=rz[:, jb, :], in0=acc_view(bank)[:, :, 64], scalar1=1e-30, scalar2=None, op0=ALU.max),
                                  r=[f"pb{bank}"], w=[f"rz{jb}"])
                                V(lambda e: e.reciprocal(out=rz[:, jb, :], in_=rz[:, jb, :]), r=[f"rz{jb}"], w=[f"rz{jb}"])
                            gv = gsig[:, qb, :].rearrange("p (h j) -> p j h", j=3)
                            V(lambda e: e.tensor_tensor(out=coef[:], in0=rz[:], in1=gv[:, :, 4 * g:4 * g + 4], op=ALU.mult),
                              r=["rz0", "rz1", "rz2", "gsig"], w=["coef"])
                            for hl in range(4):
                                h_ = 4 * g + hl
                                V(lambda e: e.tensor_scalar(out=atmp[:], in0=pbank[2][:, hl * 128:hl * 128 + 64], scalar1=coef[:, 0, hl:hl + 1], scalar2=None, op0=ALU.mult),
                                  r=["pb2", "coef"], w=["atmp"])
                                V(lambda e: e.scalar_tensor_tensor(out=atmp[:], in0=pbank[5][:, hl * 128:hl * 128 + 64], scalar=coef[:, 1, hl:hl + 1], in1=atmp[:],
                                                                   op0=ALU.mult, op1=ALU.add), r=["pb5", "coef", "atmp"], w=["atmp"])
                                V(lambda e: e.scalar_tensor_tensor(out=aout[:, qb, 64 * h_:64 * h_ + 64], in0=pbank[6][:, hl * 128:hl * 128 + 64], scalar=coef[:, 2, hl:hl + 1],
                                                                   in1=atmp[:], op0=ALU.mult, op1=ALU.add), r=["pb6", "coef", "atmp"], w=["aout"])
                S.barrier()
                if debug == "B2":
                    def doutt(name, shape, dt):
                        dbg_out[name] = nc.dram_tensor("dbg_" + name, list(shape), dt, kind="ExternalOutput").ap()
                        return dbg_out[name]
                    DMA(doutt(f"aout{jt}", [128, 2048], BF16), aout[:].rearrange("p a b -> p (a b)"), r=["aout"])
                    DMA(doutt(f"qTz{jt}", [128, 4096], BF16), qTz[:].rearrange("p a b c -> p (a b c)"), r=["qTz"])
                    DMA(doutt(f"kwT{jt}", [128, 1024], BF16), kwT[:], r=["kwT"])
                    DMA(doutt(f"kslocT{jt}", [128, 640], BF16), kslocT[:], r=["kslocT"])
                    DMA(doutt(f"gsig{jt}", [128, 96], F32), gsig[:].rearrange("p a b -> p (a b)"), r=["gsig"])
                    DMA(doutt(f"utm{jt}", [128, 2048], BF16), utm[:].rearrange("p a b -> p (a b)"), r=["utmB"])
                    S.barrier()

                if DBG.get("stopB2"):
                    continue
                with ExitStack() as s3:
                    sb3 = lambda name, shape, dt: sb(name, shape, dt, s3)
                    cosT = sb3("cosT", [128, 16, 128], F32)
                    sinT = sb3("sinT", [128, 16, 128], F32)
                    BbT = sb3("BbT", [128, 2, 16, 128], BF16)
                    Cp = sb3("Cp", [128, 2, 16, 32], BF16)
                    dsk = sb3("dsk", [128, 512], F32)
                    wglu = sb3("wglu", [128, 4, 1024], BF16)
                    rho = sb3("rho", [128, 16], F32)
                    z0 = sb3("z0", [128, 32], F32)
                    with ExitStack() as s3a:
                        sb3a = lambda name, shape, dt: sb(name, shape, dt, s3a)
                        ti_ = sb3a("ti_", [128, 1024], I32)
                        ta_ = sb3a("ta_", [128, 1024], F32)
                        tb_ = sb3a("tb_", [128, 1024], F32)
                        ang = sb3a("ang", [128, 1024], F32)
                        trw = sb3a("trw", [128, 128], F32)
                        Bexp = sb3a("Bexp", [128, 2, 16, 128], BF16)
                        cst = sb3a("cst", [128, 16, 16], F32)
                        ohs = sb3a("ohs", [128, NT], F32)
                        tmpX = sb3a("tmpX", [128, 32, NT], F32)
                        DMA(trw[:], trow, w=["trw"])
                        DMA(dsk[:], dskip_d, w=["dsk"])
                        DMA(ohs[:], onehot[:, jt, :], w=["ohs"])

                        def sincos3(out_ap, n, shift, key_out):
                            A_, B_, I_ = ta_[:, 0:n], tb_[:, 0:n], ti_[:, 0:n]
                            V(lambda e: e.tensor_scalar(out=A_, in0=ang[:, 0:n], scalar1=shift, scalar2=1.0 / (2 * PI), op0=ALU.add, op1=ALU.mult), r=["ang"], w=["ta_"])
                            V(lambda e: e.tensor_copy(out=I_, in_=A_), r=["ta_"], w=["ti_"])
                            V(lambda e: e.tensor_copy(out=B_, in_=I_), r=["ti_"], w=["tb_"])
                            V(lambda e: e.tensor_tensor(out=A_, in0=A_, in1=B_, op=ALU.subtract), r=["ta_", "tb_"], w=["ta_"])
                            V(lambda e: e.tensor_scalar(out=B_, in0=A_, scalar1=0.5, scalar2=None, op0=ALU.is_gt), r=["ta_"], w=["tb_"])
                            V(lambda e: e.tensor_tensor(out=A_, in0=A_, in1=B_, op=ALU.subtract), r=["ta_", "tb_"], w=["ta_"])
                            V(lambda e: e.tensor_scalar(out=B_, in0=A_, scalar1=-0.5, scalar2=None, op0=ALU.is_lt), r=["ta_"], w=["tb_"])
                            V(lambda e: e.tensor_tensor(out=A_, in0=A_, in1=B_, op=ALU.add), r=["ta_", "tb_"], w=["ta_"])
                            V(lambda e: e.activation(out=out_ap, in_=A_, func=AF.Sin, scale=2 * PI), r=["ta_"], w=[key_out], e="act")

                        for hh in range(2):
                            ks8 = slice(8 * hh, 8 * hh + 8)
                            V(lambda e: e.tensor_tensor(out=ang[:].rearrange("p (k t) -> p k t", k=8),
                                                        in0=thf[:, ks8].unsqueeze(2).to_broadcast([128, 8, 128]),
                                                        in1=trw[:].unsqueeze(1).to_broadcast([128, 8, 128]), op=ALU.mult), r=["thf", "trw"], w=["ang"])
                            sincos3(cosT[:, ks8, :].rearrange("p k t -> p (k t)"), 1024, PI / 2, "cosT")
                            sincos3(sinT[:, ks8, :].rearrange("p k t -> p (k t)"), 1024, 0.0, "sinT")
                        V(lambda e: e.activation(out=rho[:], in_=af[:], func=AF.Exp), r=["af"], w=["rho"], e="act")
                        V(lambda e: e.memset(Bexp[:], 0.0), w=["Bexp"], e="pool")
                        Bb5 = Bbar[:].rearrange("p r (a b) c -> p r a b c", b=4)
                        Be5 = Bexp[:].rearrange("p r (a b) x -> p r a b x", b=4)
                        for glo in range(2):
                            ps_ = slice(64 * glo, 64 * glo + 64)
                            for k4 in range(4):
                                V(lambda e: e.tensor_copy(out=Be5[ps_, :, :, k4, 32 * k4 + 16 * glo:32 * k4 + 16 * glo + 16], in_=Bb5[ps_, :, :, k4, :]),
                                  r=["Bbr", "Bbi", "Bexp"], w=["Bexp"])
                        for ri in range(2):
                            for k8 in range(2):
                                pbb = pbank[ri * 2 + k8][:].bitcast(BF16)
                                for kk in range(8):
                                    S.op("pe", lambda e: e.transpose(out=pbb[:, kk * 128:(kk + 1) * 128], in_=Bexp[:, ri, 8 * k8 + kk, :], identity=identb[:]),
                                         reads=["Bexp", "identb"], writes=[f"pb{ri * 2 + k8}"])
                                CP(BbT[:, ri, 8 * k8:8 * k8 + 8, :].rearrange("p k q -> p (k q)"), pbb, r=[f"pb{ri * 2 + k8}"], w=["BbT"])
                        V(lambda e: e.memset(Cp[:], 0.0), w=["Cp"], e="pool")
                        for ri, nm in ((0, "cre_f"), (1, "cim_f")):
                            DMA(cst[:], ssm_b[nm], w=["cst"])
                            for glo in range(2):
                                ps_ = slice(64 * glo, 64 * glo + 64)
                                V(lambda e: e.tensor_scalar(out=Cp[ps_, ri, :, 16 * glo:16 * glo + 16], in0=cst[ps_], scalar1=(1.0 if ri == 0 else -1.0),
                                                            scalar2=None, op0=ALU.mult), r=["cst", "Cp"], w=["Cp"])
                        for c4 in range(4):
                            st_ = wstB[c4 % 2]
                            stv = st_[:].rearrange("p a b -> p (a b)").rearrange("p (c n) -> p c n", c=4)
                            DMA(stv, wglu_d[:, 256 * c4:256 * c4 + 256].rearrange("(c p) n -> p c n", p=128), w=[f"wstB{c4 % 2}"])
                            CP(wglu[:, :, 256 * c4:256 * c4 + 256], stv, r=[f"wstB{c4 % 2}"], w=["wglu"])
                        V(lambda e: e.tensor_tensor(out=tmpX[:], in0=Xtile[:].rearrange("p t c -> p c t"),
                                                    in1=ohs[:].unsqueeze(1).to_broadcast([128, 32, NT]), op=ALU.mult), r=["Xtile", "ohs"], w=["tmpX"])
                        V(lambda e: e.tensor_reduce(out=z0[:], in_=tmpX[:], axis=AX.X, op=ALU.add), r=["tmpX"], w=["z0"])
                        S.barrier()
                    wr = sb3("wr", [128, 4, 128], F32)
                    wi = sb3("wi", [128, 4, 128], F32)
                    q1 = sb3("q1", [128, 4, 128], F32)
                    q2 = sb3("q2", [128, 4, 128], F32)
                    zr = sb3("zr", [128, 4, 128], F32)
                    zi = sb3("zi", [128, 4, 128], F32)
                    xr = sb3("xr", [128, 4, 128], F32)
                    xi = sb3("xi", [128, 4, 128], F32)
                    xrb = sb3("xrb", [128, 4, 128], BF16)
                    xib = sb3("xib", [128, 4, 128], BF16)
                    ypre = sb3("ypre", [128, 512], F32)
                    ygb = sb3("ygb", [128, 512], BF16)
                    ygT = sb3("ygT", [128, 4, 128], BF16)
                    sg = sb3("sg", [128, 512], F32)
                    fl4 = lambda t: t[:].rearrange("p k t -> p (k t)")
                    for qb in range(4):
                        tsl = slice(128 * qb, 128 * qb + 128)
                        for qq in range(4):
                            k4s = slice(4 * qq, 4 * qq + 4)
                            cq = cosT[:, k4s, :].rearrange("p k t -> p (k t)")
                            sq = sinT[:, k4s, :].rearrange("p k t -> p (k t)")
                            for ri in range(2):
                                for kk in range(4):
                                    MM(pbank[ri][:, kk * 128:(kk + 1) * 128], BbT[:, ri, 4 * qq + kk, :], uT[:, qq, tsl], True, True, r=["BbT", "uT"], w=[f"pb{ri}"])
                            V(lambda e: e.tensor_tensor(out=fl4(wr), in0=pbank[0][:], in1=cq, op=ALU.mult), r=["pb0", "cosT"], w=["wr"])
                            V(lambda e: e.tensor_tensor(out=fl4(q1), in0=pbank[1][:], in1=sq, op=ALU.mult), r=["pb1", "sinT"], w=["q1"])
                            V(lambda e: e.tensor_tensor(out=fl4(wi), in0=pbank[1][:], in1=cq, op=ALU.mult), r=["pb1", "cosT"], w=["wi"])
                            V(lambda e: e.tensor_tensor(out=fl4(q2), in0=pbank[0][:], in1=sq, op=ALU.mult), r=["pb0", "sinT"], w=["q2"])
                            V(lambda e: e.tensor_tensor(out=fl4(wr), in0=fl4(wr), in1=fl4(q1), op=ALU.add), r=["wr", "q1"], w=["wr"], e="pool")
                            V(lambda e: e.tensor_tensor(out=fl4(wi), in0=fl4(wi), in1=fl4(q2), op=ALU.subtract), r=["wi", "q2"], w=["wi"], e="pool")
                            for kk in range(4):
                                k = 4 * qq + kk
                                V(lambda e: e.tensor_tensor_scan(out=zr[:, kk, :], data0=rho[:, k:k + 1].to_broadcast([128, 128]), data1=wr[:, kk, :],
                                                                 initial=z0[:, k:k + 1], op0=ALU.mult, op1=ALU.add), r=["rho", "wr", "z0"], w=["zr"])
                                V(lambda e: e.tensor_tensor_scan(out=zi[:, kk, :], data0=rho[:, k:k + 1].to_broadcast([128, 128]), data1=wi[:, kk, :],
                                                                 initial=z0[:, 16 + k:17 + k], op0=ALU.mult, op1=ALU.add), r=["rho", "wi", "z0"], w=["zi"])
                            V(lambda e: e.tensor_tensor(out=fl4(xr), in0=fl4(zr), in1=cq, op=ALU.mult), r=["zr", "cosT"], w=["xr"])
                            V(lambda e: e.tensor_tensor(out=fl4(q1), in0=fl4(zi), in1=sq, op=ALU.mult), r=["zi", "sinT"], w=["q1"], e="pool")
                            V(lambda e: e.tensor_tensor(out=fl4(xr), in0=fl4(xr), in1=fl4(q1), op=ALU.subtract), r=["xr", "q1"], w=["xr"])
                            V(lambda e: e.tensor_tensor(out=fl4(xi), in0=fl4(zr), in1=sq, op=ALU.mult), r=["zr", "sinT"], w=["xi"], e="pool")
                            V(lambda e: e.tensor_tensor(out=fl4(q2), in0=fl4(zi), in1=cq, op=ALU.mult), r=["zi", "cosT"], w=["q2"])
                            V(lambda e: e.tensor_tensor(out=fl4(xi), in0=fl4(xi), in1=fl4(q2), op=ALU.add), r=["xi", "q2"], w=["xi"], e="pool")
                            CP(fl4(xrb), fl4(xr), r=["xr"], w=["xrb"], e="act")
                            CP(fl4(xib), fl4(xi), r=["xi"], w=["xib"], e="act")
                            V(lambda e: e.tensor_copy(out=z0[:, k4s], in_=xr[:, :, 127]), r=["xr", "z0"], w=["z0"])
                            V(lambda e: e.tensor_copy(out=z0[:, 16 + 4 * qq:20 + 4 * qq], in_=xi[:, :, 127]), r=["xi", "z0"], w=["z0"])
                            for kk in range(4):
                                k = 4 * qq + kk
                                MM(pbank[4][:, 32 * k:32 * k + 32], xrb[:, kk, :], Cp[:, 0, k, :], qq == 0 and kk == 0, False, r=["xrb", "Cp"], w=["pb4"])
                                MM(pbank[4][:, 32 * k:32 * k + 32], xib[:, kk, :], Cp[:, 1, k, :], False, True, r=["xib", "Cp"], w=["pb4"])
                        V(lambda e: e.tensor_tensor(out=sg[:], in0=utm[:, qb, :], in1=dsk[:], op=ALU.mult), r=["utmB", "dsk"], w=["sg"], e="pool")
                        V(lambda e: e.tensor_tensor(out=ypre[:], in0=pbank[4][:], in1=sg[:], op=ALU.add), r=["pb4", "sg"], w=["ypre"])
                        V(lambda e: e.activation(out=ygb[:], in_=ypre[:], func=AF.Gelu_apprx_tanh), r=["ypre"], w=["ygb"], e="act")
                        pbb5 = pbank[5][:].bitcast(BF16)
                        for cc in range(4):
                            S.op("pe", lambda e: e.transpose(out=pbb5[:, cc * 128:(cc + 1) * 128], in_=ygb[:, cc * 128:(cc + 1) * 128], identity=identb[:]),
                                 reads=["ygb", "identb"], writes=["pb5"])
                        CP(ygT[:].rearrange("p c t -> p (c t)"), pbb5[:, 0:512], r=["pb5"], w=["ygT"])
                        for half in range(2):
                            for cc in range(4):
                                MM(pbank[6 + half][:], ygT[:, cc, :], wglu[:, cc, half * 512:(half + 1) * 512], cc == 0, cc == 3, r=["ygT", "wglu"], w=[f"pb{6 + half}"])
                        V(lambda e: e.activation(out=sg[:], in_=pbank[7][:], func=AF.Sigmoid), r=["pb7"], w=["sg"], e="act")
                        V(lambda e: e.tensor_tensor(out=sout[:, qb, :], in0=pbank[6][:], in1=sg[:], op=ALU.mult), r=["pb6", "sg"], w=["sout"])
                S.barrier()
                with ExitStack() as s4:
                    sb4 = lambda name, shape, dt: sb(name, shape, dt, s4)
                    wout = sb4("wout", [128, 8, 1024], BF16)
                    mixT = sb4("mixT", [128, 8, 128], BF16)
                    xrow = [sb4(f"xrow{i}", [128, D], F32) for i in range(2)]
                    h1t = [sb4(f"h1t{i}", [128, D], F32) for i in range(2)]
                    for c8 in range(8):
                        st_ = wstB[c8 % 2]
                        DMA(st_[:], wout_d[:, 128 * c8:128 * c8 + 128].rearrange("(c p) n -> p c n", p=128), w=[f"wstB{c8 % 2}"])
                        CP(wout[:, :, 128 * c8:128 * c8 + 128], st_[:], r=[f"wstB{c8 % 2}"], w=["wout"], e=("act" if c8 % 2 else "dve"))
                    for qb in range(4):
                        i2 = qb % 2
                        DMA(xrow[i2][:], x_loc[jt, 512 + 128 * qb:512 + 128 * qb + 128, :], w=[f"xrow{i2}"])
                        pbb5 = pbank[5][:].bitcast(BF16)
                        for cc in range(8):
                            src = aout if cc < 4 else sout
                            kx = "aout" if cc < 4 else "sout"
                            S.op("pe", lambda e: e.transpose(out=pbb5[:, cc * 128:(cc + 1) * 128], in_=src[:, qb, (cc % 4) * 128:(cc % 4) * 128 + 128], identity=identb[:]),
                                 reads=[kx, "identb"], writes=["pb5"])
                        CP(mixT[:].rearrange("p c t -> p (c t)"), pbb5, r=["pb5"], w=["mixT"])
                        for half in range(2):
                            for cc in range(8):
                                MM(pbank[6 + half][:], mixT[:, cc, :], wout[:, cc, half * 512:(half + 1) * 512], cc == 0, cc == 7, r=["mixT", "wout"], w=[f"pb{6 + half}"])
                            V(lambda e: e.tensor_tensor(out=h1t[i2][:, half * 512:(half + 1) * 512], in0=pbank[6 + half][:], in1=xrow[i2][:, half * 512:(half + 1) * 512], op=ALU.add),
                              r=[f"pb{6 + half}", f"xrow{i2}"], w=[f"h1t{i2}"])
                        DMA(h1d[4 * jt + qb], h1t[i2][:], r=[f"h1t{i2}"], w=["h1d"])
                    if debug == "B4":
                        def doutt(name, shape, dt):
                            dbg_out[name] = nc.dram_tensor("dbg_" + name, list(shape), dt, kind="ExternalOutput").ap()
                            return dbg_out[name]
                        DMA(doutt(f"aout{jt}", [128, 2048], BF16), aout[:].rearrange("p a b -> p (a b)"), r=["aout"])
                        DMA(doutt(f"sout{jt}", [128, 2048], BF16), sout[:].rearrange("p a b -> p (a b)"), r=["sout"])
                        DMA(doutt(f"gsig{jt}", [128, 96], F32), gsig[:].rearrange("p a b -> p (a b)"), r=["gsig"])
                        DMA(doutt(f"utm{jt}", [128, 2048], BF16), utm[:].rearrange("p a b -> p (a b)"), r=["utmB"])
                        dh = doutt(f"h1{jt}", [4, 128, D], F32)
                        for qb in range(4):
                            DMA(xrow[0][:], h1d[4 * jt + qb], r=["h1d"], w=["xrow0"])
                            DMA(dh[qb], xrow[0][:], r=["xrow0"])
                S.barrier()

            while bg:
                bg_step(1)
        S.barrier()
        sctx.close()
        if not DBG.get("noC"):
          with ExitStack() as sC:
            sbc = lambda name, shape, dt: sb(name, shape, dt, sC)
            wq = sbc("wq", [128, 8, 2048], BF16)
            subT = sbc("subT", [128, 2, 128], BF16)
            gffn = sbc("gffn", [128, D], F32)
            gfin = sbc("gfin", [128, D], F32)
            wstC = [sbc(f"wstC{i}", [128, 8, 128], F32) for i in range(2)]
            DMA(gffn[:], gffn_d.partition_broadcast(128), w=["gffn"])
            DMA(gfin[:], fnorm.partition_broadcast(128), w=["gfin"])
            for c16 in range(16):
                st_ = wstC[c16 % 2]
                DMA(st_[:], wq_d[:, 128 * c16:128 * c16 + 128].rearrange("(c p) n -> p c n", p=128), w=[f"wstC{c16 % 2}"])
                CP(wq[:, :, 128 * c16:128 * c16 + 128], st_[:], r=[f"wstC{c16 % 2}"], w=["wq"], e=("act" if c16 % 2 else "dve"))
            for i_, d_ in enumerate((sub1T_d, sub2T_d)):
                DMA(wstC[0][:, 0, :], d_, w=["wstC0"])
                CP(subT[:, i_, :], wstC[0][:, 0, :], r=["wstC0"], w=["subT"])
            hb = [sbc(f"hb{i}", [128, D], F32) for i in range(2)]
            xn2 = [sbc(f"xn{i}", [128, D], F32) for i in range(2)]
            xnb = sbc("xnb", [128, D], BF16)
            xnT = sbc("xnT", [128, 8, 128], BF16)
            qTb = sbc("qTb", [128, 16, 128], BF16)
            sc = sbc("sc", [128, 16, 128], F32)
            scr = sbc("scr", [128, 128], F32)
            vtop = sbc("vtop", [128, 16, 16], F32)
            itop = sbc("itop", [128, 16, 16], U32)
            itf = sbc("itf", [128, 16, 16], F32)
            cand = sbc("cand", [128, 256], F32)
            cand2 = sbc("cand2", [128, 256], F32)
            cidx = sbc("cidx", [128, 256], F32)
            cjk = sbc("cjk", [128, 256], F32)
            top = sbc("top", [128, 8, 16], F32)
            eidf = sbc("eidf", [128, 128], F32)
            eidx2 = [sbc(f"eidx{i}", [128, 128], I32) for i in range(2)]
            gate2 = [sbc(f"gate{i}", [128, 8, 16], F32) for i in range(2)]
            ntop = sbc("ntop", [128, 8], F32)
            zs = sbc("zs", [128, 8], F32)
            hid = sbc("hid", [128, 128], F32)
            actw = sbc("actw", [128, 128], F32)
            ssc = sbc("ssc", [128, 2], F32)
            rsc = sbc("rsc", [128, 2], F32)
            gbuf = [sbc(f"gbuf{i}", [128, 2 * D], BF16) for i in range(NGB)]
            gjk = sbc("gjk", [128, D], F32)
            acc = sbc("acc", [128, D], F32)
            scb = [sbc(f"scb{i}", [128, D], BF16) for i in range(4)]
            otile = sbc("otile", [128, D], F32)
            gctr = [0]
            NQ = DBG.get("nqC", 16)

            def frontend(qi):
                p = qi % 2
                h_, hk_ = hb[p], f"hb{p}"
                xn, xk = xn2[p], f"xn{p}"
                eidx, ek_ = eidx2[p], f"eidx{p}"
                gate, gk = gate2[p], f"gate{p}"
                DMA(h_[:], h1d[qi], r=["h1d"], w=[hk_])
                V(lambda e: e.activation(out=xn[:], in_=h_[:], func=AF.Square, accum_out=ssc[:, 0:1]), r=[hk_], w=[xk, "ssc0"], e="act")
                V(lambda e: e.activation(out=rsc[:, 0:1], in_=ssc[:, 0:1], func=AF.Sqrt, bias=EPS, scale=1.0 / D), r=["ssc0"], w=["rsc0"], e="act")
                V(lambda e: e.reciprocal(out=rsc[:, 0:1], in_=rsc[:, 0:1]), r=["rsc0"], w=["rsc0"])
                yield
                V(lambda e: e.scalar_tensor_tensor(out=xn[:], in0=h_[:], scalar=rsc[:, 0:1], in1=gffn[:], op0=ALU.mult, op1=ALU.mult),
                  r=[hk_, "rsc0", "gffn"], w=[xk])
                CP(xnb[:], xn[:], r=[xk], w=["xnb"], e="act")
                yield
                pbb = pbank[0][:].bitcast(BF16)
                for dc in range(8):
                    S.op("pe", lambda e: e.transpose(out=pbb[:, dc * 128:(dc + 1) * 128], in_=xnb[:, dc * 128:(dc + 1) * 128], identity=identb[:]),
                         reads=["xnb", "identb"], writes=["pb0"])
                CP(xnT[:].rearrange("p c t -> p (c t)"), pbb, r=["pb0"], w=["xnT"])
                yield
                for b4 in range(4):
                    bank = 1 + b4 % 2
                    for bb in range(4):
                        blk = 4 * b4 + bb
                        for dc in range(8):
                            MM(pbank[bank][:, bb * 128:(bb + 1) * 128], wq[:, dc, blk * 128:(blk + 1) * 128], xnT[:, dc, :], dc == 0, dc == 7,
                               r=["wq", "xnT"], w=[f"pb{bank}"])
                        yield
                    CP(qTb[:, 4 * b4:4 * b4 + 4, :].rearrange("p a b -> p (a b)"), pbank[bank][:], r=[f"pb{bank}"], w=["qTb"], e=("act" if b4 % 2 else "dve"))
                    yield
                for b4 in range(4):
                    bank = 3 + b4 % 2
                    for bb in range(4):
                        blk = 4 * b4 + bb
                        MM(pbank[bank][:, bb * 128:(bb + 1) * 128], qTb[:, blk, :], subT[:, blk % 2, :], True, True, r=["qTb", "subT"], w=[f"pb{bank}"])
                    CP(sc[:, 4 * b4:4 * b4 + 4, :].rearrange("p a b -> p (a b)"), pbank[bank][:], r=[f"pb{bank}"], w=["sc"], e=("act" if b4 % 2 else "dve"))
                    yield
                for blk in range(16):
                    V(lambda e: e.max(out=vtop[:, blk, 0:8], in_=sc[:, blk, :]), r=["sc"], w=["vtop"])
                    V(lambda e: e.max_index(out=itop[:, blk, 0:8], in_max=vtop[:, blk, 0:8], in_values=sc[:, blk, :]), r=["sc", "vtop"], w=["itop"])
                    yield
                    V(lambda e: e.match_replace(out=scr[:], in_to_replace=vtop[:, blk, 0:8], in_values=sc[:, blk, :], imm_value=-1e30), r=["sc", "vtop"], w=["scr"])
                    V(lambda e: e.max(out=vtop[:, blk, 8:16], in_=scr[:]), r=["scr"], w=["vtop"])
                    yield
                    V(lambda e: e.max_index(out=itop[:, blk, 8:16], in_max=vtop[:, blk, 8:16], in_values=scr[:]), r=["scr", "vtop"], w=["itop"])
                    yield
                V(lambda e: e.tensor_copy(out=itf[:], in_=itop[:]), r=["itop"], w=["itf"])
                yield
                for hh in range(8):
                    c3 = cand[:].rearrange("p (a b) -> p a b", a=16)
                    i3 = cidx[:].rearrange("p (a b) -> p a b", a=16)
                    V(lambda e: e.tensor_tensor(out=c3, in0=vtop[:, 2 * hh, :].unsqueeze(2).to_broadcast([128, 16, 16]),
                                                in1=vtop[:, 2 * hh + 1, :].unsqueeze(1).to_broadcast([128, 16, 16]), op=ALU.add), r=["vtop"], w=["cand"])
                    V(lambda e: e.scalar_tensor_tensor(out=i3, in0=itf[:, 2 * hh, :].unsqueeze(2).to_broadcast([128, 16, 16]), scalar=128.0,
                                                       in1=itf[:, 2 * hh + 1, :].unsqueeze(1).to_broadcast([128, 16, 16]), op0=ALU.mult, op1=ALU.add),
                      r=["itf"], w=["cidx"])
                    yield
                    V(lambda e: e.max(out=top[:, hh, 0:8], in_=cand[:]), r=["cand"], w=["top"])
                    V(lambda e: e.match_replace(out=cand2[:], in_to_replace=top[:, hh, 0:8], in_values=cand[:], imm_value=-1e30), r=["cand", "top"], w=["cand2"])
                    V(lambda e: e.max(out=top[:, hh, 8:16], in_=cand2[:]), r=["cand2"], w=["top"])
                    yield
                    for kk in range(16):
                        V(lambda e: e.scalar_tensor_tensor(out=cjk[:], in0=cand[:], scalar=top[:, hh, kk:kk + 1], in1=cidx[:], op0=ALU.is_equal, op1=ALU.mult,
                                                           accum_out=eidf[:, 16 * hh + kk:16 * hh + kk + 1]), r=["cand", "cidx", "top"], w=[f"eidf{16 * hh + kk}"])
                        if kk % 2:
                            yield
                    V(lambda e: e.tensor_scalar(out=ntop[:, hh:hh + 1], in0=top[:, hh, 0:1], scalar1=-1.0, scalar2=None, op0=ALU.mult), r=["top"], w=["ntop"])
                    V(lambda e: e.activation(out=gate[:, hh, :], in_=top[:, hh, :], func=AF.Exp, bias=ntop[:, hh:hh + 1], scale=1.0, accum_out=zs[:, hh:hh + 1]),
                      r=["top", "ntop"], w=[gk, "zs"], e="act")
                    yield
                V(lambda e: e.reciprocal(out=zs[:], in_=zs[:]), r=["zs"], w=["zs"])
                V(lambda e: e.tensor_tensor(out=gate[:], in0=gate[:], in1=zs[:].unsqueeze(2).to_broadcast([128, 8, 16]), op=ALU.mult), r=[gk, "zs"], w=[gk])
                V(lambda e: e.tensor_scalar(out=eidf[:], in0=eidf[:], scalar1=16383.0, scalar2=0.0, op0=ALU.min, op1=ALU.max), r=[f"eidf{i}" for i in range(128)], w=["eidf"])
                V(lambda e: e.tensor_copy(out=eidx[:], in_=eidf[:]), r=["eidf"], w=[ek_])
                yield

            def drain(gen, n=None):
                if gen is None:
                    return
                k = 0
                for _ in gen:
                    k += 1
                    if n is not None and k >= n:
                        return

            drain(frontend(0))
            for qi in range(NQ):
                p = qi % 2
                h_, hk_ = hb[p], f"hb{p}"
                xn, xk = xn2[p], f"xn{p}"
                eidx, ek_ = eidx2[p], f"eidx{p}"
                gate, gk = gate2[p], f"gate{p}"
                fe_next = frontend(qi + 1) if qi + 1 < NQ else None
                gflat = gate[:].rearrange("p a b -> p (a b)")
                for grp in range(16):
                    gl = []
                    for hk in range(8 * grp, 8 * grp + 8):
                        gi = gctr[0] % NGB
                        gctr[0] += 1
                        gb = gbuf[gi]
                        gl.append((hk, gi, gb))
                        S.dma("pool", lambda q: q.indirect_dma_start(out=gb[:], out_offset=None, in_=uvb,
                                                                     in_offset=bass.IndirectOffsetOnAxis(ap=eidx[:, hk:hk + 1], axis=0)), reads=[ek_], writes=[f"gbuf{gi}"])
                        V(lambda e: e.scalar_tensor_tensor(out=gjk[:], in0=gb[:, 0:D], scalar=1.0, in1=xn[:], op0=ALU.mult, op1=ALU.mult, accum_out=hid[:, hk:hk + 1]),
                          r=[f"gbuf{gi}", xk], w=[f"hid{hk}"])
                        drain(fe_next, 2)
                    gs = slice(8 * grp, 8 * grp + 8)
                    V(lambda e: e.activation(out=actw[:, gs], in_=hid[:, gs], func=AF.Gelu_apprx_tanh), r=[f"hid{i}" for i in range(8 * grp, 8 * grp + 8)], w=[f"actw{grp}"], e="act")
                    V(lambda e: e.tensor_tensor(out=actw[:, gs], in0=actw[:, gs], in1=gflat[:, gs], op=ALU.mult), r=[f"actw{grp}", gk], w=[f"actw{grp}"])
                    for (hk, gi, gb) in gl:
                        si = hk % 4
                        V(lambda e: e.activation(out=scb[si][:], in_=gb[:, D:2 * D], func=AF.Copy, scale=actw[:, hk:hk + 1]), r=[f"gbuf{gi}", f"actw{grp}"], w=[f"scb{si}"], e="act")
                        for half in range(2):
                            MM(pbank[6 + half][:], identb[:], scb[si][:, half * 512:(half + 1) * 512], hk == 0, hk == 127, r=[f"scb{si}", "identb"], w=[f"pb{6 + half}"])
                        drain(fe_next, 1)
                drain(fe_next)
                for half in range(2):
                    V(lambda e: e.tensor_tensor(out=acc[:, half * 512:(half + 1) * 512], in0=pbank[6 + half][:], in1=h_[:, half * 512:(half + 1) * 512], op=ALU.add),
                      r=[f"pb{6 + half}", hk_], w=["acc"])
                V(lambda e: e.activation(out=otile[:], in_=acc[:], func=AF.Square, accum_out=ssc[:, 1:2]), r=["acc"], w=["otile", "ssc1"], e="act")
                V(lambda e: e.activation(out=rsc[:, 1:2], in_=ssc[:, 1:2], func=AF.Sqrt, bias=EPS, scale=1.0 / D), r=["ssc1"], w=["rsc1"], e="act")
                V(lambda e: e.reciprocal(out=rsc[:, 1:2], in_=rsc[:, 1:2]), r=["rsc1"], w=["rsc1"])
                V(lambda e: e.scalar_tensor_tensor(out=otile[:], in0=acc[:], scalar=rsc[:, 1:2], in1=gfin[:], op0=ALU.mult, op1=ALU.mult),
                  r=["acc", "rsc1", "gfin"], w=["otile"])
                DMA(y[qi // 4, 128 * (qi % 4):128 * (qi % 4) + 128, :], otile[:], r=["otile"])

        S.finish()
    return nc


def kernel(**inputs):
    com, per = _prep(inputs)
    nc = build_nc()
    in_maps = [dict(com, **per[c]) for c in range(NCORES)]
    res = run_bass_kernel_spmd(nc, in_maps, core_ids=list(range(NCORES)))
    out = np.zeros((NT, 512, D), np.float32)
    for c in range(NCORES):
        for j in range(NOWN):
            out[8 * j + c] = res.results[c]["y"][j]
    return out.reshape(1, SEQ, D)
```

```python
import math
import numpy as np
import concourse.bass as bass
import concourse.mybir as mybir
from concourse.bass_utils import run_bass_kernel_spmd
from contextlib import ExitStack

F32 = mybir.dt.float32
BF16 = mybir.dt.bfloat16
I32 = mybir.dt.int32
U32 = mybir.dt.uint32
AF = mybir.ActivationFunctionType
ALU = mybir.AluOpType
AX = mybir.AxisListType

NCORES = 8
SEQ = 16384
D = 1024
NT = 32
NOWN = 4
EPS = 1e-6
NGB = 16
PI = math.pi


class Sync:
    def __init__(self, nc, es):
        self.nc = nc
        self.engs = {"pe": nc.tensor, "act": nc.scalar, "dve": nc.vector,
                     "pool": nc.gpsimd, "sp": nc.sync}
        self.sem = {k: es.enter_context(nc.semaphore("sem_" + k)) for k in self.engs}
        self.cnt = {k: 0 for k in self.engs}
        self.dsem = [es.enter_context(nc.semaphore(f"dsem{i}")) for i in range(32)]
        self.dcnt = [0] * len(self.dsem)
        self.qsl = {"sp": list(range(0, 24)), "act": list(range(0, 24)), "pool": list(range(24, 32))}
        self.drr = {"sp": 0, "act": 0, "pool": 0}
        self.waited = {k: {} for k in self.engs}
        self.lastw = {}
        self.reads = {}

    def _wait(self, e, ev):
        if ev is None:
            return
        sem, val, src = ev
        if src == e and e == "pe":
            return
        key = id(sem)
        if self.waited[e].get(key, 0) >= val:
            return
        self.engs[e].wait_ge(sem, val)
        self.waited[e][key] = val

    def deps(self, e, reads, writes):
        for b in reads:
            self._wait(e, self.lastw.get(b))
        for b in writes:
            self._wait(e, self.lastw.get(b))
            for ev in self.reads.get(b, []):
                self._wait(e, ev)

    def commit(self, ev, reads, writes):
        for b in reads:
            self.reads.setdefault(b, []).append(ev)
        for b in writes:
            self.lastw[b] = ev
            self.reads[b] = []

    def op(self, e, inst_fn, reads=(), writes=()):
        self.deps(e, reads, writes)
        inst = inst_fn(self.engs[e])
        self.cnt[e] += 1
        inst.then_inc(self.sem[e], 1)
        ev = (self.sem[e], self.cnt[e], e)
        self.commit(ev, reads, writes)
        return ev

    def dma(self, e, inst_fn, reads=(), writes=()):
        self.deps(e, reads, writes)
        sl = self.qsl[e]
        k = sl[self.drr[e] % len(sl)]
        self.drr[e] += 1
        inst = inst_fn(self.engs[e])
        self.dcnt[k] += 16
        inst.then_inc(self.dsem[k], 16)
        ev = (self.dsem[k], self.dcnt[k], "dma")
        self.commit(ev, reads, writes)
        return ev

    def barrier(self):
        for e in self.engs:
            for e2 in self.engs:
                if e2 != e and self.cnt[e2]:
                    self._wait(e, (self.sem[e2], self.cnt[e2], e2))
            for k in range(len(self.dsem)):
                if self.dcnt[k]:
                    self._wait(e, (self.dsem[k], self.dcnt[k], "dma"))
        self.lastw = {}
        self.reads = {}

    def finish(self):
        for k in range(len(self.dsem)):
            if self.dcnt[k]:
                self._wait("sp", (self.dsem[k], self.dcnt[k], "dma"))
        for e2 in self.engs:
            if e2 != "sp" and self.cnt[e2]:
                self._wait("sp", (self.sem[e2], self.cnt[e2], e2))


HD = 64
ROPE_THETA = 500000.0


def _rope_tables_T(pos):
    pos = np.asarray(pos, np.float32)
    inv = (ROPE_THETA ** (-np.arange(8, dtype=np.float32) * 2.0 / 16.0)).astype(np.float32)
    ang = pos[None, :] * inv[:, None]
    c, s = np.cos(ang).astype(np.float32), np.sin(ang).astype(np.float32)
    C = np.ones((64, len(pos)), np.float32)
    S = np.zeros((64, len(pos)), np.float32)
    C[0:8] = c
    C[8:16] = c
    S[0:8] = -s
    S[8:16] = s
    return np.concatenate([C, C], 0), np.concatenate([S, S], 0)


def _swap_cols(cols):
    cols = np.asarray(cols).reshape(-1, 64).copy()
    out = cols.copy()
    out[:, 0:8] = cols[:, 8:16]
    out[:, 8:16] = cols[:, 0:8]
    return out.reshape(-1)


Q0, KC0, VC0, KS0, VS0, KW0, VW0, GT0, U0 = 0, 512, 640, 768, 896, 1024, 1152, 1280, 1304
NWA = 1152
NWB = 2944


def _prep(inputs):
    f = lambda k: np.ascontiguousarray(np.asarray(inputs[k], dtype=np.float32))
    x = f("x").reshape(SEQ, D)
    w_in = f("w_in")[0]
    ar = np.arange
    ks_c, kc_c, vc_c, vs_c = KS0 + ar(128), KC0 + ar(128), VC0 + ar(128), VS0 + ar(128)
    kw_c, vw_c, u_c = KW0 + ar(128), VW0 + ar(128), U0 + ar(512)
    colsA = np.concatenate([ks_c, _swap_cols(ks_c), kc_c, vc_c, vs_c, u_c])
    q_c = np.concatenate([np.concatenate([Q0 + 64 * hl + ar(64), Q0 + 64 * (4 + hl) + ar(64)])
                          for hl in range(4)])
    g_c = np.concatenate([GT0 + ar(24), np.full(104, GT0)])
    colsB = np.concatenate([q_c, _swap_cols(q_c), ks_c, _swap_cols(ks_c), kw_c, _swap_cols(kw_c),
                            u_c, vs_c, vw_c, g_c, u_c])
    assert len(colsA) == NWA and len(colsB) == NWB
    com = {}
    com["x_all"] = x.reshape(NT, 512, D)
    com["wA"] = np.ascontiguousarray(w_in[:, colsA])
    com["wB"] = np.ascontiguousarray(w_in[:, colsB])
    com["gA"] = np.ascontiguousarray(f("attn_norm")[0].reshape(8, 128).T)
    com["ident"] = np.eye(128, dtype=np.float32)
    C, S = _rope_tables_T(np.arange(SEQ))
    com["ropeC"] = np.ascontiguousarray(C.reshape(128, NT, 512).transpose(1, 0, 2))
    com["ropeS"] = np.ascontiguousarray(S.reshape(128, NT, 512).transpose(1, 0, 2))
    cmp_end = np.arange(1024) * 16 + 31
    Cc, Sc = _rope_tables_T(cmp_end)
    com["cmpC"], com["cmpS"] = Cc, Sc
    for kv in ("k", "v"):
        w1 = f(f"cmp_{kv}_w1")[0].reshape(32, 64, 128).transpose(1, 0, 2)
        com[f"w1{kv}"] = np.ascontiguousarray(np.concatenate([w1, w1], 0))
        com[f"pe{kv}T"] = np.ascontiguousarray(np.concatenate([f(f"cmp_{kv}_pe")[0].T] * 2, 0))
        w2 = f(f"cmp_{kv}_w2")[0]
        z = np.zeros_like(w2)
        com[f"w2{kv}"] = np.ascontiguousarray(np.stack(
            [np.concatenate([w2, z], 1), np.concatenate([z, w2], 1)], 1))
        if kv == "k":
            w2s = w2[:, _swap_cols(np.arange(64))]
            com["w2ksw"] = np.ascontiguousarray(np.stack(
                [np.concatenate([w2s, z], 1), np.concatenate([z, w2s], 1)], 1))
    def fm(a):
        a = a.reshape((16, 2) + a.shape[1:])
        a = np.moveaxis(a, 0, 2)
        return np.ascontiguousarray(a.reshape((128, 16) + a.shape[3:]))
    lam_re, lam_im = f("ssm_lam_re")[0], f("ssm_lam_im")[0]
    log_dt = f("ssm_log_dt")[0]
    com["lamre_f"], com["lamim_f"] = fm(lam_re), fm(lam_im)
    com["logdt_f"] = fm(np.repeat(log_dt[:, None], 64, 1))
    com["bre_f"], com["bim_f"] = fm(f("ssm_b_re")[0]), fm(f("ssm_b_im")[0])
    com["cre_f"] = fm(f("ssm_c_re")[0].transpose(0, 2, 1))
    com["cim_f"] = fm(f("ssm_c_im")[0].transpose(0, 2, 1))
    def rowb(a):
        return np.ascontiguousarray(np.broadcast_to(fm(a).transpose(1, 0)[None], (128, 16, 128)))
    com["lamre_r"], com["lamim_r"] = rowb(lam_re), rowb(lam_im)
    com["logdt_r"] = rowb(np.repeat(log_dt[:, None], 64, 1))
    com["tcol"] = np.ascontiguousarray(np.broadcast_to((127 - np.arange(128, dtype=np.float32))[:, None], (128, 1)))
    com["trow"] = np.ascontiguousarray(np.broadcast_to(np.arange(1, 129, dtype=np.float32)[None], (128, 128)))
    com["final_norm"] = f("final_norm").reshape(1, D)
    import ml_dtypes
    bf = lambda a: np.ascontiguousarray(np.asarray(a, np.float32).astype(ml_dtypes.bfloat16))
    cc = np.arange(8192)
    com["Fbase"] = bf((cc[None, :] // 64) == np.arange(128)[:, None])
    n_all = np.arange(1024)
    m_all = np.arange(256)
    ov = ((16 * n_all[:, None] < 64 * m_all[None, :] + 64) & (16 * n_all[:, None] + 32 > 64 * m_all[None, :]))
    ov[1023] = False
    com["OV"] = bf(ov.reshape(8, 128, 256).transpose(1, 0, 2))
    kk_, tt_ = np.arange(128)[:, None], np.arange(128)[None, :]
    com["mWM0"] = bf(kk_ > tt_)
    com["mTRI"] = bf(kk_ <= tt_)
    com["mLMA"] = bf((kk_ >= 64) & (tt_ < 64))
    com["blk64"] = np.ascontiguousarray(np.broadcast_to((64.0 * np.arange(256, dtype=np.float32))[None], (128, 256)))
    com["cmpend"] = np.ascontiguousarray((16.0 * n_all + 31.0).astype(np.float32).reshape(8, 128).T)
    com["dskip"] = np.ascontiguousarray(np.broadcast_to(f("ssm_d")[0][None], (128, 512)))
    com["wglu"] = f("ssm_w_glu")[0]
    com["wout"] = f("w_out")[0]
    com["ffn_norm"] = f("ffn_norm")[0].reshape(1, D)
    com["wq"] = f("peer_w_q")[0]
    com["sub1T"] = np.ascontiguousarray(f("peer_subkeys_1")[0].T)
    com["sub2T"] = np.ascontiguousarray(f("peer_subkeys_2")[0].T)
    com["peer_u"] = f("peer_u")[0]
    com["peer_v"] = f("peer_v")[0]
    per = []
    for c in range(NCORES):
        d = {}
        tiles = [8 * j + c for j in range(NOWN)]
        xl = np.zeros((NOWN, 1024, D), np.float32)
        for j, T in enumerate(tiles):
            lo = 512 * T - 512
            if lo >= 0:
                xl[j] = x[lo:lo + 1024]
            else:
                xl[j, 512:] = x[0:512]
        d["x_loc"] = xl
        oh = np.zeros((128, NOWN, NT), np.float32)
        for j, T in enumerate(tiles):
            oh[:, j, T] = 1.0
        d["onehot"] = oh
        rCl = np.zeros((NOWN, 2, 128, 512), np.float32)
        rSl = np.zeros((NOWN, 2, 128, 512), np.float32)
        tpB = np.zeros((NOWN, 128, 512), np.float32)
        tpt = np.zeros((128, 16), np.float32)
        vW = np.zeros((128, 16, 5), np.float32)
        vA = np.zeros((128, 16), np.float32)
        for j, T in enumerate(tiles):
            pos = 512 * T - 512 + np.arange(1024)
            Cl, Sl = _rope_tables_T(np.maximum(pos, 0))
            rCl[j] = Cl.reshape(128, 2, 512).transpose(1, 0, 2)
            rSl[j] = Sl.reshape(128, 2, 512).transpose(1, 0, 2)
            tpB[j] = np.broadcast_to((512 * T + np.arange(512)).astype(np.float32)[None], (128, 512))
            for qb in range(4):
                t0 = 512 * T + 128 * qb
                tpt[:, 4 * j + qb] = t0 + np.arange(128)
                for c5 in range(5):
                    vW[:, 4 * j + qb, c5] = 1.0 if t0 - 512 + 128 * c5 >= 0 else 0.0
                vA[:, 4 * j + qb] = 1.0 if t0 - 128 >= 0 else 0.0
        d.update(ropeCl=rCl, ropeSl=rSl, tposB=tpB, tpos_tm=tpt, tposm128=tpt - 128.0, validW=vW, validA=vA)
        per.append(d)
    return com, per


DBG = {}


def build_nc(debug=None):
    nc = bass.Bass("TRN2", target_bir_lowering=False)
    dins = {}

    def din(name, shape, dt=F32):
        dins[name] = nc.dram_tensor(name, list(shape), dt, kind="ExternalInput").ap()
        return dins[name]

    x_all = din("x_all", [NT, 512, D])
    x_loc = din("x_loc", [NOWN, 1024, D])
    wA = din("wA", [D, NWA])
    wB = din("wB", [D, NWB])
    gA = din("gA", [128, 8])
    ident = din("ident", [128, 128])
    ropeC = din("ropeC", [NT, 128, 512])
    ropeS = din("ropeS", [NT, 128, 512])
    cmpC = din("cmpC", [128, 1024])
    cmpS = din("cmpS", [128, 1024])
    w1d = {kv: din(f"w1{kv}", [128, 32, 128]) for kv in "kv"}
    peTd = {kv: din(f"pe{kv}T", [128, 32]) for kv in "kv"}
    w2d = {"k": din("w2k", [128, 2, 128]), "v": din("w2v", [128, 2, 128]), "ksw": din("w2ksw", [128, 2, 128])}
    ssm_f = {n: din(n, [128, 16]) for n in ("lamre_f", "lamim_f", "logdt_f")}
    ssm_b = {n: din(n, [128, 16, 16]) for n in ("bre_f", "bim_f", "cre_f", "cim_f")}
    ssm_r = {n: din(n, [128, 16, 128]) for n in ("lamre_r", "lamim_r", "logdt_r")}
    tcol = din("tcol", [128, 1])
    trow = din("trow", [128, 128])
    onehot = din("onehot", [128, NOWN, NT])
    fnorm = din("final_norm", [1, D])
    Fbase_d = din("Fbase", [128, 8192], BF16)
    OV_d = din("OV", [128, 8, 256], BF16)
    mWM0_d, mTRI_d, mLMA_d = din("mWM0", [128, 128], BF16), din("mTRI", [128, 128], BF16), din("mLMA", [128, 128], BF16)
    blk64_d = din("blk64", [128, 256])
    cmpend_d = din("cmpend", [128, 8])
    dskip_d = din("dskip", [128, 512])
    wglu_d = din("wglu", [512, 1024])
    wout_d = din("wout", [1024, 1024])
    ropeCl = din("ropeCl", [NOWN, 2, 128, 512])
    ropeSl = din("ropeSl", [NOWN, 2, 128, 512])
    tposB_d = din("tposB", [NOWN, 128, 512])
    tpos_d = din("tpos_tm", [128, 16])
    tposm_d = din("tposm128", [128, 16])
    validW_d = din("validW", [128, 16, 5])
    validA_d = din("validA", [128, 16])
    h1d = nc.dram_tensor("h1scr", [16, 128, D], F32, kind="Internal").ap()
    uvb = nc.dram_tensor("uvtab_bf", [16384, 2 * D], BF16, kind="Internal").ap()
    wBb = nc.dram_tensor("wB_bf", [NWB // 128, 128, 8, 128], BF16, kind="Internal").ap()
    gffn_d = din("ffn_norm", [1, D])
    wq_d = din("wq", [D, 2048])
    sub1T_d = din("sub1T", [128, 128])
    sub2T_d = din("sub2T", [128, 128])
    utab = din("peer_u", [16384, D])
    vtab = din("peer_v", [16384, D])
    y = nc.dram_tensor("y", [NOWN, 512, D], F32, kind="ExternalOutput").ap()
    dbg_out = {}

    def dout(name, shape):
        dbg_out[name] = nc.dram_tensor("dbg_" + name, list(shape), F32, kind="ExternalOutput").ap()
        return dbg_out[name]

    with ExitStack() as es:
        S = Sync(nc, es)
        _nctr = [0]

        def sb(name, shape, dt, st=es):
            _nctr[0] += 1
            return st.enter_context(nc.sbuf_tensor(f"s{_nctr[0]}_{name}", list(shape), dt))
        pbank = [es.enter_context(nc.psum_tensor(f"pb{i}", [128, 512], F32)) for i in range(8)]

        def DMA(out, in_, r=(), w=(), q="sp"):
            return S.dma(q, lambda e: e.dma_start(out=out, in_=in_), reads=r, writes=w)

        def V(fn, r=(), w=(), e="dve"):
            return S.op(e, fn, reads=r, writes=w)

        def CP(out, in_, r=(), w=(), e="dve"):
            if e == "act":
                return S.op(e, lambda en: en.copy(out=out, in_=in_), reads=r, writes=w)
            return S.op(e, lambda en: en.tensor_copy(out=out, in_=in_), reads=r, writes=w)

        def MM(out, lhsT, rhs, start, stop, r=(), w=()):
            return S.op("pe", lambda e: e.matmul(out=out, lhsT=lhsT, rhs=rhs, start=start, stop=stop, skip_group_check=True),
                        reads=r, writes=w)

        identf = sb("identf", [128, 128], F32)
        identb = sb("identb", [128, 128], BF16)
        gAs = sb("gAs", [128, 8], F32)
        sctx = ExitStack()
        ksT = sb("ksT", [128, SEQ], BF16, sctx)
        vsaug = sb("vsaug", [128, 128, 2, 65], BF16, sctx)
        kcT = sb("kcT", [128, 1024], BF16, sctx)
        vcT = sb("vcT", [128, 1024], BF16, sctx)
        Xtile = sb("Xtile", [128, NT, 32], F32, sctx)
        kap = sb("kap", [128, 2, 16], F32, sctx)
        Bbar = sb("Bbar", [128, 2, 16, 16], F32, sctx)
        af = sb("af", [128, 16], F32, sctx)
        thf = sb("thf", [128, 16], F32, sctx)
        DMA(identf[:], ident, w=["identf"])
        DMA(gAs[:], gA, w=["gAs"])
        V(lambda e: e.tensor_copy(out=identb[:], in_=identf[:]), r=["identf"], w=["identb"])
        V(lambda e: e.memset(vsaug[:], 1.0), w=["vsaug"], e="pool")
        V(lambda e: e.memset(kcT[:], 0.0), w=["kcT"], e="pool")
        V(lambda e: e.memset(vcT[:], 0.0), w=["vcT"], e="pool")

        with ExitStack() as sa:
            sba = lambda name, shape, dt: sb(name, shape, dt, sa)
            Wa = sba("Wa", [128, 8, NWA], BF16)
            W1 = {kv: sba(f"W1{kv}", [128, 32, 128], BF16) for kv in "kv"}
            biasc = {kv: sba(f"biasc{kv}", [128, 1], F32) for kv in "kv"}
            W2 = {nm: sba(f"W2{nm}", [128, 2, 128], BF16) for nm in ("k", "ksw", "v")}
            L128 = sba("L128", [128, 2, 16], F32)
            Bs1 = sba("Bs1", [128, 16, 2, 32], F32)
            Bs2 = sba("Bs2", [128, 16, 2, 32], F32)
            Gt = sba("Gt", [128, 2, 16, 128], BF16)
            sp_ = sa.enter_context(ExitStack())
            sba_persist = sba
            sba = lambda name, shape, dt: sb(name, shape, dt, sp_)
            wst = [sba(f"wst{i}", [128, 8, 128], F32) for i in range(2)]
            for ci, c0 in enumerate(range(0, NWA, 128)):
                w_ = wst[ci % 2]
                DMA(w_[:], wA[:, c0:c0 + 128].rearrange("(c p) n -> p c n", p=128), w=[f"wst{ci % 2}"])
                for dc in range(8):
                    V(lambda e: e.tensor_scalar(out=Wa[:, dc, c0:c0 + 128], in0=w_[:, dc, :],
                                                scalar1=gAs[:, dc:dc + 1], scalar2=None, op0=ALU.mult),
                      r=[f"wst{ci % 2}", "gAs"], w=["Wa"], e=("dve" if dc % 2 else "pool"))
            wbo = [sba(f"wbo{i}", [128, 8, 128], BF16) for i in range(2)]
            for ci, c0 in enumerate(range(0, NWB, 128)):
                w_ = wst[ci % 2]
                o_ = wbo[ci % 2]
                DMA(w_[:], wB[:, c0:c0 + 128].rearrange("(c p) n -> p c n", p=128), w=[f"wst{ci % 2}"])
                for dc in range(8):
                    V(lambda e: e.tensor_scalar(out=o_[:, dc, :], in0=w_[:, dc, :], scalar1=gAs[:, dc:dc + 1], scalar2=None, op0=ALU.mult),
                      r=[f"wst{ci % 2}", "gAs"], w=[f"wbo{ci % 2}"], e=("dve" if dc % 2 else "pool"))
                DMA(wBb[ci], o_[:], r=[f"wbo{ci % 2}"], w=["wBb"])
            w1st = sba("w1st", [128, 16, 128], F32)
            w2st = sba("w2st", [128, 2, 128], F32)
            pest = sba("pest", [128, 32], F32)
            peb = sba("peb", [128, 32], BF16)
            for kv in "kv":
                for hf in range(2):
                    DMA(w1st[:], w1d[kv][:, 16 * hf:16 * hf + 16, :], w=["w1st"])
                    V(lambda e: e.tensor_copy(out=W1[kv][:, 16 * hf:16 * hf + 16, :], in_=w1st[:]), r=["w1st"], w=[f"W1{kv}"])
                DMA(pest[:], peTd[kv], w=["pest"])
                V(lambda e: e.tensor_copy(out=peb[:], in_=pest[:]), r=["pest"], w=["peb"])
                for l in range(32):
                    MM(pbank[5][:, 0:1], W1[kv][0:64, l, :], peb[0:64, l:l + 1], l == 0, l == 31,
                       r=[f"W1{kv}", "peb"], w=["pb5"])
                V(lambda e: e.tensor_copy(out=biasc[kv][:], in_=pbank[5][:, 0:1]), r=["pb5"], w=[f"biasc{kv}"])
            for nm in ("k", "ksw", "v"):
                DMA(w2st[:], w2d[nm], w=["w2st"])
                V(lambda e: e.tensor_copy(out=W2[nm][:], in_=w2st[:]), r=["w2st"], w=[f"W2{nm}"])

            sf = {n: sba(n, [128, 16], F32) for n in ssm_f}
            for n in ssm_f:
                DMA(sf[n][:], ssm_f[n], w=[n])
            bf_ = {n: sba(n, [128, 16, 16], F32) for n in ("bre_f", "bim_f")}
            for n in bf_:
                DMA(bf_[n][:], ssm_b[n], w=[n])
            dtf = sba("dtf", [128, 16], F32)
            V(lambda e: e.activation(out=dtf[:], in_=sf["logdt_f"][:], func=AF.Exp), r=["logdt_f"], w=["dtf"], e="act")
            V(lambda e: e.tensor_tensor(out=af[:], in0=sf["lamre_f"][:], in1=dtf[:], op=ALU.mult), r=["lamre_f", "dtf"], w=["af"])
            V(lambda e: e.tensor_tensor(out=thf[:], in0=sf["lamim_f"][:], in1=dtf[:], op=ALU.mult), r=["lamim_f", "dtf"], w=["thf"])

            tmpi = sba("tmpi", [128, 1024], I32)
            tmpa = sba("tmpa", [128, 1024], F32)
            tmpb = sba("tmpb", [128, 1024], F32)

            def sincos(out_ap, ang_ap, n, shift, key_out, key_ang):
                A_, B_, I_ = tmpa[:, 0:n], tmpb[:, 0:n], tmpi[:, 0:n]
                V(lambda e: e.tensor_scalar(out=A_, in0=ang_ap, scalar1=shift, scalar2=1.0 / (2 * PI),
                                            op0=ALU.add, op1=ALU.mult), r=[key_ang], w=["tmpa"])
                V(lambda e: e.tensor_copy(out=I_, in_=A_), r=["tmpa"], w=["tmpi"])
                V(lambda e: e.tensor_copy(out=B_, in_=I_), r=["tmpi"], w=["tmpb"])
                V(lambda e: e.tensor_tensor(out=A_, in0=A_, in1=B_, op=ALU.subtract), r=["tmpa", "tmpb"], w=["tmpa"])
                V(lambda e: e.tensor_scalar(out=B_, in0=A_, scalar1=0.5, scalar2=None, op0=ALU.is_gt),
                  r=["tmpa"], w=["tmpb"])
                V(lambda e: e.tensor_tensor(out=A_, in0=A_, in1=B_, op=ALU.subtract), r=["tmpa", "tmpb"], w=["tmpa"])
                V(lambda e: e.tensor_scalar(out=B_, in0=A_, scalar1=-0.5, scalar2=None, op0=ALU.is_lt),
                  r=["tmpa"], w=["tmpb"])
                V(lambda e: e.tensor_tensor(out=A_, in0=A_, in1=B_, op=ALU.add), r=["tmpa", "tmpb"], w=["tmpa"])
                V(lambda e: e.activation(out=out_ap, in_=A_, func=AF.Sin, scale=2 * PI), r=["tmpa"], w=[key_out], e="act")

            mag128 = sba("mag128", [128, 16], F32)
            ang128 = sba("ang128", [128, 16], F32)
            V(lambda e: e.activation(out=mag128[:], in_=af[:], func=AF.Exp, scale=128.0), r=["af"], w=["mag128"], e="act")
            V(lambda e: e.tensor_scalar(out=ang128[:], in0=thf[:], scalar1=128.0, scalar2=None, op0=ALU.mult), r=["thf"], w=["ang128"])
            sincos(L128[:, 0, :], ang128[:], 16, PI / 2, "L128c", "ang128")
            sincos(L128[:, 1, :], ang128[:], 16, 0.0, "L128s", "ang128")
            V(lambda e: e.tensor_tensor(out=L128[:, 0, :], in0=L128[:, 0, :], in1=mag128[:], op=ALU.mult), r=["L128c", "mag128"], w=["L128c"])
            V(lambda e: e.tensor_tensor(out=L128[:, 1, :], in0=L128[:, 1, :], in1=mag128[:], op=ALU.mult), r=["L128s", "mag128"], w=["L128s"])
            L1 = sba("L1", [128, 2, 16], F32)
            mag1 = sba("mag1", [128, 16], F32)
            V(lambda e: e.activation(out=mag1[:], in_=af[:], func=AF.Exp), r=["af"], w=["mag1"], e="act")
            sincos(L1[:, 0, :], thf[:], 16, PI / 2, "L1c", "thf")
            sincos(L1[:, 1, :], thf[:], 16, 0.0, "L1s", "thf")
            V(lambda e: e.tensor_tensor(out=L1[:, 0, :], in0=L1[:, 0, :], in1=mag1[:], op=ALU.mult), r=["L1c", "mag1"], w=["L1c"])
            V(lambda e: e.tensor_tensor(out=L1[:, 1, :], in0=L1[:, 1, :], in1=mag1[:], op=ALU.mult), r=["L1s", "mag1"], w=["L1s"])
            t1 = sba("t1", [128, 16], F32)
            t2 = sba("t2", [128, 16], F32)
            den = sba("den", [128, 16], F32)
            lr, li = sf["lamre_f"], sf["lamim_f"]
            V(lambda e: e.tensor_tensor(out=den[:], in0=lr[:], in1=lr[:], op=ALU.mult), r=["lamre_f"], w=["den"])
            V(lambda e: e.tensor_tensor(out=t1[:], in0=li[:], in1=li[:], op=ALU.mult), r=["lamim_f"], w=["t1"])
            V(lambda e: e.tensor_tensor(out=den[:], in0=den[:], in1=t1[:], op=ALU.add), r=["den", "t1"], w=["den"])
            V(lambda e: e.reciprocal(out=den[:], in_=den[:]), r=["den"], w=["den"])
            V(lambda e: e.tensor_scalar(out=t1[:], in0=L1[:, 0, :], scalar1=-1.0, scalar2=None, op0=ALU.add), r=["L1c"], w=["t1"])
            V(lambda e: e.tensor_tensor(out=kap[:, 0, :], in0=t1[:], in1=lr[:], op=ALU.mult), r=["t1", "lamre_f"], w=["kapr"])
            V(lambda e: e.tensor_tensor(out=t2[:], in0=L1[:, 1, :], in1=li[:], op=ALU.mult), r=["L1s", "lamim_f"], w=["t2"])
            V(lambda e: e.tensor_tensor(out=kap[:, 0, :], in0=kap[:, 0, :], in1=t2[:], op=ALU.add), r=["kapr", "t2"], w=["kapr"])
            V(lambda e: e.tensor_tensor(out=kap[:, 0, :], in0=kap[:, 0, :], in1=den[:], op=ALU.mult), r=["kapr", "den"], w=["kapr"])
            V(lambda e: e.tensor_tensor(out=kap[:, 1, :], in0=L1[:, 1, :], in1=lr[:], op=ALU.mult), r=["L1s", "lamre_f"], w=["kapi"])
            V(lambda e: e.tensor_tensor(out=t2[:], in0=t1[:], in1=li[:], op=ALU.mult), r=["t1", "lamim_f"], w=["t2"])
            V(lambda e: e.tensor_tensor(out=kap[:, 1, :], in0=kap[:, 1, :], in1=t2[:], op=ALU.subtract), r=["kapi", "t2"], w=["kapi"])
            V(lambda e: e.tensor_tensor(out=kap[:, 1, :], in0=kap[:, 1, :], in1=den[:], op=ALU.mult), r=["kapi", "den"], w=["kapi"])
            tb1 = sba("tb1", [128, 16, 16], F32)
            kr_b = kap[:, 0, :].unsqueeze(2).to_broadcast([128, 16, 16])
            ki_b = kap[:, 1, :].unsqueeze(2).to_broadcast([128, 16, 16])
            V(lambda e: e.tensor_tensor(out=Bbar[:, 0], in0=bf_["bre_f"][:], in1=kr_b, op=ALU.mult), r=["bre_f", "kapr"], w=["Bbr"])
            V(lambda e: e.tensor_tensor(out=tb1[:], in0=bf_["bim_f"][:], in1=ki_b, op=ALU.mult), r=["bim_f", "kapi"], w=["tb1"])
            V(lambda e: e.tensor_tensor(out=Bbar[:, 0], in0=Bbar[:, 0], in1=tb1[:], op=ALU.subtract), r=["Bbr", "tb1"], w=["Bbr"])
            V(lambda e: e.tensor_tensor(out=Bbar[:, 1], in0=bf_["bim_f"][:], in1=kr_b, op=ALU.mult), r=["bim_f", "kapr"], w=["Bbi"])
            V(lambda e: e.tensor_tensor(out=tb1[:], in0=bf_["bre_f"][:], in1=ki_b, op=ALU.mult), r=["bre_f", "kapi"], w=["tb1"])
            V(lambda e: e.tensor_tensor(out=Bbar[:, 1], in0=Bbar[:, 1], in1=tb1[:], op=ALU.add), r=["Bbi", "tb1"], w=["Bbi"])
            V(lambda e: e.memset(Bs1[:], 0.0), w=["Bs1"], e="pool")
            V(lambda e: e.memset(Bs2[:], 0.0), w=["Bs2"], e="pool")
            for glo in range(2):
                ps_ = slice(64 * glo, 64 * glo + 64)
                cs_ = slice(16 * glo, 16 * glo + 16)
                V(lambda e: e.tensor_copy(out=Bs1[ps_, :, 0, cs_], in_=Bbar[ps_, 0]), r=["Bbr"], w=["Bs1"])
                V(lambda e: e.tensor_scalar(out=Bs1[ps_, :, 1, cs_], in0=Bbar[ps_, 1], scalar1=-1.0, scalar2=None, op0=ALU.mult), r=["Bbi"], w=["Bs1"])
                V(lambda e: e.tensor_copy(out=Bs2[ps_, :, 0, cs_], in_=Bbar[ps_, 1]), r=["Bbi"], w=["Bs2"])
                V(lambda e: e.tensor_copy(out=Bs2[ps_, :, 1, cs_], in_=Bbar[ps_, 0]), r=["Bbr"], w=["Bs2"])
            rrb = sba("rrb", [128, 8, 128], F32)
            tcs = sba("tcs", [128, 1], F32)
            DMA(tcs[:], tcol, w=["tcs"])
            dtr = sba("dtr", [128, 1024], F32)
            ar_ = sba("ar_", [128, 1024], F32)
            magr = sba("magr", [128, 1024], F32)
            trg = sba("trg", [128, 1024], F32)
            rrf = rrb[:].rearrange("p a b -> p (a b)")
            for kh in range(2):
                ksl = slice(8 * kh, 8 * kh + 8)
                DMA(rrb[:], ssm_r["logdt_r"][:, ksl, :], w=["rrb"])
                V(lambda e: e.activation(out=dtr[:], in_=rrf, func=AF.Exp), r=["rrb"], w=["dtr"], e="act")
                DMA(rrb[:], ssm_r["lamre_r"][:, ksl, :], w=["rrb"])
                V(lambda e: e.tensor_tensor(out=ar_[:], in0=rrf, in1=dtr[:], op=ALU.mult), r=["rrb", "dtr"], w=["ar_"])
                V(lambda e: e.activation(out=magr[:], in_=ar_[:], func=AF.Exp, scale=tcs[:, 0:1]), r=["ar_", "tcs"], w=["magr"], e="act")
                DMA(rrb[:], ssm_r["lamim_r"][:, ksl, :], w=["rrb"])
                V(lambda e: e.tensor_tensor(out=ar_[:], in0=rrf, in1=dtr[:], op=ALU.mult), r=["rrb", "dtr", "magr"], w=["ar_"])
                V(lambda e: e.tensor_scalar(out=ar_[:], in0=ar_[:], scalar1=tcs[:, 0:1], scalar2=None, op0=ALU.mult), r=["ar_", "tcs"], w=["ar_"])
                for ri, sh in ((0, PI / 2), (1, 0.0)):
                    sincos(trg[:], ar_[:], 1024, sh, "trg", "ar_")
                    V(lambda e: e.tensor_tensor(out=Gt[:, ri, ksl, :].rearrange("p a b -> p (a b)"), in0=trg[:], in1=magr[:], op=ALU.mult),
                      r=["trg", "magr"], w=["Gt"])
            S.barrier()
            sp_.close()
            sba = sba_persist

            xt = [sba(f"xt{i}", [128, 4, D], F32) for i in range(2)]
            xs = sba("xs", [128, 4, D], BF16)
            ss = sba("ss", [128, 4], F32)
            rstd = sba("rstd", [128, 4], F32)
            zT = sba("zT", [128, 8, 512], BF16)
            rC = [sba(f"rC{i}", [128, 512], F32) for i in range(2)]
            rS = [sba(f"rS{i}", [128, 512], F32) for i in range(2)]
            rtmp = sba("rtmp", [128, 512], F32)
            craw = {kv: [sba(f"craw{kv}{i}", [128, 528], BF16) for i in range(2)] for kv in "kv"}
            utm = sba("utm", [128, 512], BF16)
            hid = sba("hid", [128, 4, 32], BF16)
            Eblk = sba("Eblk", [128, 32], F32)
            cC = sba("cC", [128, 32], F32)
            cS = sba("cS", [128, 32], F32)
            Xc = sba("Xc", [128, 32], F32)
            Xn = sba("Xn", [128, 32], F32)
            ta = sba("ta", [128, 16], F32)
            V(lambda e: e.memset(Xc[:], 0.0), w=["Xc"])
            Lr, Li = L128[:, 0, :], L128[:, 1, :]
            lk = ["L128c", "L128s"]
            Msb = sba("Msb", [128, 1024], F32)
            cmb1 = sba("cmb1", [128, 1024], F32)
            cmb2 = sba("cmb2", [128, 1024], F32)
            for kv in "kv":
                for i in range(2):
                    V(lambda e: e.memset(craw[kv][i][:], 0.0), w=[f"craw{kv}{i}"], e="pool")

            def compress(Tc, nblk):
                pi_ = Tc % 2
                ph = pbank[5]
                for a, kv in enumerate("kv"):
                    for g in range(2):
                        col = 128 + 32 * (2 * a + g)
                        for l in range(32):
                            MM(ph[:, col:col + nblk], W1[kv][64 * g:64 * g + 64, l, :],
                               craw[kv][pi_][64 * g:64 * g + 64, l:l + 16 * (nblk - 1) + 1:16],
                               l == 0, l == 31, r=[f"W1{kv}", f"craw{kv}{pi_}", "hidall"], w=["pb5"])
                        if DBG.get("cstage", 9) < 1:
                            continue
                        V(lambda e: e.activation(out=hid[:, 2 * a + g, 0:nblk], in_=ph[:, col:col + nblk],
                                                 func=AF.Gelu_apprx_tanh, bias=biasc[kv][:, 0:1], scale=1.0),
                          r=["pb5", f"biasc{kv}"], w=[f"hid{a}{g}", "hidall"], e="act")
                n0 = 32 * Tc
                if DBG.get("cstage", 9) < 2:
                    return
                DMA(cC[:], cmpC[:, n0:n0 + 32], w=["cC"], q="pool")
                DMA(cS[:], cmpS[:, n0:n0 + 32], w=["cS"], q="pool")
                for j, nm in enumerate(("k", "ksw", "v")):
                    a = 0 if nm != "v" else 1
                    col = 256 + 32 * j
                    for g in range(2):
                        MM(ph[:, col:col + nblk], W2[nm][:, g, :], hid[:, 2 * a + g, 0:nblk], g == 0, g == 1,
                           r=[f"W2{nm}", f"hid{a}{g}"], w=["pb5"])
                if DBG.get("cstage", 9) < 3:
                    return
                V(lambda e: e.tensor_tensor(out=rtmp[:, 0:nblk], in0=ph[:, 256:256 + nblk], in1=cC[:, 0:nblk], op=ALU.mult),
                  r=["pb5", "cC"], w=["rtmp"])
                V(lambda e: e.tensor_tensor(out=rtmp[:, 32:32 + nblk], in0=ph[:, 288:288 + nblk], in1=cS[:, 0:nblk], op=ALU.mult),
                  r=["pb5", "cS"], w=["rtmp"])
                V(lambda e: e.tensor_tensor(out=kcT[:, n0:n0 + nblk], in0=rtmp[:, 0:nblk], in1=rtmp[:, 32:32 + nblk], op=ALU.add),
                  r=["rtmp"], w=["kcT"])
                V(lambda e: e.tensor_copy(out=vcT[:, n0:n0 + nblk], in_=ph[:, 320:320 + nblk]), r=["pb5"], w=["vcT"])

            for T in range(DBG.get("ntiles", NT)):
                p_ = T % 2
                x_ = xt[p_]
                DMA(x_[:], x_all[T].rearrange("(a p) d -> p a d", p=128), w=[f"xt{p_}"])
                DMA(rC[p_][:], ropeC[T], w=[f"rC{p_}"], q="pool")
                DMA(rS[p_][:], ropeS[T], w=[f"rS{p_}"], q="pool")
                for a in range(4):
                    V(lambda e: e.activation(out=xs[:, a, :], in_=x_[:, a, :], func=AF.Square, accum_out=ss[:, a:a + 1]),
                      r=[f"xt{p_}"], w=[f"xs{a}", "ss"], e="act")
                V(lambda e: e.activation(out=rstd[:], in_=ss[:], func=AF.Sqrt, bias=EPS, scale=1.0 / D),
                  r=["ss"], w=["rstd"], e="act")
                V(lambda e: e.reciprocal(out=rstd[:], in_=rstd[:]), r=["rstd"], w=["rstd"])
                for a in range(4):
                    V(lambda e: e.tensor_scalar(out=xs[:, a, :], in0=x_[:, a, :], scalar1=rstd[:, a:a + 1], scalar2=None, op0=ALU.mult),
                      r=[f"xt{p_}", "rstd"], w=[f"xs{a}"], e=("dve" if a % 2 else "pool"))
                for k2 in range(4):
                    pbb = pbank[k2][:].bitcast(BF16)
                    for dd in range(2):
                        dc = 2 * k2 + dd
                        for a in range(4):
                            S.op("pe", lambda e: e.transpose(out=pbb[:, dd * 512 + a * 128: dd * 512 + a * 128 + 128],
                                                             in_=xs[:, a, dc * 128:(dc + 1) * 128], identity=identb[:]),
                                 reads=[f"xs{a}", "identb"], writes=[f"pb{k2}"])
                    CP(zT[:, 2 * k2:2 * k2 + 2, :].rearrange("p a b -> p (a b)"), pbb,
                       r=[f"pb{k2}"], w=[f"zT{k2}"], e=("act" if k2 % 2 else "dve"))
                zk = [f"zT{k2}" for k2 in range(4)]
                for blk in range(4):
                    for dc in range(8):
                        MM(pbank[blk][:], Wa[:, dc, blk * 128:(blk + 1) * 128], zT[:, dc, :], dc == 0, dc == 7,
                           r=["Wa"] + zk, w=[f"pb{blk}"])
                V(lambda e: e.tensor_tensor(out=rtmp[:], in0=pbank[1][:], in1=rS[p_][:], op=ALU.mult), r=["pb1", f"rS{p_}"], w=["rtmp"])
                V(lambda e: e.tensor_tensor(out=rC[p_][:], in0=pbank[0][:], in1=rC[p_][:], op=ALU.mult), r=["pb0", f"rC{p_}"], w=[f"rC{p_}"])
                V(lambda e: e.tensor_tensor(out=ksT[:, 512 * T:512 * T + 512], in0=rC[p_][:], in1=rtmp[:], op=ALU.add),
                  r=[f"rC{p_}", "rtmp"], w=["ksT"])
                for a, kv in enumerate("kv"):
                    V(lambda e: e.activation(out=craw[kv][p_][:, 0:512], in_=pbank[2 + a][:], func=AF.Copy),
                      r=[f"pb{2 + a}"], w=[f"craw{kv}{p_}"], e="act")
                    if T > 0:
                        V(lambda e: e.tensor_copy(out=craw[kv][1 - p_][:, 512:528], in_=craw[kv][p_][:, 0:16]),
                          r=[f"craw{kv}{p_}"], w=[f"craw{kv}{1 - p_}"], e="pool")
                for a in range(0 if not DBG.get("nossm") else 4, 4):
                    b = 4 * T + a
                    for dc in range(8):
                        MM(pbank[5][:, 0:128], zT[:, dc, a * 128:(a + 1) * 128], Wa[:, dc, 512:640], dc == 0, dc == 7,
                           r=["Wa"] + zk, w=["pb5"])
                    CP(vsaug[:, b, :, 0:64], pbank[5][:, 0:128].rearrange("p (g d) -> p g d", g=2),
                       r=["pb5"], w=["vsaug"], e="act")
                    for dc in range(8):
                        MM(pbank[4][:], zT[:, dc, a * 128:(a + 1) * 128], Wa[:, dc, 640:1152], dc == 0, dc == 7,
                           r=["Wa"] + zk, w=["pb4"])
                    V(lambda e: e.tensor_copy(out=utm[:], in_=pbank[4][:]), r=["pb4"], w=["utm"])
                    for k in range(16):
                        for ri in range(2):
                            bank = pbank[6 + k // 8]
                            c0 = (k % 8) * 64 + ri * 32
                            MM(bank[:, c0:c0 + 32], Gt[:, ri, k, :], utm[:, 32 * k:32 * k + 32], True, True,
                               r=["Gt", "utm"], w=[f"pb{6 + k // 8}"])
                    for hh in range(2):
                        CP(Msb[:, hh * 512:(hh + 1) * 512], pbank[6 + hh][:], r=[f"pb{6 + hh}"], w=[f"Msb{hh}"], e="act")
                        V(lambda e: e.tensor_tensor(out=cmb1[:, hh * 512:(hh + 1) * 512], in0=Msb[:, hh * 512:(hh + 1) * 512],
                                                    in1=Bs1[:, 8 * hh:8 * hh + 8].rearrange("p a b c -> p (a b c)"), op=ALU.mult),
                          r=[f"Msb{hh}", "Bs1"], w=["cmb1"])
                        V(lambda e: e.tensor_tensor(out=cmb2[:, hh * 512:(hh + 1) * 512], in0=Msb[:, hh * 512:(hh + 1) * 512],
                                                    in1=Bs2[:, 8 * hh:8 * hh + 8].rearrange("p a b c -> p (a b c)"), op=ALU.mult),
                          r=[f"Msb{hh}", "Bs2"], w=["cmb2"], e="pool")
                    V(lambda e: e.tensor_reduce(out=Eblk[:, 0:16], in_=cmb1[:].rearrange("p (k x) -> p k x", k=16), axis=AX.X, op=ALU.add),
                      r=["cmb1"], w=["Eblk"])
                    V(lambda e: e.tensor_reduce(out=Eblk[:, 16:32], in_=cmb2[:].rearrange("p (k x) -> p k x", k=16), axis=AX.X, op=ALU.add),
                      r=["cmb2"], w=["Eblk"])
                    if b % 4 == 0:
                        V(lambda e: e.tensor_copy(out=Xtile[:, b // 4, :], in_=Xc[:]), r=["Xc"], w=["Xtile"])
                    V(lambda e: e.tensor_tensor(out=Xn[:, 0:16], in0=Xc[:, 0:16], in1=Lr, op=ALU.mult), r=["Xc"] + lk, w=["Xn"])
                    V(lambda e: e.tensor_tensor(out=ta[:], in0=Xc[:, 16:32], in1=Li, op=ALU.mult), r=["Xc"] + lk, w=["ta"])
                    V(lambda e: e.tensor_tensor(out=Xn[:, 0:16], in0=Xn[:, 0:16], in1=ta[:], op=ALU.subtract), r=["Xn", "ta"], w=["Xn"])
                    V(lambda e: e.tensor_tensor(out=Xn[:, 16:32], in0=Xc[:, 0:16], in1=Li, op=ALU.mult), r=["Xc"] + lk, w=["Xn"])
                    V(lambda e: e.tensor_tensor(out=ta[:], in0=Xc[:, 16:32], in1=Lr, op=ALU.mult), r=["Xc"] + lk, w=["ta"])
                    V(lambda e: e.tensor_tensor(out=Xn[:, 16:32], in0=Xn[:, 16:32], in1=ta[:], op=ALU.add), r=["Xn", "ta"], w=["Xn"])
                    V(lambda e: e.tensor_tensor(out=Xc[:], in0=Xn[:], in1=Eblk[:], op=ALU.add), r=["Xn", "Eblk"], w=["Xc"])
                if T > 0 and not DBG.get("nocompress"):
                    compress(T - 1, 32)
            if not DBG.get("nocompress"):
                compress(NT - 1, 31)

            if debug == "A":
                def doutt(name, shape, dt):
                    dbg_out[name] = nc.dram_tensor("dbg_" + name, list(shape), dt, kind="ExternalOutput").ap()
                    return dbg_out[name]
                DMA(doutt("ksT", [128, SEQ], BF16), ksT[:], r=["ksT"])
                DMA(doutt("vs", [128, 128 * 130], BF16), vsaug[:].rearrange("p a g d -> p (a g d)"), r=["vsaug"])
                DMA(doutt("kcT", [128, 1024], BF16), kcT[:], r=["kcT"])
                DMA(doutt("vcT", [128, 1024], BF16), vcT[:], r=["vcT"])
                DMA(doutt("Xtile", [128, NT * 32], F32), Xtile[:].rearrange("p a b -> p (a b)"), r=["Xtile"])
            S.barrier()


        with ExitStack() as sB:
            sbb = lambda name, shape, dt, st=sB: sb(name, shape, dt, st)
            mWM0, mTRI, mLMA = sbb("mWM0", [128, 128], BF16), sbb("mTRI", [128, 128], BF16), sbb("mLMA", [128, 128], BF16)
            blk64 = sbb("blk64", [128, 256], F32)
            cmpend = sbb("cmpend", [128, 8], F32)
            tpos = sbb("tpos", [128, 16], F32)
            tposm = sbb("tposm", [128, 16], F32)
            validW = sbb("validW", [128, 16, 5], F32)
            validA = sbb("validA", [128, 16], F32)
            vcaug = sbb("vcaug", [128, 8, 2, 65], BF16)
            qTz = sbb("qTz", [128, 2, 4, 512], BF16)
            kslocT = sbb("kslocT", [128, 640], BF16)
            kwT = sbb("kwT", [128, 1024], BF16)
            vslaug = sbb("vslaug", [128, 5, 2, 65], BF16)
            vwaug = sbb("vwaug", [128, 8, 2, 65], BF16)
            uT = sbb("uT", [128, 4, 512], BF16)
            utm = sbb("utmB", [128, 4, 512], BF16)
            gsig = sbb("gsig", [128, 4, 24], F32)
            aout = sbb("aout", [128, 4, 512], BF16)
            sout = sbb("sout", [128, 4, 512], BF16)
            tposBt = sbb("tposBt", [128, 512], F32)
            for (t_, d_, k_) in ((mWM0, mWM0_d, "mWM0"), (mTRI, mTRI_d, "mTRI"),
                                 (mLMA, mLMA_d, "mLMA"), (blk64, blk64_d, "blk64"), (cmpend, cmpend_d, "cmpend"),
                                 (tpos, tpos_d, "tpos"), (tposm, tposm_d, "tposm"), (validW, validW_d, "validW"),
                                 (validA, validA_d, "validA")):
                DMA(t_[:], d_, w=[k_])
            V(lambda e: e.memset(vcaug[:], 1.0), w=["vcaug"], e="pool")
            V(lambda e: e.memset(vslaug[:], 1.0), w=["vslaug"], e="pool")
            V(lambda e: e.memset(vwaug[:], 1.0), w=["vwaug"], e="pool")
            V(lambda e: e.memset(qTz[:], 0.0), w=["qTz"], e="pool")
            for m in range(8):
                pbb = pbank[7][:].bitcast(BF16)
                S.op("pe", lambda e: e.transpose(out=pbb[:, 0:128], in_=vcT[:, 128 * m:128 * m + 128], identity=identb[:]),
                     reads=["vcT", "identb"], writes=["pb7"])
                CP(vcaug[:, m, :, 0:64], pbb[:, 0:128].rearrange("p (g d) -> p g d", g=2), r=["pb7"], w=["vcaug"])

            cvin = [sbb(f"cvin{i}", [128, D], F32) for i in range(2)]
            cvout = [sbb(f"cvout{i}", [128, D], BF16) for i in range(2)]
            bg = []
            for ti, (src_t, dst_t) in enumerate(((utab, uvb[:, 0:D]), (vtab, uvb[:, D:2 * D]))):
                def op_in(ch, src_t=src_t):
                    i2 = ch % 2
                    return lambda: DMA(cvin[i2][:], src_t[128 * ch:128 * ch + 128, :], w=[f"cvin{i2}"])

                def op_cast(ch):
                    i2 = ch % 2
                    return lambda: CP(cvout[i2][:], cvin[i2][:], r=[f"cvin{i2}"], w=[f"cvout{i2}"], e="pool")

                def op_out(ch, dst_t=dst_t):
                    i2 = ch % 2
                    return lambda: DMA(dst_t[128 * ch:128 * ch + 128, :], cvout[i2][:], r=[f"cvout{i2}"], w=["tabbf"])
                bg.append(op_in(0))
                bg.append(op_in(1))
                for ch in range(128):
                    bg.append(op_cast(ch))
                    bg.append(op_out(ch))
                    if ch + 2 < 128:
                        bg.append(op_in(ch + 2))

            def bg_step(n=1):
                for _ in range(n):
                    if bg:
                        bg.pop(0)()

            wstB = [sbb(f"wstB{i}", [128, 8, 128], F32) for i in range(2)]
            wblB = [sbb(f"wblB{i}", [128, 8, 128], BF16) for i in range(3)]
            wctr = [0]

            def load_wblk(c0):
                i = wctr[0]
                wctr[0] += 1
                bl_ = wblB[i % 3]
                DMA(bl_[:], wBb[c0 // 128], r=["wBb"], w=[f"wblB{i % 3}"])
                return bl_, f"wblB{i % 3}"

            for jt in range(DBG.get("ntilesB", NOWN)):
                with ExitStack() as s1:
                    sb1 = lambda name, shape, dt: sb(name, shape, dt, s1)
                    xb = [sb1(f"xb{i}", [128, D], F32) for i in range(2)]
                    xsb = [sb1(f"xsb{i}", [128, D], BF16) for i in range(2)]
                    ssb = sb1("ssb", [128, 8], F32)
                    rsb = sb1("rsb", [128, 8], F32)
                    zT = sb1("zTB", [128, 8, 512], BF16)
                    rCt = sb1("rCt", [128, 512], F32)
                    rSt = sb1("rSt", [128, 512], F32)
                    rt1 = sb1("rt1", [128, 512], F32)
                    rt2 = sb1("rt2", [128, 512], F32)
                    DMA(tposBt[:], tposB_d[jt], w=["tposBt"])
                    for st in range(2):
                        DMA(rCt[:], ropeCl[jt, st], w=["rCt"], q="pool")
                        DMA(rSt[:], ropeSl[jt, st], w=["rSt"], q="pool")
                        for a in range(4):
                            i2 = a % 2
                            DMA(xb[i2][:], x_loc[jt, st * 512 + a * 128: st * 512 + a * 128 + 128, :], w=[f"xb{i2}"])
                            cidx = 4 * st + a
                            V(lambda e: e.activation(out=xsb[i2][:], in_=xb[i2][:], func=AF.Square, accum_out=ssb[:, cidx:cidx + 1]),
                              r=[f"xb{i2}"], w=[f"xsb{i2}", "ssb"], e="act")
                            V(lambda e: e.activation(out=rsb[:, cidx:cidx + 1], in_=ssb[:, cidx:cidx + 1], func=AF.Sqrt, bias=EPS, scale=1.0 / D),
                              r=["ssb"], w=["rsb"], e="act")
                            V(lambda e: e.reciprocal(out=rsb[:, cidx:cidx + 1], in_=rsb[:, cidx:cidx + 1]), r=["rsb"], w=["rsb"])
                            V(lambda e: e.tensor_scalar(out=xsb[i2][:], in0=xb[i2][:], scalar1=rsb[:, cidx:cidx + 1], scalar2=None, op0=ALU.mult),
                              r=[f"xb{i2}", "rsb"], w=[f"xsb{i2}"])
                            pbb = pbank[a % 2][:].bitcast(BF16)
                            for dc in range(8):
                                S.op("pe", lambda e: e.transpose(out=pbb[:, dc * 128:(dc + 1) * 128], in_=xsb[i2][:, dc * 128:(dc + 1) * 128],
                                                                 identity=identb[:]), reads=[f"xsb{i2}", "identb"], writes=[f"pb{a % 2}"])
                            CP(zT[:, :, a * 128:(a + 1) * 128], pbb.rearrange("p (c t) -> p c t", c=8), r=[f"pb{a % 2}"], w=["zTB"],
                               e=("act" if a % 2 else "dve"))

                        def fm_block(c0, bank):
                            wb_, wk_ = load_wblk(c0)
                            for dc in range(8):
                                MM(pbank[bank][:], wb_[:, dc, :], zT[:, dc, :], dc == 0, dc == 7, r=[wk_, "zTB"], w=[f"pb{bank}"])

                        def rope_evac(bA, bS, outs):
                            V(lambda e: e.tensor_tensor(out=rt1[:], in0=pbank[bA][:], in1=rCt[:], op=ALU.mult), r=[f"pb{bA}", "rCt"], w=["rt1"])
                            V(lambda e: e.tensor_tensor(out=rt2[:], in0=pbank[bS][:], in1=rSt[:], op=ALU.mult), r=[f"pb{bS}", "rSt"], w=["rt2"])
                            for (o_, ps_, k_, cs_) in outs:
                                V(lambda e: e.tensor_tensor(out=o_, in0=rt1[ps_, cs_], in1=rt2[ps_, cs_], op=ALU.add), r=["rt1", "rt2"], w=[k_], e="pool")

                        if st == 1:
                            for hl in range(4):
                                fm_block(128 * hl, 2)
                                fm_block(512 + 128 * hl, 3)
                                rope_evac(2, 3, [(qTz[0:64, 0, hl, :], slice(0, 64), "qTz", slice(0, 512)), (qTz[64:128, 1, hl, :], slice(64, 128), "qTz", slice(0, 512))])
                        fm_block(1024, 2)
                        fm_block(1152, 3)
                        if st == 0:
                            rope_evac(2, 3, [(kslocT[:, 0:128], slice(0, 128), "kslocT", slice(384, 512))])
                        else:
                            rope_evac(2, 3, [(kslocT[:, 128:640], slice(0, 128), "kslocT", slice(0, 512))])
                        fm_block(1280, 2)
                        fm_block(1408, 3)
                        rope_evac(2, 3, [(kwT[:, st * 512:(st + 1) * 512], slice(0, 128), "kwT", slice(0, 512))])
                        if st == 1:
                            for ub in range(4):
                                fm_block(1536 + 128 * ub, 2)
                                CP(uT[:, ub, :], pbank[2][:], r=["pb2"], w=["uT"], e="act")
                        chunks = [("vw", 2176)] + ([("vs", 2048)] if True else [])
                        if st == 1:
                            chunks += [("gt", 2304)] + [(f"u{i}", 2432 + 128 * i) for i in range(4)]
                        for (nm, c0) in chunks:
                            wb_, wk_ = load_wblk(c0)
                            for a in range(4):
                                if nm == "vs" and st == 0 and a != 3:
                                    continue
                                for dc in range(8):
                                    MM(pbank[4][:, a * 128:(a + 1) * 128], zT[:, dc, a * 128:(a + 1) * 128], wb_[:, dc, :], dc == 0, dc == 7,
                                       r=[wk_, "zTB"], w=["pb4"])
                            for a in range(4):
                                src = pbank[4][:, a * 128:(a + 1) * 128]
                                if nm == "vw":
                                    CP(vwaug[:, 4 * st + a, :, 0:64], src.rearrange("p (g d) -> p g d", g=2), r=["pb4"], w=["vwaug"], e="act")
                                elif nm == "vs":
                                    if st == 0 and a != 3:
                                        continue
                                    CP(vslaug[:, (0 if st == 0 else 1 + a), :, 0:64], src.rearrange("p (g d) -> p g d", g=2), r=["pb4"], w=["vslaug"], e="act")
                                elif nm == "gt":
                                    V(lambda e: e.activation(out=gsig[:, a, :], in_=src[:, 0:24], func=AF.Sigmoid), r=["pb4"], w=["gsig"], e="act")
                                else:
                                    ui = int(nm[1])
                                    CP(utm[:, a, 128 * ui:128 * ui + 128], src, r=["pb4"], w=["utmB"], e="act")
                S.barrier()

                if DBG.get("stopB1"):
                    continue
                with ExitStack() as s2:
                    sb2 = lambda name, shape, dt: sb(name, shape, dt, s2)
                    Fb = sb2("Fb", [128, 8192], BF16)
                    OV = sb2("OV", [128, 8, 256], BF16)
                    DMA(Fb[:], Fbase_d, w=["Fb"])
                    DMA(OV[:], OV_d, w=["OV"])
                    Eb = [sb2(f"Eb{i}", [128, 512], BF16) for i in range(3)]
                    cmall = sb2("cmall", [128, 8, 128], BF16)
                    negT4 = sb2("negT4", [128, 2, 512], BF16)
                    negsel = sb2("negsel", [128, 256], BF16)
                    impq = sb2("impq", [128, 256], F32)
                    w1_ = sb2("w1_", [128, 256], F32)
                    w2_ = sb2("w2_", [128, 256], F32)
                    w3_ = sb2("w3_", [128, 256], F32)
                    valid_ = sb2("valid_", [128, 256], F32)
                    local_ = sb2("local_", [128, 256], F32)
                    m8a = sb2("m8a", [128, 8], F32)
                    m8b = sb2("m8b", [128, 8], F32)
                    rz = sb2("rz", [128, 3, 4], F32)
                    coef = sb2("coef", [128, 3, 4], F32)
                    atmp = sb2("atmp", [128, 64], F32)
                    ectr = [0]

                    def next_E():
                        i = ectr[0] % 3
                        ectr[0] += 1
                        return Eb[i], f"Eb{i}"

                    sctr = [0]

                    def next_S():
                        i = (0, 1, 7)[sctr[0] % 3]
                        sctr[0] += 1
                        return pbank[i], f"pb{i}"

                    def acc_view(bank):
                        return pbank[bank][:].rearrange("p (h x) -> p h x", h=4)

                    for qb in range(4):
                        col = 4 * jt + qb
                        tsl = slice(128 * qb, 128 * qb + 128)
                        for m in range(8):
                            V(lambda e: e.tensor_scalar(out=cmall[:, m, :], in0=tposBt[:, tsl], scalar1=cmpend[:, m:m + 1], scalar2=None, op0=ALU.is_ge),
                              r=["tposBt", "cmpend"], w=["cmall"])
                        V(lambda e: e.tensor_scalar(out=valid_[:], in0=blk64[:], scalar1=tpos[:, col:col + 1], scalar2=None, op0=ALU.is_le),
                          r=["blk64", "tpos"], w=["valid_"])
                        V(lambda e: e.tensor_scalar(out=local_[:], in0=blk64[:], scalar1=tposm[:, col:col + 1], scalar2=None, op0=ALU.is_gt),
                          r=["blk64", "tposm"], w=["local_"])
                        V(lambda e: e.tensor_tensor(out=local_[:], in0=local_[:], in1=valid_[:], op=ALU.mult), r=["local_", "valid_"], w=["local_"])
                        for g in range(2):
                            qrhs = qTz[:, g, :, tsl]
                            def run_pass(items):
                                def issue_S(it):
                                    sbk, skey = next_S()
                                    it["s"](sbk, skey)
                                    return sbk, skey
                                pend = [issue_S(items[0])]
                                if len(items) > 1:
                                    pend.append(issue_S(items[1]))
                                for i, it in enumerate(items):
                                    if i + 2 < len(items):
                                        pend.append(issue_S(items[i + 2]))
                                    sbk, skey = pend.pop(0)
                                    E_, ek = next_E()
                                    V(lambda e: e.activation(out=E_[:], in_=sbk[:], func=AF.Exp, scale=0.125), r=[skey], w=[ek], e="act")
                                    if it.get("post"):
                                        it["post"](E_, ek)
                                    it["pv"](E_, ek)
                                    if it.get("bg"):
                                        bg_step(1)

                            def mk_cmp(m):
                                def s_(sbk, skey):
                                    MM(sbk[:], kcT[:, 128 * m:128 * m + 128], qrhs, True, True, r=["kcT", "qTz"], w=[skey])

                                def post(E_, ek):
                                    E3 = E_[:].rearrange("p (h t) -> p h t", h=4)
                                    V(lambda e: e.tensor_tensor(out=E3, in0=E3, in1=cmall[:, m, :].unsqueeze(1).to_broadcast([128, 4, 128]), op=ALU.mult),
                                      r=[ek, "cmall"], w=[ek])

                                def pv(E_, ek):
                                    for hl in range(4):
                                        MM(pbank[2][:, hl * 128:hl * 128 + 65], E_[:, hl * 128:(hl + 1) * 128], vcaug[:, m, g, :], m == 0 and hl == 0, m == 7,
                                           r=[ek, "vcaug"], w=["pb2"])
                                    for hl in range(4):
                                        bk = 3 + hl // 2
                                        MM(pbank[bk][:, (hl % 2) * 256:(hl % 2) * 256 + 256], E_[:, hl * 128:(hl + 1) * 128], OV[:, m, :], m == 0 and hl % 2 == 0, m == 7,
                                           r=[ek, "OV"], w=[f"pb{bk}"])
                                return dict(s=s_, post=post, pv=pv)

                            run_pass([mk_cmp(m) for m in range(8)])
                            V(lambda e: e.tensor_scalar(out=rz[:, 0, :], in0=acc_view(2)[:, :, 64], scalar1=1e-30, scalar2=None, op0=ALU.max),
                              r=["pb2"], w=["rz0"])
                            V(lambda e: e.reciprocal(out=rz[:, 0, :], in_=rz[:, 0, :]), r=["rz0"], w=["rz0"])
                            for hl in range(4):
                                bk = 3 + hl // 2
                                src = pbank[bk][:, (hl % 2) * 256:(hl % 2) * 256 + 256]
                                if hl == 0:
                                    V(lambda e: e.tensor_scalar(out=impq[:], in0=src, scalar1=rz[:, 0, 0:1], scalar2=None, op0=ALU.mult),
                                      r=[f"pb{bk}", "rz0"], w=["impq"])
                                else:
                                    V(lambda e: e.scalar_tensor_tensor(out=impq[:], in0=src, scalar=rz[:, 0, hl:hl + 1], in1=impq[:],
                                                                       op0=ALU.mult, op1=ALU.add), r=[f"pb{bk}", "rz0", "impq"], w=["impq"])
                            V(lambda e: e.tensor_scalar(out=w1_[:], in0=valid_[:], scalar1=-1.0, scalar2=1e30, op0=ALU.add, op1=ALU.mult), r=["valid_"], w=["w1_"])
                            V(lambda e: e.tensor_tensor(out=w2_[:], in0=impq[:], in1=valid_[:], op=ALU.mult), r=["impq", "valid_"], w=["w2_"])
                            V(lambda e: e.tensor_tensor(out=w2_[:], in0=w2_[:], in1=w1_[:], op=ALU.add), r=["w2_", "w1_"], w=["w2_"])
                            V(lambda e: e.tensor_scalar(out=w1_[:], in0=local_[:], scalar1=1e9, scalar2=None, op0=ALU.mult), r=["local_"], w=["w1_"])
                            V(lambda e: e.memset(w1_[:, 0:1], 1e9), r=[], w=["w1_"])
                            V(lambda e: e.tensor_tensor(out=w2_[:], in0=w2_[:], in1=w1_[:], op=ALU.max), r=["w2_", "w1_"], w=["w2_"])
                            V(lambda e: e.max(out=m8a[:], in_=w2_[:]), r=["w2_"], w=["m8a"])
                            V(lambda e: e.match_replace(out=w3_[:], in_to_replace=m8a[:], in_values=w2_[:], imm_value=-3e38), r=["w2_", "m8a"], w=["w3_"])
                            V(lambda e: e.max(out=m8b[:], in_=w3_[:]), r=["w3_"], w=["m8b"])
                            V(lambda e: e.tensor_scalar(out=w3_[:], in0=w2_[:], scalar1=m8b[:, 7:8], scalar2=None, op0=ALU.is_ge), r=["w2_", "m8b"], w=["w3_"])
                            V(lambda e: e.tensor_tensor(out=w3_[:], in0=w3_[:], in1=valid_[:], op=ALU.mult), r=["w3_", "valid_"], w=["w3_"])
                            V(lambda e: e.tensor_scalar(out=w1_[:], in0=local_[:], scalar1=-1.0, scalar2=-1.0, op0=ALU.add, op1=ALU.mult), r=["local_"], w=["w1_"])
                            V(lambda e: e.tensor_tensor(out=w3_[:], in0=w3_[:], in1=w1_[:], op=ALU.mult), r=["w3_", "w1_"], w=["w3_"])
                            V(lambda e: e.tensor_scalar(out=negsel[:], in0=w3_[:], scalar1=-1.0, scalar2=1e4, op0=ALU.add, op1=ALU.mult), r=["w3_"], w=["negsel"])
                            pbb7 = pbank[7][:].bitcast(BF16)
                            for hf in range(2):
                                S.op("pe", lambda e: e.transpose(out=pbb7[:, hf * 128:(hf + 1) * 128], in_=negsel[:, hf * 128:(hf + 1) * 128], identity=identb[:]),
                                     reads=["negsel", "identb"], writes=["pb7"])
                            for hf in range(2):
                                CP(negT4[:, hf, :].rearrange("p (h t) -> p h t", h=4),
                                   pbb7[:, hf * 128:(hf + 1) * 128].unsqueeze(1).to_broadcast([128, 4, 128]), r=["pb7"], w=["negT4"])
                            J = 4 * (8 * jt + 7) + qb + 1
                            J = min(J, DBG.get("maxJ", 1000))

                            def mk_win(c5):
                                c0 = 128 * qb + 128 * c5

                                def s_(sbk, skey):
                                    MM(sbk[:], kwT[:, c0:c0 + 128], qrhs, True, True, r=["kwT", "qTz"], w=[skey])

                                def post(E_, ek):
                                    if c5 in (0, 4):
                                        msk, mk = (mWM0, "mWM0") if c5 == 0 else (mTRI, "mTRI")
                                        E3 = E_[:].rearrange("p (h t) -> p h t", h=4)
                                        V(lambda e: e.tensor_tensor(out=E3, in0=E3, in1=msk[:].unsqueeze(1).to_broadcast([128, 4, 128]), op=ALU.mult), r=[ek, mk], w=[ek])
                                    V(lambda e: e.tensor_scalar(out=E_[:], in0=E_[:], scalar1=validW[:, col, c5:c5 + 1], scalar2=None, op0=ALU.mult),
                                      r=[ek, "validW"], w=[ek])

                                def pv(E_, ek):
                                    for hl in range(4):
                                        MM(pbank[6][:, hl * 128:hl * 128 + 65], E_[:, hl * 128:(hl + 1) * 128], vwaug[:, qb + c5, g, :], c5 == 0 and hl == 0, c5 == 4,
                                           r=[ek, "vwaug"], w=["pb6"])
                                return dict(s=s_, post=post, pv=pv)

                            def mk_loc(ci):
                                c0, msk, mk = ((128 * qb, mLMA, "mLMA"), (128 * qb + 128, mTRI, "mTRI"))[ci]

                                def s_(sbk, skey):
                                    MM(sbk[:], kslocT[:, c0:c0 + 128], qrhs, True, True, r=["kslocT", "qTz"], w=[skey])

                                def post(E_, ek):
                                    E3 = E_[:].rearrange("p (h t) -> p h t", h=4)
                                    V(lambda e: e.tensor_tensor(out=E3, in0=E3, in1=msk[:].unsqueeze(1).to_broadcast([128, 4, 128]), op=ALU.mult), r=[ek, mk], w=[ek])
                                    if ci == 0:
                                        V(lambda e: e.tensor_scalar(out=E_[:], in0=E_[:], scalar1=validA[:, col:col + 1], scalar2=None, op0=ALU.mult),
                                          r=[ek, "validA"], w=[ek])

                                def pv(E_, ek):
                                    for hl in range(4):
                                        MM(pbank[5][:, hl * 128:hl * 128 + 65], E_[:, hl * 128:(hl + 1) * 128], vslaug[:, qb + ci, g, :], ci == 0 and hl == 0, False,
                                           r=[ek, "vslaug"], w=["pb5"])
                                return dict(s=s_, post=post, pv=pv)

                            def mk_sel(j):
                                def s_(sbk, skey):
                                    MM(sbk[:], ksT[:, 128 * j:128 * j + 128], qrhs, True, False, r=["ksT", "qTz"], w=[skey])
                                    MM(sbk[:], Fb[:, 128 * (j % 64):128 * (j % 64) + 128], negT4[:, j // 64, :], False, True, r=["Fb", "negT4"], w=[skey])

                                def pv(E_, ek):
                                    for hl in range(4):
                                        MM(pbank[5][:, hl * 128:hl * 128 + 65], E_[:, hl * 128:(hl + 1) * 128], vsaug[:, j, g, :], False, j == J - 1,
                                           r=[ek, "vsaug"], w=["pb5"])
                                return dict(s=s_, post=None, pv=pv, bg=True)

                            run_pass([mk_win(c5) for c5 in range(5)] + [mk_loc(ci) for ci in range(2)] + [mk_sel(j) for j in range(J)])
                            for jb, bank in ((1, 5), (2, 6)):
                                V(lambda e: e.tensor_scalar(out=rz[:, jb, :], in0=acc_view(bank)[:, :, 64], scalar1=1e-30, scalar2=None, op0=ALU.max),
                                  r=[f"pb{bank}"], w=[f"rz{jb}"])
                                V(lambda e: e.reciprocal(out=rz[:, jb, :], in_=rz[:, jb, :]), r=[f"rz{jb}"], w=[f"rz{jb}"])
                            gv = gsig[:, qb, :].rearrange("p (h j) -> p j h", j=3)
                            V(lambda e: e.tensor_tensor(out=coef[:], in0=rz[:], in1=gv[:, :, 4 * g:4 * g + 4], op=ALU.mult),
                              r=["rz0", "rz1", "rz2", "gsig"], w=["coef"])
                            for hl in range(4):
                                h_ = 4 * g + hl
                                V(lambda e: e.tensor_scalar(out=atmp[:], in0=pbank[2][:, hl * 128:hl * 128 + 64], scalar1=coef[:, 0, hl:hl + 1], scalar2=None, op0=ALU.mult),
                                  r=["pb2", "coef"], w=["atmp"])
                                V(lambda e: e.scalar_tensor_tensor(out=atmp[:], in0=pbank[5][:, hl * 128:hl * 128 + 64], scalar=coef[:, 1, hl:hl + 1], in1=atmp[:],
                                                                   op0=ALU.mult, op1=ALU.add), r=["pb5", "coef", "atmp"], w=["atmp"])
                                V(lambda e: e.scalar_tensor_tensor(out=aout[:, qb, 64 * h_:64 * h_ + 64], in0=pbank[6][:, hl * 128:hl * 128 + 64], scalar=coef[:, 2, hl:hl + 1],
                                                                   in1=atmp[:], op0=ALU.mult, op1=ALU.add), r=["pb6", "coef", "atmp"], w=["aout"])
                S.barrier()
                if debug == "B2":
                    def doutt(name, shape, dt):
                        dbg_out[name] = nc.dram_tensor("dbg_" + name, list(shape), dt, kind="ExternalOutput").ap()
                        return dbg_out[name]
                    DMA(doutt(f"aout{jt}", [128, 2048], BF16), aout[:].rearrange("p a b -> p (a b)"), r=["aout"])
                    DMA(doutt(f"qTz{jt}", [128, 4096], BF16), qTz[:].rearrange("p a b c -> p (a b c)"), r=["qTz"])
                    DMA(doutt(f"kwT{jt}", [128, 1024], BF16), kwT[:], r=["kwT"])
                    DMA(doutt(f"kslocT{jt}", [128, 640], BF16), kslocT[:], r=["kslocT"])
                    DMA(doutt(f"gsig{jt}", [128, 96], F32), gsig[:].rearrange("p a b -> p (a b)"), r=["gsig"])
                    DMA(doutt(f"utm{jt}", [128, 2048], BF16), utm[:].rearrange("p a b -> p (a b)"), r=["utmB"])
                    S.barrier()

                if DBG.get("stopB2"):
                    continue
                with ExitStack() as s3:
                    sb3 = lambda name, shape, dt: sb(name, shape, dt, s3)
                    cosT = sb3("cosT", [128, 16, 128], F32)
                    sinT = sb3("sinT", [128, 16, 128], F32)
                    BbT = sb3("BbT", [128, 2, 16, 128], BF16)
                    Cp = sb3("Cp", [128, 2, 16, 32], BF16)
                    dsk = sb3("dsk", [128, 512], F32)
                    wglu = sb3("wglu", [128, 4, 1024], BF16)
                    rho = sb3("rho", [128, 16], F32)
                    z0 = sb3("z0", [128, 32], F32)
                    with ExitStack() as s3a:
                        sb3a = lambda name, shape, dt: sb(name, shape, dt, s3a)
                        ti_ = sb3a("ti_", [128, 1024], I32)
                        ta_ = sb3a("ta_", [128, 1024], F32)
                        tb_ = sb3a("tb_", [128, 1024], F32)
                        ang = sb3a("ang", [128, 1024], F32)
                        trw = sb3a("trw", [128, 128], F32)
                        Bexp = sb3a("Bexp", [128, 2, 16, 128], BF16)
                        cst = sb3a("cst", [128, 16, 16], F32)
                        ohs = sb3a("ohs", [128, NT], F32)
                        tmpX = sb3a("tmpX", [128, 32, NT], F32)
                        DMA(trw[:], trow, w=["trw"])
                        DMA(dsk[:], dskip_d, w=["dsk"])
                        DMA(ohs[:], onehot[:, jt, :], w=["ohs"])

                        def sincos3(out_ap, n, shift, key_out):
                            A_, B_, I_ = ta_[:, 0:n], tb_[:, 0:n], ti_[:, 0:n]
                            V(lambda e: e.tensor_scalar(out=A_, in0=ang[:, 0:n], scalar1=shift, scalar2=1.0 / (2 * PI), op0=ALU.add, op1=ALU.mult), r=["ang"], w=["ta_"])
                            V(lambda e: e.tensor_copy(out=I_, in_=A_), r=["ta_"], w=["ti_"])
                            V(lambda e: e.tensor_copy(out=B_, in_=I_), r=["ti_"], w=["tb_"])
                            V(lambda e: e.tensor_tensor(out=A_, in0=A_, in1=B_, op=ALU.subtract), r=["ta_", "tb_"], w=["ta_"])
                            V(lambda e: e.tensor_scalar(out=B_, in0=A_, scalar1=0.5, scalar2=None, op0=ALU.is_gt), r=["ta_"], w=["tb_"])
                            V(lambda e: e.tensor_tensor(out=A_, in0=A_, in1=B_, op=ALU.subtract), r=["ta_", "tb_"], w=["ta_"])
                            V(lambda e: e.tensor_scalar(out=B_, in0=A_, scalar1=-0.5, scalar2=None, op0=ALU.is_lt), r=["ta_"], w=["tb_"])
                            V(lambda e: e.tensor_tensor(out=A_, in0=A_, in1=B_, op=ALU.add), r=["ta_", "tb_"], w=["ta_"])
                            V(lambda e: e.activation(out=out_ap, in_=A_, func=AF.Sin, scale=2 * PI), r=["ta_"], w=[key_out], e="act")

                        for hh in range(2):
                            ks8 = slice(8 * hh, 8 * hh + 8)
                            V(lambda e: e.tensor_tensor(out=ang[:].rearrange("p (k t) -> p k t", k=8),
                                                        in0=thf[:, ks8].unsqueeze(2).to_broadcast([128, 8, 128]),
                                                        in1=trw[:].unsqueeze(1).to_broadcast([128, 8, 128]), op=ALU.mult), r=["thf", "trw"], w=["ang"])
                            sincos3(cosT[:, ks8, :].rearrange("p k t -> p (k t)"), 1024, PI / 2, "cosT")
                            sincos3(sinT[:, ks8, :].rearrange("p k t -> p (k t)"), 1024, 0.0, "sinT")
                        V(lambda e: e.activation(out=rho[:], in_=af[:], func=AF.Exp), r=["af"], w=["rho"], e="act")
                        V(lambda e: e.memset(Bexp[:], 0.0), w=["Bexp"], e="pool")
                        Bb5 = Bbar[:].rearrange("p r (a b) c -> p r a b c", b=4)
                        Be5 = Bexp[:].rearrange("p r (a b) x -> p r a b x", b=4)
                        for glo in range(2):
                            ps_ = slice(64 * glo, 64 * glo + 64)
                            for k4 in range(4):
                                V(lambda e: e.tensor_copy(out=Be5[ps_, :, :, k4, 32 * k4 + 16 * glo:32 * k4 + 16 * glo + 16], in_=Bb5[ps_, :, :, k4, :]),
                                  r=["Bbr", "Bbi", "Bexp"], w=["Bexp"])
                        for ri in range(2):
                            for k8 in range(2):
                                pbb = pbank[ri * 2 + k8][:].bitcast(BF16)
                                for kk in range(8):
                                    S.op("pe", lambda e: e.transpose(out=pbb[:, kk * 128:(kk + 1) * 128], in_=Bexp[:, ri, 8 * k8 + kk, :], identity=identb[:]),
                                         reads=["Bexp", "identb"], writes=[f"pb{ri * 2 + k8}"])
                                CP(BbT[:, ri, 8 * k8:8 * k8 + 8, :].rearrange("p k q -> p (k q)"), pbb, r=[f"pb{ri * 2 + k8}"], w=["BbT"])
                        V(lambda e: e.memset(Cp[:], 0.0), w=["Cp"], e="pool")
                        for ri, nm in ((0, "cre_f"), (1, "cim_f")):
                            DMA(cst[:], ssm_b[nm], w=["cst"])
                            for glo in range(2):
                                ps_ = slice(64 * glo, 64 * glo + 64)
                                V(lambda e: e.tensor_scalar(out=Cp[ps_, ri, :, 16 * glo:16 * glo + 16], in0=cst[ps_], scalar1=(1.0 if ri == 0 else -1.0),
                                                            scalar2=None, op0=ALU.mult), r=["cst", "Cp"], w=["Cp"])
                        for c4 in range(4):
                            st_ = wstB[c4 % 2]
                            stv = st_[:].rearrange("p a b -> p (a b)").rearrange("p (c n) -> p c n", c=4)
                            DMA(stv, wglu_d[:, 256 * c4:256 * c4 + 256].rearrange("(c p) n -> p c n", p=128), w=[f"wstB{c4 % 2}"])
                            CP(wglu[:, :, 256 * c4:256 * c4 + 256], stv, r=[f"wstB{c4 % 2}"], w=["wglu"])
                        V(lambda e: e.tensor_tensor(out=tmpX[:], in0=Xtile[:].rearrange("p t c -> p c t"),
                                                    in1=ohs[:].unsqueeze(1).to_broadcast([128, 32, NT]), op=ALU.mult), r=["Xtile", "ohs"], w=["tmpX"])
                        V(lambda e: e.tensor_reduce(out=z0[:], in_=tmpX[:], axis=AX.X, op=ALU.add), r=["tmpX"], w=["z0"])
                        S.barrier()
                    wr = sb3("wr", [128, 4, 128], F32)
                    wi = sb3("wi", [128, 4, 128], F32)
                    q1 = sb3("q1", [128, 4, 128], F32)
                    q2 = sb3("q2", [128, 4, 128], F32)
                    zr = sb3("zr", [128, 4, 128], F32)
                    zi = sb3("zi", [128, 4, 128], F32)
                    xr = sb3("xr", [128, 4, 128], F32)
                    xi = sb3("xi", [128, 4, 128], F32)
                    xrb = sb3("xrb", [128, 4, 128], BF16)
                    xib = sb3("xib", [128, 4, 128], BF16)
                    ypre = sb3("ypre", [128, 512], F32)
                    ygb = sb3("ygb", [128, 512], BF16)
                    ygT = sb3("ygT", [128, 4, 128], BF16)
                    sg = sb3("sg", [128, 512], F32)
                    fl4 = lambda t: t[:].rearrange("p k t -> p (k t)")
                    for qb in range(4):
                        tsl = slice(128 * qb, 128 * qb + 128)
                        for qq in range(4):
                            k4s = slice(4 * qq, 4 * qq + 4)
                            cq = cosT[:, k4s, :].rearrange("p k t -> p (k t)")
                            sq = sinT[:, k4s, :].rearrange("p k t -> p (k t)")
                            for ri in range(2):
                                for kk in range(4):
                                    MM(pbank[ri][:, kk * 128:(kk + 1) * 128], BbT[:, ri, 4 * qq + kk, :], uT[:, qq, tsl], True, True, r=["BbT", "uT"], w=[f"pb{ri}"])
                            V(lambda e: e.tensor_tensor(out=fl4(wr), in0=pbank[0][:], in1=cq, op=ALU.mult), r=["pb0", "cosT"], w=["wr"])
                            V(lambda e: e.tensor_tensor(out=fl4(q1), in0=pbank[1][:], in1=sq, op=ALU.mult), r=["pb1", "sinT"], w=["q1"])
                            V(lambda e: e.tensor_tensor(out=fl4(wi), in0=pbank[1][:], in1=cq, op=ALU.mult), r=["pb1", "cosT"], w=["wi"])
                            V(lambda e: e.tensor_tensor(out=fl4(q2), in0=pbank[0][:], in1=sq, op=ALU.mult), r=["pb0", "sinT"], w=["q2"])
                            V(lambda e: e.tensor_tensor(out=fl4(wr), in0=fl4(wr), in1=fl4(q1), op=ALU.add), r=["wr", "q1"], w=["wr"], e="pool")
                            V(lambda e: e.tensor_tensor(out=fl4(wi), in0=fl4(wi), in1=fl4(q2), op=ALU.subtract), r=["wi", "q2"], w=["wi"], e="pool")
                            for kk in range(4):
                                k = 4 * qq + kk
                                V(lambda e: e.tensor_tensor_scan(out=zr[:, kk, :], data0=rho[:, k:k + 1].to_broadcast([128, 128]), data1=wr[:, kk, :],
                                                                 initial=z0[:, k:k + 1], op0=ALU.mult, op1=ALU.add), r=["rho", "wr", "z0"], w=["zr"])
                                V(lambda e: e.tensor_tensor_scan(out=zi[:, kk, :], data0=rho[:, k:k + 1].to_broadcast([128, 128]), data1=wi[:, kk, :],
                                                                 initial=z0[:, 16 + k:17 + k], op0=ALU.mult, op1=ALU.add), r=["rho", "wi", "z0"], w=["zi"])
                            V(lambda e: e.tensor_tensor(out=fl4(xr), in0=fl4(zr), in1=cq, op=ALU.mult), r=["zr", "cosT"], w=["xr"])
                            V(lambda e: e.tensor_tensor(out=fl4(q1), in0=fl4(zi), in1=sq, op=ALU.mult), r=["zi", "sinT"], w=["q1"], e="pool")
                            V(lambda e: e.tensor_tensor(out=fl4(xr), in0=fl4(xr), in1=fl4(q1), op=ALU.subtract), r=["xr", "q1"], w=["xr"])
                            V(lambda e: e.tensor_tensor(out=fl4(xi), in0=fl4(zr), in1=sq, op=ALU.mult), r=["zr", "sinT"], w=["xi"], e="pool")
                            V(lambda e: e.tensor_tensor(out=fl4(q2), in0=fl4(zi), in1=cq, op=ALU.mult), r=["zi", "cosT"], w=["q2"])
                            V(lambda e: e.tensor_tensor(out=fl4(xi), in0=fl4(xi), in1=fl4(q2), op=ALU.add), r=["xi", "q2"], w=["xi"], e="pool")
                            CP(fl4(xrb), fl4(xr), r=["xr"], w=["xrb"], e="act")
                            CP(fl4(xib), fl4(xi), r=["xi"], w=["xib"], e="act")
                            V(lambda e: e.tensor_copy(out=z0[:, k4s], in_=xr[:, :, 127]), r=["xr", "z0"], w=["z0"])
                            V(lambda e: e.tensor_copy(out=z0[:, 16 + 4 * qq:20 + 4 * qq], in_=xi[:, :, 127]), r=["xi", "z0"], w=["z0"])
                            for kk in range(4):
                                k = 4 * qq + kk
                                MM(pbank[4][:, 32 * k:32 * k + 32], xrb[:, kk, :], Cp[:, 0, k, :], qq == 0 and kk == 0, False, r=["xrb", "Cp"], w=["pb4"])
                                MM(pbank[4][:, 32 * k:32 * k + 32], xib[:, kk, :], Cp[:, 1, k, :], False, True, r=["xib", "Cp"], w=["pb4"])
                        V(lambda e: e.tensor_tensor(out=sg[:], in0=utm[:, qb, :], in1=dsk[:], op=ALU.mult), r=["utmB", "dsk"], w=["sg"], e="pool")
                        V(lambda e: e.tensor_tensor(out=ypre[:], in0=pbank[4][:], in1=sg[:], op=ALU.add), r=["pb4", "sg"], w=["ypre"])
                        V(lambda e: e.activation(out=ygb[:], in_=ypre[:], func=AF.Gelu_apprx_tanh), r=["ypre"], w=["ygb"], e="act")
                        pbb5 = pbank[5][:].bitcast(BF16)
                        for cc in range(4):
                            S.op("pe", lambda e: e.transpose(out=pbb5[:, cc * 128:(cc + 1) * 128], in_=ygb[:, cc * 128:(cc + 1) * 128], identity=identb[:]),
                                 reads=["ygb", "identb"], writes=["pb5"])
                        CP(ygT[:].rearrange("p c t -> p (c t)"), pbb5[:, 0:512], r=["pb5"], w=["ygT"])
                        for half in range(2):
                            for cc in range(4):
                                MM(pbank[6 + half][:], ygT[:, cc, :], wglu[:, cc, half * 512:(half + 1) * 512], cc == 0, cc == 3, r=["ygT", "wglu"], w=[f"pb{6 + half}"])
                        V(lambda e: e.activation(out=sg[:], in_=pbank[7][:], func=AF.Sigmoid), r=["pb7"], w=["sg"], e="act")
                        V(lambda e: e.tensor_tensor(out=sout[:, qb, :], in0=pbank[6][:], in1=sg[:], op=ALU.mult), r=["pb6", "sg"], w=["sout"])
                S.barrier()
                with ExitStack() as s4:
                    sb4 = lambda name, shape, dt: sb(name, shape, dt, s4)
                    wout = sb4("wout", [128, 8, 1024], BF16)
                    mixT = sb4("mixT", [128, 8, 128], BF16)
                    xrow = [sb4(f"xrow{i}", [128, D], F32) for i in range(2)]
                    h1t = [sb4(f"h1t{i}", [128, D], F32) for i in range(2)]
                    for c8 in range(8):
                        st_ = wstB[c8 % 2]
                        DMA(st_[:], wout_d[:, 128 * c8:128 * c8 + 128].rearrange("(c p) n -> p c n", p=128), w=[f"wstB{c8 % 2}"])
                        CP(wout[:, :, 128 * c8:128 * c8 + 128], st_[:], r=[f"wstB{c8 % 2}"], w=["wout"], e=("act" if c8 % 2 else "dve"))
                    for qb in range(4):
                        i2 = qb % 2
                        DMA(xrow[i2][:], x_loc[jt, 512 + 128 * qb:512 + 128 * qb + 128, :], w=[f"xrow{i2}"])
                        pbb5 = pbank[5][:].bitcast(BF16)
                        for cc in range(8):
                            src = aout if cc < 4 else sout
                            kx = "aout" if cc < 4 else "sout"
                            S.op("pe", lambda e: e.transpose(out=pbb5[:, cc * 128:(cc + 1) * 128], in_=src[:, qb, (cc % 4) * 128:(cc % 4) * 128 + 128], identity=identb[:]),
                                 reads=[kx, "identb"], writes=["pb5"])
                        CP(mixT[:].rearrange("p c t -> p (c t)"), pbb5, r=["pb5"], w=["mixT"])
                        for half in range(2):
                            for cc in range(8):
                                MM(pbank[6 + half][:], mixT[:, cc, :], wout[:, cc, half * 512:(half + 1) * 512], cc == 0, cc == 7, r=["mixT", "wout"], w=[f"pb{6 + half}"])
                            V(lambda e: e.tensor_tensor(out=h1t[i2][:, half * 512:(half + 1) * 512], in0=pbank[6 + half][:], in1=xrow[i2][:, half * 512:(half + 1) * 512], op=ALU.add),
                              r=[f"pb{6 + half}", f"xrow{i2}"], w=[f"h1t{i2}"])
                        DMA(h1d[4 * jt + qb], h1t[i2][:], r=[f"h1t{i2}"], w=["h1d"])
                    if debug == "B4":
                        def doutt(name, shape, dt):
                            dbg_out[name] = nc.dram_tensor("dbg_" + name, list(shape), dt, kind="ExternalOutput").ap()
                            return dbg_out[name]
                        DMA(doutt(f"aout{jt}", [128, 2048], BF16), aout[:].rearrange("p a b -> p (a b)"), r=["aout"])
                        DMA(doutt(f"sout{jt}", [128, 2048], BF16), sout[:].rearrange("p a b -> p (a b)"), r=["sout"])
                        DMA(doutt(f"gsig{jt}", [128, 96], F32), gsig[:].rearrange("p a b -> p (a b)"), r=["gsig"])
                        DMA(doutt(f"utm{jt}", [128, 2048], BF16), utm[:].rearrange("p a b -> p (a b)"), r=["utmB"])
                        dh = doutt(f"h1{jt}", [4, 128, D], F32)
                        for qb in range(4):
                            DMA(xrow[0][:], h1d[4 * jt + qb], r=["h1d"], w=["xrow0"])
                            DMA(dh[qb], xrow[0][:], r=["xrow0"])
                S.barrier()

            while bg:
                bg_step(1)
        S.barrier()
        sctx.close()
        if not DBG.get("noC"):
          with ExitStack() as sC:
            sbc = lambda name, shape, dt: sb(name, shape, dt, sC)
            wq = sbc("wq", [128, 8, 2048], BF16)
            subT = sbc("subT", [128, 2, 128], BF16)
            gffn = sbc("gffn", [128, D], F32)
            gfin = sbc("gfin", [128, D], F32)
            wstC = [sbc(f"wstC{i}", [128, 8, 128], F32) for i in range(2)]
            DMA(gffn[:], gffn_d.partition_broadcast(128), w=["gffn"])
            DMA(gfin[:], fnorm.partition_broadcast(128), w=["gfin"])
            for c16 in range(16):
                st_ = wstC[c16 % 2]
                DMA(st_[:], wq_d[:, 128 * c16:128 * c16 + 128].rearrange("(c p) n -> p c n", p=128), w=[f"wstC{c16 % 2}"])
                CP(wq[:, :, 128 * c16:128 * c16 + 128], st_[:], r=[f"wstC{c16 % 2}"], w=["wq"], e=("act" if c16 % 2 else "dve"))
            for i_, d_ in enumerate((sub1T_d, sub2T_d)):
                DMA(wstC[0][:, 0, :], d_, w=["wstC0"])
                CP(subT[:, i_, :], wstC[0][:, 0, :], r=["wstC0"], w=["subT"])
            hb = [sbc(f"hb{i}", [128, D], F32) for i in range(2)]
            xn2 = [sbc(f"xn{i}", [128, D], F32) for i in range(2)]
            xnb = sbc("xnb", [128, D], BF16)
            xnT = sbc("xnT", [128, 8, 128], BF16)
            qTb = sbc("qTb", [128, 16, 128], BF16)
            sc = sbc("sc", [128, 16, 128], F32)
            scr = sbc("scr", [128, 128], F32)
            vtop = sbc("vtop", [128, 16, 16], F32)
            itop = sbc("itop", [128, 16, 16], U32)
            itf = sbc("itf", [128, 16, 16], F32)
            cand = sbc("cand", [128, 256], F32)
            cand2 = sbc("cand2", [128, 256], F32)
            cidx = sbc("cidx", [128, 256], F32)
            cjk = sbc("cjk", [128, 256], F32)
            top = sbc("top", [128, 8, 16], F32)
            eidf = sbc("eidf", [128, 128], F32)
            eidx2 = [sbc(f"eidx{i}", [128, 128], I32) for i in range(2)]
            gate2 = [sbc(f"gate{i}", [128, 8, 16], F32) for i in range(2)]
            ntop = sbc("ntop", [128, 8], F32)
            zs = sbc("zs", [128, 8], F32)
            hid = sbc("hid", [128, 128], F32)
            actw = sbc("actw", [128, 128], F32)
            ssc = sbc("ssc", [128, 2], F32)
            rsc = sbc("rsc", [128, 2], F32)
            gbuf = [sbc(f"gbuf{i}", [128, 2 * D], BF16) for i in range(NGB)]
            gjk = sbc("gjk", [128, D], F32)
            acc = sbc("acc", [128, D], F32)
            scb = [sbc(f"scb{i}", [128, D], BF16) for i in range(4)]
            otile = sbc("otile", [128, D], F32)
            gctr = [0]
            NQ = DBG.get("nqC", 16)

            def frontend(qi):
                p = qi % 2
                h_, hk_ = hb[p], f"hb{p}"
                xn, xk = xn2[p], f"xn{p}"
                eidx, ek_ = eidx2[p], f"eidx{p}"
                gate, gk = gate2[p], f"gate{p}"
                DMA(h_[:], h1d[qi], r=["h1d"], w=[hk_])
                V(lambda e: e.activation(out=xn[:], in_=h_[:], func=AF.Square, accum_out=ssc[:, 0:1]), r=[hk_], w=[xk, "ssc0"], e="act")
                V(lambda e: e.activation(out=rsc[:, 0:1], in_=ssc[:, 0:1], func=AF.Sqrt, bias=EPS, scale=1.0 / D), r=["ssc0"], w=["rsc0"], e="act")
                V(lambda e: e.reciprocal(out=rsc[:, 0:1], in_=rsc[:, 0:1]), r=["rsc0"], w=["rsc0"])
                yield
                V(lambda e: e.scalar_tensor_tensor(out=xn[:], in0=h_[:], scalar=rsc[:, 0:1], in1=gffn[:], op0=ALU.mult, op1=ALU.mult),
                  r=[hk_, "rsc0", "gffn"], w=[xk])
                CP(xnb[:], xn[:], r=[xk], w=["xnb"], e="act")
                yield
                pbb = pbank[0][:].bitcast(BF16)
                for dc in range(8):
                    S.op("pe", lambda e: e.transpose(out=pbb[:, dc * 128:(dc + 1) * 128], in_=xnb[:, dc * 128:(dc + 1) * 128], identity=identb[:]),
                         reads=["xnb", "identb"], writes=["pb0"])
                CP(xnT[:].rearrange("p c t -> p (c t)"), pbb, r=["pb0"], w=["xnT"])
                yield
                for b4 in range(4):
                    bank = 1 + b4 % 2
                    for bb in range(4):
                        blk = 4 * b4 + bb
                        for dc in range(8):
                            MM(pbank[bank][:, bb * 128:(bb + 1) * 128], wq[:, dc, blk * 128:(blk + 1) * 128], xnT[:, dc, :], dc == 0, dc == 7,
                               r=["wq", "xnT"], w=[f"pb{bank}"])
                        yield
                    CP(qTb[:, 4 * b4:4 * b4 + 4, :].rearrange("p a b -> p (a b)"), pbank[bank][:], r=[f"pb{bank}"], w=["qTb"], e=("act" if b4 % 2 else "dve"))
                    yield
                for b4 in range(4):
                    bank = 3 + b4 % 2
                    for bb in range(4):
                        blk = 4 * b4 + bb
                        MM(pbank[bank][:, bb * 128:(bb + 1) * 128], qTb[:, blk, :], subT[:, blk % 2, :], True, True, r=["qTb", "subT"], w=[f"pb{bank}"])
                    CP(sc[:, 4 * b4:4 * b4 + 4, :].rearrange("p a b -> p (a b)"), pbank[bank][:], r=[f"pb{bank}"], w=["sc"], e=("act" if b4 % 2 else "dve"))
                    yield
                for blk in range(16):
                    V(lambda e: e.max(out=vtop[:, blk, 0:8], in_=sc[:, blk, :]), r=["sc"], w=["vtop"])
                    V(lambda e: e.max_index(out=itop[:, blk, 0:8], in_max=vtop[:, blk, 0:8], in_values=sc[:, blk, :]), r=["sc", "vtop"], w=["itop"])
                    yield
                    V(lambda e: e.match_replace(out=scr[:], in_to_replace=vtop[:, blk, 0:8], in_values=sc[:, blk, :], imm_value=-1e30), r=["sc", "vtop"], w=["scr"])
                    V(lambda e: e.max(out=vtop[:, blk, 8:16], in_=scr[:]), r=["scr"], w=["vtop"])
                    yield
                    V(lambda e: e.max_index(out=itop[:, blk, 8:16], in_max=vtop[:, blk, 8:16], in_values=scr[:]), r=["scr", "vtop"], w=["itop"])
                    yield
                V(lambda e: e.tensor_copy(out=itf[:], in_=itop[:]), r=["itop"], w=["itf"])
                yield
                for hh in range(8):
                    c3 = cand[:].rearrange("p (a b) -> p a b", a=16)
                    i3 = cidx[:].rearrange("p (a b) -> p a b", a=16)
                    V(lambda e: e.tensor_tensor(out=c3, in0=vtop[:, 2 * hh, :].unsqueeze(2).to_broadcast([128, 16, 16]),
                                                in1=vtop[:, 2 * hh + 1, :].unsqueeze(1).to_broadcast([128, 16, 16]), op=ALU.add), r=["vtop"], w=["cand"])
                    V(lambda e: e.scalar_tensor_tensor(out=i3, in0=itf[:, 2 * hh, :].unsqueeze(2).to_broadcast([128, 16, 16]), scalar=128.0,
                                                       in1=itf[:, 2 * hh + 1, :].unsqueeze(1).to_broadcast([128, 16, 16]), op0=ALU.mult, op1=ALU.add),
                      r=["itf"], w=["cidx"])
                    yield
                    V(lambda e: e.max(out=top[:, hh, 0:8], in_=cand[:]), r=["cand"], w=["top"])
                    V(lambda e: e.match_replace(out=cand2[:], in_to_replace=top[:, hh, 0:8], in_values=cand[:], imm_value=-1e30), r=["cand", "top"], w=["cand2"])
                    V(lambda e: e.max(out=top[:, hh, 8:16], in_=cand2[:]), r=["cand2"], w=["top"])
                    yield
                    for kk in range(16):
                        V(lambda e: e.scalar_tensor_tensor(out=cjk[:], in0=cand[:], scalar=top[:, hh, kk:kk + 1], in1=cidx[:], op0=ALU.is_equal, op1=ALU.mult,
                                                           accum_out=eidf[:, 16 * hh + kk:16 * hh + kk + 1]), r=["cand", "cidx", "top"], w=[f"eidf{16 * hh + kk}"])
                        if kk % 2:
                            yield
                    V(lambda e: e.tensor_scalar(out=ntop[:, hh:hh + 1], in0=top[:, hh, 0:1], scalar1=-1.0, scalar2=None, op0=ALU.mult), r=["top"], w=["ntop"])
                    V(lambda e: e.activation(out=gate[:, hh, :], in_=top[:, hh, :], func=AF.Exp, bias=ntop[:, hh:hh + 1], scale=1.0, accum_out=zs[:, hh:hh + 1]),
                      r=["top", "ntop"], w=[gk, "zs"], e="act")
                    yield
                V(lambda e: e.reciprocal(out=zs[:], in_=zs[:]), r=["zs"], w=["zs"])
                V(lambda e: e.tensor_tensor(out=gate[:], in0=gate[:], in1=zs[:].unsqueeze(2).to_broadcast([128, 8, 16]), op=ALU.mult), r=[gk, "zs"], w=[gk])
                V(lambda e: e.tensor_scalar(out=eidf[:], in0=eidf[:], scalar1=16383.0, scalar2=0.0, op0=ALU.min, op1=ALU.max), r=[f"eidf{i}" for i in range(128)], w=["eidf"])
                V(lambda e: e.tensor_copy(out=eidx[:], in_=eidf[:]), r=["eidf"], w=[ek_])
                yield

            def drain(gen, n=None):
                if gen is None:
                    return
                k = 0
                for _ in gen:
                    k += 1
                    if n is not None and k >= n:
                        return

            drain(frontend(0))
            for qi in range(NQ):
                p = qi % 2
                h_, hk_ = hb[p], f"hb{p}"
                xn, xk = xn2[p], f"xn{p}"
                eidx, ek_ = eidx2[p], f"eidx{p}"
                gate, gk = gate2[p], f"gate{p}"
                fe_next = frontend(qi + 1) if qi + 1 < NQ else None
                gflat = gate[:].rearrange("p a b -> p (a b)")
                for grp in range(16):
                    gl = []
                    for hk in range(8 * grp, 8 * grp + 8):
                        gi = gctr[0] % NGB
                        gctr[0] += 1
                        gb = gbuf[gi]
                        gl.append((hk, gi, gb))
                        S.dma("pool", lambda q: q.indirect_dma_start(out=gb[:], out_offset=None, in_=uvb,
                                                                     in_offset=bass.IndirectOffsetOnAxis(ap=eidx[:, hk:hk + 1], axis=0)), reads=[ek_], writes=[f"gbuf{gi}"])
                        V(lambda e: e.scalar_tensor_tensor(out=gjk[:], in0=gb[:, 0:D], scalar=1.0, in1=xn[:], op0=ALU.mult, op1=ALU.mult, accum_out=hid[:, hk:hk + 1]),
                          r=[f"gbuf{gi}", xk], w=[f"hid{hk}"])
                        drain(fe_next, 2)
                    gs = slice(8 * grp, 8 * grp + 8)
                    V(lambda e: e.activation(out=actw[:, gs], in_=hid[:, gs], func=AF.Gelu_apprx_tanh), r=[f"hid{i}" for i in range(8 * grp, 8 * grp + 8)], w=[f"actw{grp}"], e="act")
                    V(lambda e: e.tensor_tensor(out=actw[:, gs], in0=actw[:, gs], in1=gflat[:, gs], op=ALU.mult), r=[f"actw{grp}", gk], w=[f"actw{grp}"])
                    for (hk, gi, gb) in gl:
                        si = hk % 4
                        V(lambda e: e.activation(out=scb[si][:], in_=gb[:, D:2 * D], func=AF.Copy, scale=actw[:, hk:hk + 1]), r=[f"gbuf{gi}", f"actw{grp}"], w=[f"scb{si}"], e="act")
                        for half in range(2):
                            MM(pbank[6 + half][:], identb[:], scb[si][:, half * 512:(half + 1) * 512], hk == 0, hk == 127, r=[f"scb{si}", "identb"], w=[f"pb{6 + half}"])
                        drain(fe_next, 1)
                drain(fe_next)
                for half in range(2):
                    V(lambda e: e.tensor_tensor(out=acc[:, half * 512:(half + 1) * 512], in0=pbank[6 + half][:], in1=h_[:, half * 512:(half + 1) * 512], op=ALU.add),
                      r=[f"pb{6 + half}", hk_], w=["acc"])
                V(lambda e: e.activation(out=otile[:], in_=acc[:], func=AF.Square, accum_out=ssc[:, 1:2]), r=["acc"], w=["otile", "ssc1"], e="act")
                V(lambda e: e.activation(out=rsc[:, 1:2], in_=ssc[:, 1:2], func=AF.Sqrt, bias=EPS, scale=1.0 / D), r=["ssc1"], w=["rsc1"], e="act")
                V(lambda e: e.reciprocal(out=rsc[:, 1:2], in_=rsc[:, 1:2]), r=["rsc1"], w=["rsc1"])
                V(lambda e: e.scalar_tensor_tensor(out=otile[:], in0=acc[:], scalar=rsc[:, 1:2], in1=gfin[:], op0=ALU.mult, op1=ALU.mult),
                  r=["acc", "rsc1", "gfin"], w=["otile"])
                DMA(y[qi // 4, 128 * (qi % 4):128 * (qi % 4) + 128, :], otile[:], r=["otile"])

        S.finish()
    return nc


def kernel(**inputs):
    com, per = _prep(inputs)
    nc = build_nc()
    in_maps = [dict(com, **per[c]) for c in range(NCORES)]
    res = run_bass_kernel_spmd(nc, in_maps, core_ids=list(range(NCORES)))
    out = np.zeros((NT, 512, D), np.float32)
    for c in range(NCORES):
        for j in range(NOWN):
            out[8 * j + c] = res.results[c]["y"][j]
    return out.reshape(1, SEQ, D)
```

```python
import math
import numpy as np
import concourse.bass as bass
import concourse.mybir as mybir
from concourse.bass_utils import run_bass_kernel_spmd
from contextlib import ExitStack

F32 = mybir.dt.float32
BF16 = mybir.dt.bfloat16
I32 = mybir.dt.int32
U32 = mybir.dt.uint32
AF = mybir.ActivationFunctionType
ALU = mybir.AluOpType
AX = mybir.AxisListType

NCORES = 8
SEQ = 16384
D = 1024
NT = 32
NOWN = 4
EPS = 1e-6
NGB = 16
PI = math.pi


class Sync:
    def __init__(self, nc, es):
        self.nc = nc
        self.engs = {"pe": nc.tensor, "act": nc.scalar, "dve": nc.vector,
                     "pool": nc.gpsimd, "sp": nc.sync}
        self.sem = {k: es.enter_context(nc.semaphore("sem_" + k)) for k in self.engs}
        self.cnt = {k: 0 for k in self.engs}
        self.dsem = [es.enter_context(nc.semaphore(f"dsem{i}")) for i in range(32)]
        self.dcnt = [0] * len(self.dsem)
        self.qsl = {"sp": list(range(0, 24)), "act": list(range(0, 24)), "pool": list(range(24, 32))}
        self.drr = {"sp": 0, "act": 0, "pool": 0}
        self.waited = {k: {} for k in self.engs}
        self.lastw = {}
        self.reads = {}

    def _wait(self, e, ev):
        if ev is None:
            return
        sem, val, src = ev
        if src == e and e == "pe":
            return
        key = id(sem)
        if self.waited[e].get(key, 0) >= val:
            return
        self.engs[e].wait_ge(sem, val)
        self.waited[e][key] = val

    def deps(self, e, reads, writes):
        for b in reads:
            self._wait(e, self.lastw.get(b))
        for b in writes:
            self._wait(e, self.lastw.get(b))
            for ev in self.reads.get(b, []):
                self._wait(e, ev)

    def commit(self, ev, reads, writes):
        for b in reads:
            self.reads.setdefault(b, []).append(ev)
        for b in writes:
            self.lastw[b] = ev
            self.reads[b] = []

    def op(self, e, inst_fn, reads=(), writes=()):
        self.deps(e, reads, writes)
        inst = inst_fn(self.engs[e])
        self.cnt[e] += 1
        inst.then_inc(self.sem[e], 1)
        ev = (self.sem[e], self.cnt[e], e)
        self.commit(ev, reads, writes)
        return ev

    def dma(self, e, inst_fn, reads=(), writes=()):
        self.deps(e, reads, writes)
        sl = self.qsl[e]
        k = sl[self.drr[e] % len(sl)]
        self.drr[e] += 1
        inst = inst_fn(self.engs[e])
        self.dcnt[k] += 16
        inst.then_inc(self.dsem[k], 16)
        ev = (self.dsem[k], self.dcnt[k], "dma")
        self.commit(ev, reads, writes)
        return ev

    def barrier(self):
        for e in self.engs:
            for e2 in self.engs:
                if e2 != e and self.cnt[e2]:
                    self._wait(e, (self.sem[e2], self.cnt[e2], e2))
            for k in range(len(self.dsem)):
                if self.dcnt[k]:
                    self._wait(e, (self.dsem[k], self.dcnt[k], "dma"))
        self.lastw = {}
        self.reads = {}

    def finish(self):
        for k in range(len(self.dsem)):
            if self.dcnt[k]:
                self._wait("sp", (self.dsem[k], self.dcnt[k], "dma"))
        for e2 in self.engs:
            if e2 != "sp" and self.cnt[e2]:
                self._wait("sp", (self.sem[e2], self.cnt[e2], e2))


HD = 64
ROPE_THETA = 500000.0


def _rope_tables_T(pos):
    pos = np.asarray(pos, np.float32)
    inv = (ROPE_THETA ** (-np.arange(8, dtype=np.float32) * 2.0 / 16.0)).astype(np.float32)
    ang = pos[None, :] * inv[:, None]
    c, s = np.cos(ang).astype(np.float32), np.sin(ang).astype(np.float32)
    C = np.ones((64, len(pos)), np.float32)
    S = np.zeros((64, len(pos)), np.float32)
    C[0:8] = c
    C[8:16] = c
    S[0:8] = -s
    S[8:16] = s
    return np.concatenate([C, C], 0), np.concatenate([S, S], 0)


def _swap_cols(cols):
    cols = np.asarray(cols).reshape(-1, 64).copy()
    out = cols.copy()
    out[:, 0:8] = cols[:, 8:16]
    out[:, 8:16] = cols[:, 0:8]
    return out.reshape(-1)


Q0, KC0, VC0, KS0, VS0, KW0, VW0, GT0, U0 = 0, 512, 640, 768, 896, 1024, 1152, 1280, 1304
NWA = 1152
NWB = 2944


def _prep(inputs):
    f = lambda k: np.ascontiguousarray(np.asarray(inputs[k], dtype=np.float32))
    x = f("x").reshape(SEQ, D)
    w_in = f("w_in")[0]
    ar = np.arange
    ks_c, kc_c, vc_c, vs_c = KS0 + ar(128), KC0 + ar(128), VC0 + ar(128), VS0 + ar(128)
    kw_c, vw_c, u_c = KW0 + ar(128), VW0 + ar(128), U0 + ar(512)
    colsA = np.concatenate([ks_c, _swap_cols(ks_c), kc_c, vc_c, vs_c, u_c])
    q_c = np.concatenate([np.concatenate([Q0 + 64 * hl + ar(64), Q0 + 64 * (4 + hl) + ar(64)])
                          for hl in range(4)])
    g_c = np.concatenate([GT0 + ar(24), np.full(104, GT0)])
    colsB = np.concatenate([q_c, _swap_cols(q_c), ks_c, _swap_cols(ks_c), kw_c, _swap_cols(kw_c),
                            u_c, vs_c, vw_c, g_c, u_c])
    assert len(colsA) == NWA and len(colsB) == NWB
    com = {}
    com["x_all"] = x.reshape(NT, 512, D)
    com["wA"] = np.ascontiguousarray(w_in[:, colsA])
    com["wB"] = np.ascontiguousarray(w_in[:, colsB])
    com["gA"] = np.ascontiguousarray(f("attn_norm")[0].reshape(8, 128).T)
    com["ident"] = np.eye(128, dtype=np.float32)
    C, S = _rope_tables_T(np.arange(SEQ))
    com["ropeC"] = np.ascontiguousarray(C.reshape(128, NT, 512).transpose(1, 0, 2))
    com["ropeS"] = np.ascontiguousarray(S.reshape(128, NT, 512).transpose(1, 0, 2))
    cmp_end = np.arange(1024) * 16 + 31
    Cc, Sc = _rope_tables_T(cmp_end)
    com["cmpC"], com["cmpS"] = Cc, Sc
    for kv in ("k", "v"):
        w1 = f(f"cmp_{kv}_w1")[0].reshape(32, 64, 128).transpose(1, 0, 2)
        com[f"w1{kv}"] = np.ascontiguousarray(np.concatenate([w1, w1], 0))
        com[f"pe{kv}T"] = np.ascontiguousarray(np.concatenate([f(f"cmp_{kv}_pe")[0].T] * 2, 0))
        w2 = f(f"cmp_{kv}_w2")[0]
        z = np.zeros_like(w2)
        com[f"w2{kv}"] = np.ascontiguousarray(np.stack(
            [np.concatenate([w2, z], 1), np.concatenate([z, w2], 1)], 1))
        if kv == "k":
            w2s = w2[:, _swap_cols(np.arange(64))]
            com["w2ksw"] = np.ascontiguousarray(np.stack(
                [np.concatenate([w2s, z], 1), np.concatenate([z, w2s], 1)], 1))
    def fm(a):
        a = a.reshape((16, 2) + a.shape[1:])
        a = np.moveaxis(a, 0, 2)
        return np.ascontiguousarray(a.reshape((128, 16) + a.shape[3:]))
    lam_re, lam_im = f("ssm_lam_re")[0], f("ssm_lam_im")[0]
    log_dt = f("ssm_log_dt")[0]
    com["lamre_f"], com["lamim_f"] = fm(lam_re), fm(lam_im)
    com["logdt_f"] = fm(np.repeat(log_dt[:, None], 64, 1))
    com["bre_f"], com["bim_f"] = fm(f("ssm_b_re")[0]), fm(f("ssm_b_im")[0])
    com["cre_f"] = fm(f("ssm_c_re")[0].transpose(0, 2, 1))
    com["cim_f"] = fm(f("ssm_c_im")[0].transpose(0, 2, 1))
    def rowb(a):
        return np.ascontiguousarray(np.broadcast_to(fm(a).transpose(1, 0)[None], (128, 16, 128)))
    com["lamre_r"], com["lamim_r"] = rowb(lam_re), rowb(lam_im)
    com["logdt_r"] = rowb(np.repeat(log_dt[:, None], 64, 1))
    com["tcol"] = np.ascontiguousarray(np.broadcast_to((127 - np.arange(128, dtype=np.float32))[:, None], (128, 1)))
    com["trow"] = np.ascontiguousarray(np.broadcast_to(np.arange(1, 129, dtype=np.float32)[None], (128, 128)))
    com["final_norm"] = f("final_norm").reshape(1, D)
    import ml_dtypes
    bf = lambda a: np.ascontiguousarray(np.asarray(a, np.float32).astype(ml_dtypes.bfloat16))
    cc = np.arange(8192)
    com["Fbase"] = bf((cc[None, :] // 64) == np.arange(128)[:, None])
    n_all = np.arange(1024)
    m_all = np.arange(256)
    ov = ((16 * n_all[:, None] < 64 * m_all[None, :] + 64) & (16 * n_all[:, None] + 32 > 64 * m_all[None, :]))
    ov[1023] = False
    com["OV"] = bf(ov.reshape(8, 128, 256).transpose(1, 0, 2))
    kk_, tt_ = np.arange(128)[:, None], np.arange(128)[None, :]
    com["mWM0"] = bf(kk_ > tt_)
    com["mTRI"] = bf(kk_ <= tt_)
    com["mLMA"] = bf((kk_ >= 64) & (tt_ < 64))
    com["blk64"] = np.ascontiguousarray(np.broadcast_to((64.0 * np.arange(256, dtype=np.float32))[None], (128, 256)))
    com["cmpend"] = np.ascontiguousarray((16.0 * n_all + 31.0).astype(np.float32).reshape(8, 128).T)
    com["dskip"] = np.ascontiguousarray(np.broadcast_to(f("ssm_d")[0][None], (128, 512)))
    com["wglu"] = f("ssm_w_glu")[0]
    com["wout"] = f("w_out")[0]
    com["ffn_norm"] = f("ffn_norm")[0].reshape(1, D)
    com["wq"] = f("peer_w_q")[0]
    com["sub1T"] = np.ascontiguousarray(f("peer_subkeys_1")[0].T)
    com["sub2T"] = np.ascontiguousarray(f("peer_subkeys_2")[0].T)
    com["peer_u"] = f("peer_u")[0]
    com["peer_v"] = f("peer_v")[0]
    per = []
    for c in range(NCORES):
        d = {}
        tiles = [8 * j + c for j in range(NOWN)]
        xl = np.zeros((NOWN, 1024, D), np.float32)
        for j, T in enumerate(tiles):
            lo = 512 * T - 512
            if lo >= 0:
                xl[j] = x[lo:lo + 1024]
            else:
                xl[j, 512:] = x[0:512]
        d["x_loc"] = xl
        oh = np.zeros((128, NOWN, NT), np.float32)
        for j, T in enumerate(tiles):
            oh[:, j, T] = 1.0
        d["onehot"] = oh
        rCl = np.zeros((NOWN, 2, 128, 512), np.float32)
        rSl = np.zeros((NOWN, 2, 128, 512), np.float32)
        tpB = np.zeros((NOWN, 128, 512), np.float32)
        tpt = np.zeros((128, 16), np.float32)
        vW = np.zeros((128, 16, 5), np.float32)
        vA = np.zeros((128, 16), np.float32)
        for j, T in enumerate(tiles):
            pos = 512 * T - 512 + np.arange(1024)
            Cl, Sl = _rope_tables_T(np.maximum(pos, 0))
            rCl[j] = Cl.reshape(128, 2, 512).transpose(1, 0, 2)
            rSl[j] = Sl.reshape(128, 2, 512).transpose(1, 0, 2)
            tpB[j] = np.broadcast_to((512 * T + np.arange(512)).astype(np.float32)[None], (128, 512))
            for qb in range(4):
                t0 = 512 * T + 128 * qb
                tpt[:, 4 * j + qb] = t0 + np.arange(128)
                for c5 in range(5):
                    vW[:, 4 * j + qb, c5] = 1.0 if t0 - 512 + 128 * c5 >= 0 else 0.0
                vA[:, 4 * j + qb] = 1.0 if t0 - 128 >= 0 else 0.0
        d.update(ropeCl=rCl, ropeSl=rSl, tposB=tpB, tpos_tm=tpt, tposm128=tpt - 128.0, validW=vW, validA=vA)
        per.append(d)
    return com, per


DBG = {}


def build_nc(debug=None):
    nc = bass.Bass("TRN2", target_bir_lowering=False)
    dins = {}

    def din(name, shape, dt=F32):
        dins[name] = nc.dram_tensor(name, list(shape), dt, kind="ExternalInput").ap()
        return dins[name]

    x_all = din("x_all", [NT, 512, D])
    x_loc = din("x_loc", [NOWN, 1024, D])
    wA = din("wA", [D, NWA])
    wB = din("wB", [D, NWB])
    gA = din("gA", [128, 8])
    ident = din("ident", [128, 128])
    ropeC = din("ropeC", [NT, 128, 512])
    ropeS = din("ropeS", [NT, 128, 512])
    cmpC = din("cmpC", [128, 1024])
    cmpS = din("cmpS", [128, 1024])
    w1d = {kv: din(f"w1{kv}", [128, 32, 128]) for kv in "kv"}
    peTd = {kv: din(f"pe{kv}T", [128, 32]) for kv in "kv"}
    w2d = {"k": din("w2k", [128, 2, 128]), "v": din("w2v", [128, 2, 128]), "ksw": din("w2ksw", [128, 2, 128])}
    ssm_f = {n: din(n, [128, 16]) for n in ("lamre_f", "lamim_f", "logdt_f")}
    ssm_b = {n: din(n, [128, 16, 16]) for n in ("bre_f", "bim_f", "cre_f", "cim_f")}
    ssm_r = {n: din(n, [128, 16, 128]) for n in ("lamre_r", "lamim_r", "logdt_r")}
    tcol = din("tcol", [128, 1])
    trow = din("trow", [128, 128])
    onehot = din("onehot", [128, NOWN, NT])
    fnorm = din("final_norm", [1, D])
    Fbase_d = din("Fbase", [128, 8192], BF16)
    OV_d = din("OV", [128, 8, 256], BF16)
    mWM0_d, mTRI_d, mLMA_d = din("mWM0", [128, 128], BF16), din("mTRI", [128, 128], BF16), din("mLMA", [128, 128], BF16)
    blk64_d = din("blk64", [128, 256])
    cmpend_d = din("cmpend", [128, 8])
    dskip_d = din("dskip", [128, 512])
    wglu_d = din("wglu", [512, 1024])
    wout_d = din("wout", [1024, 1024])
    ropeCl = din("ropeCl", [NOWN, 2, 128, 512])
    ropeSl = din("ropeSl", [NOWN, 2, 128, 512])
    tposB_d = din("tposB", [NOWN, 128, 512])
    tpos_d = din("tpos_tm", [128, 16])
    tposm_d = din("tposm128", [128, 16])
    validW_d = din("validW", [128, 16, 5])
    validA_d = din("validA", [128, 16])
    h1d = nc.dram_tensor("h1scr", [16, 128, D], F32, kind="Internal").ap()
    uvb = nc.dram_tensor("uvtab_bf", [16384, 2 * D], BF16, kind="Internal").ap()
    wBb = nc.dram_tensor("wB_bf", [NWB // 128, 128, 8, 128], BF16, kind="Internal").ap()
    gffn_d = din("ffn_norm", [1, D])
    wq_d = din("wq", [D, 2048])
    sub1T_d = din("sub1T", [128, 128])
    sub2T_d = din("sub2T", [128, 128])
    utab = din("peer_u", [16384, D])
    vtab = din("peer_v", [16384, D])
    y = nc.dram_tensor("y", [NOWN, 512, D], F32, kind="ExternalOutput").ap()
    dbg_out = {}

    def dout(name, shape):
        dbg_out[name] = nc.dram_tensor("dbg_" + name, list(shape), F32, kind="ExternalOutput").ap()
        return dbg_out[name]

    with ExitStack() as es:
        S = Sync(nc, es)
        _nctr = [0]

        def sb(name, shape, dt, st=es):
            _nctr[0] += 1
            return st.enter_context(nc.sbuf_tensor(f"s{_nctr[0]}_{name}", list(shape), dt))
        pbank = [es.enter_context(nc.psum_tensor(f"pb{i}", [128, 512], F32)) for i in range(8)]

        def DMA(out, in_, r=(), w=(), q="sp"):
            return S.dma(q, lambda e: e.dma_start(out=out, in_=in_), reads=r, writes=w)

        def V(fn, r=(), w=(), e="dve"):
            return S.op(e, fn, reads=r, writes=w)

        def CP(out, in_, r=(), w=(), e="dve"):
            if e == "act":
                return S.op(e, lambda en: en.copy(out=out, in_=in_), reads=r, writes=w)
            return S.op(e, lambda en: en.tensor_copy(out=out, in_=in_), reads=r, writes=w)

        def MM(out, lhsT, rhs, start, stop, r=(), w=()):
            return S.op("pe", lambda e: e.matmul(out=out, lhsT=lhsT, rhs=rhs, start=start, stop=stop, skip_group_check=True),
                        reads=r, writes=w)

        identf = sb("identf", [128, 128], F32)
        identb = sb("identb", [128, 128], BF16)
        gAs = sb("gAs", [128, 8], F32)
        sctx = ExitStack()
        ksT = sb("ksT", [128, SEQ], BF16, sctx)
        vsaug = sb("vsaug", [128, 128, 2, 65], BF16, sctx)
        kcT = sb("kcT", [128, 1024], BF16, sctx)
        vcT = sb("vcT", [128, 1024], BF16, sctx)
        Xtile = sb("Xtile", [128, NT, 32], F32, sctx)
        kap = sb("kap", [128, 2, 16], F32, sctx)
        Bbar = sb("Bbar", [128, 2, 16, 16], F32, sctx)
        af = sb("af", [128, 16], F32, sctx)
        thf = sb("thf", [128, 16], F32, sctx)
        DMA(identf[:], ident, w=["identf"])
        DMA(gAs[:], gA, w=["gAs"])
        V(lambda e: e.tensor_copy(out=identb[:], in_=identf[:]), r=["identf"], w=["identb"])
        V(lambda e: e.memset(vsaug[:], 1.0), w=["vsaug"], e="pool")
        V(lambda e: e.memset(kcT[:], 0.0), w=["kcT"], e="pool")
        V(lambda e: e.memset(vcT[:], 0.0), w=["vcT"], e="pool")

        with ExitStack() as sa:
            sba = lambda name, shape, dt: sb(name, shape, dt, sa)
            Wa = sba("Wa", [128, 8, NWA], BF16)
            W1 = {kv: sba(f"W1{kv}", [128, 32, 128], BF16) for kv in "kv"}
            biasc = {kv: sba(f"biasc{kv}", [128, 1], F32) for kv in "kv"}
            W2 = {nm: sba(f"W2{nm}", [128, 2, 128], BF16) for nm in ("k", "ksw", "v")}
            L128 = sba("L128", [128, 2, 16], F32)
            Bs1 = sba("Bs1", [128, 16, 2, 32], F32)
            Bs2 = sba("Bs2", [128, 16, 2, 32], F32)
            Gt = sba("Gt", [128, 2, 16, 128], BF16)
            sp_ = sa.enter_context(ExitStack())
            sba_persist = sba
            sba = lambda name, shape, dt: sb(name, shape, dt, sp_)
            wst = [sba(f"wst{i}", [128, 8, 128], F32) for i in range(2)]
            for ci, c0 in enumerate(range(0, NWA, 128)):
                w_ = wst[ci % 2]
                DMA(w_[:], wA[:, c0:c0 + 128].rearrange("(c p) n -> p c n", p=128), w=[f"wst{ci % 2}"])
                for dc in range(8):
                    V(lambda e: e.tensor_scalar(out=Wa[:, dc, c0:c0 + 128], in0=w_[:, dc, :],
                                                scalar1=gAs[:, dc:dc + 1], scalar2=None, op0=ALU.mult),
                      r=[f"wst{ci % 2}", "gAs"], w=["Wa"], e=("dve" if dc % 2 else "pool"))
            wbo = [sba(f"wbo{i}", [128, 8, 128], BF16) for i in range(2)]
            for ci, c0 in enumerate(range(0, NWB, 128)):
                w_ = wst[ci % 2]
                o_ = wbo[ci % 2]
                DMA(w_[:], wB[:, c0:c0 + 128].rearrange("(c p) n -> p c n", p=128), w=[f"wst{ci % 2}"])
                for dc in range(8):
                    V(lambda e: e.tensor_scalar(out=o_[:, dc, :], in0=w_[:, dc, :], scalar1=gAs[:, dc:dc + 1], scalar2=None, op0=ALU.mult),
                      r=[f"wst{ci % 2}", "gAs"], w=[f"wbo{ci % 2}"], e=("dve" if dc % 2 else "pool"))
                DMA(wBb[ci], o_[:], r=[f"wbo{ci % 2}"], w=["wBb"])
            w1st = sba("w1st", [128, 16, 128], F32)
            w2st = sba("w2st", [128, 2, 128], F32)
            pest = sba("pest", [128, 32], F32)
            peb = sba("peb", [128, 32], BF16)
            for kv in "kv":
                for hf in range(2):
                    DMA(w1st[:], w1d[kv][:, 16 * hf:16 * hf + 16, :], w=["w1st"])
                    V(lambda e: e.tensor_copy(out=W1[kv][:, 16 * hf:16 * hf + 16, :], in_=w1st[:]), r=["w1st"], w=[f"W1{kv}"])
                DMA(pest[:], peTd[kv], w=["pest"])
                V(lambda e: e.tensor_copy(out=peb[:], in_=pest[:]), r=["pest"], w=["peb"])
                for l in range(32):
                    MM(pbank[5][:, 0:1], W1[kv][0:64, l, :], peb[0:64, l:l + 1], l == 0, l == 31,
                       r=[f"W1{kv}", "peb"], w=["pb5"])
                V(lambda e: e.tensor_copy(out=biasc[kv][:], in_=pbank[5][:, 0:1]), r=["pb5"], w=[f"biasc{kv}"])
            for nm in ("k", "ksw", "v"):
                DMA(w2st[:], w2d[nm], w=["w2st"])
                V(lambda e: e.tensor_copy(out=W2[nm][:], in_=w2st[:]), r=["w2st"], w=[f"W2{nm}"])

            sf = {n: sba(n, [128, 16], F32) for n in ssm_f}
            for n in ssm_f:
                DMA(sf[n][:], ssm_f[n], w=[n])
            bf_ = {n: sba(n, [128, 16, 16], F32) for n in ("bre_f", "bim_f")}
            for n in bf_:
                DMA(bf_[n][:], ssm_b[n], w=[n])
            dtf = sba("dtf", [128, 16], F32)
            V(lambda e: e.activation(out=dtf[:], in_=sf["logdt_f"][:], func=AF.Exp), r=["logdt_f"], w=["dtf"], e="act")
            V(lambda e: e.tensor_tensor(out=af[:], in0=sf["lamre_f"][:], in1=dtf[:], op=ALU.mult), r=["lamre_f", "dtf"], w=["af"])
            V(lambda e: e.tensor_tensor(out=thf[:], in0=sf["lamim_f"][:], in1=dtf[:], op=ALU.mult), r=["lamim_f", "dtf"], w=["thf"])

            tmpi = sba("tmpi", [128, 1024], I32)
            tmpa = sba("tmpa", [128, 1024], F32)
            tmpb = sba("tmpb", [128, 1024], F32)

            def sincos(out_ap, ang_ap, n, shift, key_out, key_ang):
                A_, B_, I_ = tmpa[:, 0:n], tmpb[:, 0:n], tmpi[:, 0:n]
                V(lambda e: e.tensor_scalar(out=A_, in0=ang_ap, scalar1=shift, scalar2=1.0 / (2 * PI),
                                            op0=ALU.add, op1=ALU.mult), r=[key_ang], w=["tmpa"])
                V(lambda e: e.tensor_copy(out=I_, in_=A_), r=["tmpa"], w=["tmpi"])
                V(lambda e: e.tensor_copy(out=B_, in_=I_), r=["tmpi"], w=["tmpb"])
                V(lambda e: e.tensor_tensor(out=A_, in0=A_, in1=B_, op=ALU.subtract), r=["tmpa", "tmpb"], w=["tmpa"])
                V(lambda e: e.tensor_scalar(out=B_, in0=A_, scalar1=0.5, scalar2=None, op0=ALU.is_gt),
                  r=["tmpa"], w=["tmpb"])
                V(lambda e: e.tensor_tensor(out=A_, in0=A_, in1=B_, op=ALU.subtract), r=["tmpa", "tmpb"], w=["tmpa"])
                V(lambda e: e.tensor_scalar(out=B_, in0=A_, scalar1=-0.5, scalar2=None, op0=ALU.is_lt),
                  r=["tmpa"], w=["tmpb"])
                V(lambda e: e.tensor_tensor(out=A_, in0=A_, in1=B_, op=ALU.add), r=["tmpa", "tmpb"], w=["tmpa"])
                V(lambda e: e.activation(out=out_ap, in_=A_, func=AF.Sin, scale=2 * PI), r=["tmpa"], w=[key_out], e="act")

            mag128 = sba("mag128", [128, 16], F32)
            ang128 = sba("ang128", [128, 16], F32)
            V(lambda e: e.activation(out=mag128[:], in_=af[:], func=AF.Exp, scale=128.0), r=["af"], w=["mag128"], e="act")
            V(lambda e: e.tensor_scalar(out=ang128[:], in0=thf[:], scalar1=128.0, scalar2=None, op0=ALU.mult), r=["thf"], w=["ang128"])
            sincos(L128[:, 0, :], ang128[:], 16, PI / 2, "L128c", "ang128")
            sincos(L128[:, 1, :], ang128[:], 16, 0.0, "L128s", "ang128")
            V(lambda e: e.tensor_tensor(out=L128[:, 0, :], in0=L128[:, 0, :], in1=mag128[:], op=ALU.mult), r=["L128c", "mag128"], w=["L128c"])
            V(lambda e: e.tensor_tensor(out=L128[:, 1, :], in0=L128[:, 1, :], in1=mag128[:], op=ALU.mult), r=["L128s", "mag128"], w=["L128s"])
            L1 = sba("L1", [128, 2, 16], F32)
            mag1 = sba("mag1", [128, 16], F32)
            V(lambda e: e.activation(out=mag1[:], in_=af[:], func=AF.Exp), r=["af"], w=["mag1"], e="act")
            sincos(L1[:, 0, :], thf[:], 16, PI / 2, "L1c", "thf")
            sincos(L1[:, 1, :], thf[:], 16, 0.0, "L1s", "thf")
            V(lambda e: e.tensor_tensor(out=L1[:, 0, :], in0=L1[:, 0, :], in1=mag1[:], op=ALU.mult), r=["L1c", "mag1"], w=["L1c"])
            V(lambda e: e.tensor_tensor(out=L1[:, 1, :], in0=L1[:, 1, :], in1=mag1[:], op=ALU.mult), r=["L1s", "mag1"], w=["L1s"])
            t1 = sba("t1", [128, 16], F32)
            t2 = sba("t2", [128, 16], F32)
            den = sba("den", [128, 16], F32)
            lr, li = sf["lamre_f"], sf["lamim_f"]
            V(lambda e: e.tensor_tensor(out=den[:], in0=lr[:], in1=lr[:], op=ALU.mult), r=["lamre_f"], w=["den"])
            V(lambda e: e.tensor_tensor(out=t1[:], in0=li[:], in1=li[:], op=ALU.mult), r=["lamim_f"], w=["t1"])
            V(lambda e: e.tensor_tensor(out=den[:], in0=den[:], in1=t1[:], op=ALU.add), r=["den", "t1"], w=["den"])
            V(lambda e: e.reciprocal(out=den[:], in_=den[:]), r=["den"], w=["den"])
            V(lambda e: e.tensor_scalar(out=t1[:], in0=L1[:, 0, :], scalar1=-1.0, scalar2=None, op0=ALU.add), r=["L1c"], w=["t1"])
            V(lambda e: e.tensor_tensor(out=kap[:, 0, :], in0=t1[:], in1=lr[:], op=ALU.mult), r=["t1", "lamre_f"], w=["kapr"])
            V(lambda e: e.tensor_tensor(out=t2[:], in0=L1[:, 1, :], in1=li[:], op=ALU.mult), r=["L1s", "lamim_f"], w=["t2"])
            V(lambda e: e.tensor_tensor(out=kap[:, 0, :], in0=kap[:, 0, :], in1=t2[:], op=ALU.add), r=["kapr", "t2"], w=["kapr"])
            V(lambda e: e.tensor_tensor(out=kap[:, 0, :], in0=kap[:, 0, :], in1=den[:], op=ALU.mult), r=["kapr", "den"], w=["kapr"])
            V(lambda e: e.tensor_tensor(out=kap[:, 1, :], in0=L1[:, 1, :], in1=lr[:], op=ALU.mult), r=["L1s", "lamre_f"], w=["kapi"])
            V(lambda e: e.tensor_tensor(out=t2[:], in0=t1[:], in1=li[:], op=ALU.mult), r=["t1", "lamim_f"], w=["t2"])
            V(lambda e: e.tensor_tensor(out=kap[:, 1, :], in0=kap[:, 1, :], in1=t2[:], op=ALU.subtract), r=["kapi", "t2"], w=["kapi"])
            V(lambda e: e.tensor_tensor(out=kap[:, 1, :], in0=kap[:, 1, :], in1=den[:], op=ALU.mult), r=["kapi", "den"], w=["kapi"])
            tb1 = sba("tb1", [128, 16, 16], F32)
            kr_b = kap[:, 0, :].unsqueeze(2).to_broadcast([128, 16, 16])
            ki_b = kap[:, 1, :].unsqueeze(2).to_broadcast([128, 16, 16])
            V(lambda e: e.tensor_tensor(out=Bbar[:, 0], in0=bf_["bre_f"][:], in1=kr_b, op=ALU.mult), r=["bre_f", "kapr"], w=["Bbr"])
            V(lambda e: e.tensor_tensor(out=tb1[:], in0=bf_["bim_f"][:], in1=ki_b, op=ALU.mult), r=["bim_f", "kapi"], w=["tb1"])
            V(lambda e: e.tensor_tensor(out=Bbar[:, 0], in0=Bbar[:, 0], in1=tb1[:], op=ALU.subtract), r=["Bbr", "tb1"], w=["Bbr"])
            V(lambda e: e.tensor_tensor(out=Bbar[:, 1], in0=bf_["bim_f"][:], in1=kr_b, op=ALU.mult), r=["bim_f", "kapr"], w=["Bbi"])
            V(lambda e: e.tensor_tensor(out=tb1[:], in0=bf_["bre_f"][:], in1=ki_b, op=ALU.mult), r=["bre_f", "kapi"], w=["tb1"])
            V(lambda e: e.tensor_tensor(out=Bbar[:, 1], in0=Bbar[:, 1], in1=tb1[:], op=ALU.add), r=["Bbi", "tb1"], w=["Bbi"])
            V(lambda e: e.memset(Bs1[:], 0.0), w=["Bs1"], e="pool")
            V(lambda e: e.memset(Bs2[:], 0.0), w=["Bs2"], e="pool")
            for glo in range(2):
                ps_ = slice(64 * glo, 64 * glo + 64)
                cs_ = slice(16 * glo, 16 * glo + 16)
                V(lambda e: e.tensor_copy(out=Bs1[ps_, :, 0, cs_], in_=Bbar[ps_, 0]), r=["Bbr"], w=["Bs1"])
                V(lambda e: e.tensor_scalar(out=Bs1[ps_, :, 1, cs_], in0=Bbar[ps_, 1], scalar1=-1.0, scalar2=None, op0=ALU.mult), r=["Bbi"], w=["Bs1"])
                V(lambda e: e.tensor_copy(out=Bs2[ps_, :, 0, cs_], in_=Bbar[ps_, 1]), r=["Bbi"], w=["Bs2"])
                V(lambda e: e.tensor_copy(out=Bs2[ps_, :, 1, cs_], in_=Bbar[ps_, 0]), r=["Bbr"], w=["Bs2"])
            rrb = sba("rrb", [128, 8, 128], F32)
            tcs = sba("tcs", [128, 1], F32)
            DMA(tcs[:], tcol, w=["tcs"])
            dtr = sba("dtr", [128, 1024], F32)
            ar_ = sba("ar_", [128, 1024], F32)
            magr = sba("magr", [128, 1024], F32)
            trg = sba("trg", [128, 1024], F32)
            rrf = rrb[:].rearrange("p a b -> p (a b)")
            for kh in range(2):
                ksl = slice(8 * kh, 8 * kh + 8)
                DMA(rrb[:], ssm_r["logdt_r"][:, ksl, :], w=["rrb"])
                V(lambda e: e.activation(out=dtr[:], in_=rrf, func=AF.Exp), r=["rrb"], w=["dtr"], e="act")
                DMA(rrb[:], ssm_r["lamre_r"][:, ksl, :], w=["rrb"])
                V(lambda e: e.tensor_tensor(out=ar_[:], in0=rrf, in1=dtr[:], op=ALU.mult), r=["rrb", "dtr"], w=["ar_"])
                V(lambda e: e.activation(out=magr[:], in_=ar_[:], func=AF.Exp, scale=tcs[:, 0:1]), r=["ar_", "tcs"], w=["magr"], e="act")
                DMA(rrb[:], ssm_r["lamim_r"][:, ksl, :], w=["rrb"])
                V(lambda e: e.tensor_tensor(out=ar_[:], in0=rrf, in1=dtr[:], op=ALU.mult), r=["rrb", "dtr", "magr"], w=["ar_"])
                V(lambda e: e.tensor_scalar(out=ar_[:], in0=ar_[:], scalar1=tcs[:, 0:1], scalar2=None, op0=ALU.mult), r=["ar_", "tcs"], w=["ar_"])
                for ri, sh in ((0, PI / 2), (1, 0.0)):
                    sincos(trg[:], ar_[:], 1024, sh, "trg", "ar_")
                    V(lambda e: e.tensor_tensor(out=Gt[:, ri, ksl, :].rearrange("p a b -> p (a b)"), in0=trg[:], in1=magr[:], op=ALU.mult),
                      r=["trg", "magr"], w=["Gt"])
            S.barrier()
            sp_.close()
            sba = sba_persist

            xt = [sba(f"xt{i}", [128, 4, D], F32) for i in range(2)]
            xs = sba("xs", [128, 4, D], BF16)
            ss = sba("ss", [128, 4], F32)
            rstd = sba("rstd", [128, 4], F32)
            zT = sba("zT", [128, 8, 512], BF16)
            rC = [sba(f"rC{i}", [128, 512], F32) for i in range(2)]
            rS = [sba(f"rS{i}", [128, 512], F32) for i in range(2)]
            rtmp = sba("rtmp", [128, 512], F32)
            craw = {kv: [sba(f"craw{kv}{i}", [128, 528], BF16) for i in range(2)] for kv in "kv"}
            utm = sba("utm", [128, 512], BF16)
            hid = sba("hid", [128, 4, 32], BF16)
            Eblk = sba("Eblk", [128, 32], F32)
            cC = sba("cC", [128, 32], F32)
            cS = sba("cS", [128, 32], F32)
            Xc = sba("Xc", [128, 32], F32)
            Xn = sba("Xn", [128, 32], F32)
            ta = sba("ta", [128, 16], F32)
            V(lambda e: e.memset(Xc[:], 0.0), w=["Xc"])
            Lr, Li = L128[:, 0, :], L128[:, 1, :]
            lk = ["L128c", "L128s"]
            Msb = sba("Msb", [128, 1024], F32)
            cmb1 = sba("cmb1", [128, 1024], F32)
            cmb2 = sba("cmb2", [128, 1024], F32)
            for kv in "kv":
                for i in range(2):
                    V(lambda e: e.memset(craw[kv][i][:], 0.0), w=[f"craw{kv}{i}"], e="pool")

            def compress(Tc, nblk):
                pi_ = Tc % 2
                ph = pbank[5]
                for a, kv in enumerate("kv"):
                    for g in range(2):
                        col = 128 + 32 * (2 * a + g)
                        for l in range(32):
                            MM(ph[:, col:col + nblk], W1[kv][64 * g:64 * g + 64, l, :],
                               craw[kv][pi_][64 * g:64 * g + 64, l:l + 16 * (nblk - 1) + 1:16],
                               l == 0, l == 31, r=[f"W1{kv}", f"craw{kv}{pi_}", "hidall"], w=["pb5"])
                        if DBG.get("cstage", 9) < 1:
                            continue
                        V(lambda e: e.activation(out=hid[:, 2 * a + g, 0:nblk], in_=ph[:, col:col + nblk],
                                                 func=AF.Gelu_apprx_tanh, bias=biasc[kv][:, 0:1], scale=1.0),
                          r=["pb5", f"biasc{kv}"], w=[f"hid{a}{g}", "hidall"], e="act")
                n0 = 32 * Tc
                if DBG.get("cstage", 9) < 2:
                    return
                DMA(cC[:], cmpC[:, n0:n0 + 32], w=["cC"])
                DMA(cS[:], cmpS[:, n0:n0 + 32], w=["cS"])
                for j, nm in enumerate(("k", "ksw", "v")):
                    a = 0 if nm != "v" else 1
                    col = 256 + 32 * j
                    for g in range(2):
                        MM(ph[:, col:col + nblk], W2[nm][:, g, :], hid[:, 2 * a + g, 0:nblk], g == 0, g == 1,
                           r=[f"W2{nm}", f"hid{a}{g}"], w=["pb5"])
                if DBG.get("cstage", 9) < 3:
                    return
                V(lambda e: e.tensor_tensor(out=rtmp[:, 0:nblk], in0=ph[:, 256:256 + nblk], in1=cC[:, 0:nblk], op=ALU.mult),
                  r=["pb5", "cC"], w=["rtmp"])
                V(lambda e: e.tensor_tensor(out=rtmp[:, 32:32 + nblk], in0=ph[:, 288:288 + nblk], in1=cS[:, 0:nblk], op=ALU.mult),
                  r=["pb5", "cS"], w=["rtmp"])
                V(lambda e: e.tensor_tensor(out=kcT[:, n0:n0 + nblk], in0=rtmp[:, 0:nblk], in1=rtmp[:, 32:32 + nblk], op=ALU.add),
                  r=["rtmp"], w=["kcT"])
                V(lambda e: e.tensor_copy(out=vcT[:, n0:n0 + nblk], in_=ph[:, 320:320 + nblk]), r=["pb5"], w=["vcT"])

            for T in range(DBG.get("ntiles", NT)):
                p_ = T % 2
                x_ = xt[p_]
                DMA(x_[:], x_all[T].rearrange("(a p) d -> p a d", p=128), w=[f"xt{p_}"])
                DMA(rC[p_][:], ropeC[T], w=[f"rC{p_}"])
                DMA(rS[p_][:], ropeS[T], w=[f"rS{p_}"])
                for a in range(4):
                    V(lambda e: e.activation(out=xs[:, a, :], in_=x_[:, a, :], func=AF.Square, accum_out=ss[:, a:a + 1]),
                      r=[f"xt{p_}"], w=[f"xs{a}", "ss"], e="act")
                V(lambda e: e.activation(out=rstd[:], in_=ss[:], func=AF.Sqrt, bias=EPS, scale=1.0 / D),
                  r=["ss"], w=["rstd"], e="act")
                V(lambda e: e.reciprocal(out=rstd[:], in_=rstd[:]), r=["rstd"], w=["rstd"])
                for a in range(4):
                    V(lambda e: e.tensor_scalar(out=xs[:, a, :], in0=x_[:, a, :], scalar1=rstd[:, a:a + 1], scalar2=None, op0=ALU.mult),
                      r=[f"xt{p_}", "rstd"], w=[f"xs{a}"], e=("dve" if a % 2 else "pool"))
                for k2 in range(4):
                    pbb = pbank[k2][:].bitcast(BF16)
                    for dd in range(2):
                        dc = 2 * k2 + dd
                        for a in range(4):
                            S.op("pe", lambda e: e.transpose(out=pbb[:, dd * 512 + a * 128: dd * 512 + a * 128 + 128],
                                                             in_=xs[:, a, dc * 128:(dc + 1) * 128], identity=identb[:]),
                                 reads=[f"xs{a}", "identb"], writes=[f"pb{k2}"])
                    CP(zT[:, 2 * k2:2 * k2 + 2, :].rearrange("p a b -> p (a b)"), pbb,
                       r=[f"pb{k2}"], w=[f"zT{k2}"], e=("act" if k2 % 2 else "dve"))
                zk = [f"zT{k2}" for k2 in range(4)]
                for blk in range(4):
                    for dc in range(8):
                        MM(pbank[blk][:], Wa[:, dc, blk * 128:(blk + 1) * 128], zT[:, dc, :], dc == 0, dc == 7,
                           r=["Wa"] + zk, w=[f"pb{blk}"])
                V(lambda e: e.tensor_tensor(out=rtmp[:], in0=pbank[1][:], in1=rS[p_][:], op=ALU.mult), r=["pb1", f"rS{p_}"], w=["rtmp"])
                V(lambda e: e.tensor_tensor(out=rC[p_][:], in0=pbank[0][:], in1=rC[p_][:], op=ALU.mult), r=["pb0", f"rC{p_}"], w=[f"rC{p_}"])
                V(lambda e: e.tensor_tensor(out=ksT[:, 512 * T:512 * T + 512], in0=rC[p_][:], in1=rtmp[:], op=ALU.add),
                  r=[f"rC{p_}", "rtmp"], w=["ksT"])
                for a, kv in enumerate("kv"):
                    V(lambda e: e.activation(out=craw[kv][p_][:, 0:512], in_=pbank[2 + a][:], func=AF.Copy),
                      r=[f"pb{2 + a}"], w=[f"craw{kv}{p_}"], e="act")
                    if T > 0:
                        V(lambda e: e.tensor_copy(out=craw[kv][1 - p_][:, 512:528], in_=craw[kv][p_][:, 0:16]),
                          r=[f"craw{kv}{p_}"], w=[f"craw{kv}{1 - p_}"], e="pool")
                for a in range(0 if not DBG.get("nossm") else 4, 4):
                    b = 4 * T + a
                    for dc in range(8):
                        MM(pbank[5][:, 0:128], zT[:, dc, a * 128:(a + 1) * 128], Wa[:, dc, 512:640], dc == 0, dc == 7,
                           r=["Wa"] + zk, w=["pb5"])
                    CP(vsaug[:, b, :, 0:64], pbank[5][:, 0:128].rearrange("p (g d) -> p g d", g=2),
                       r=["pb5"], w=["vsaug"], e="act")
                    for dc in range(8):
                        MM(pbank[4][:], zT[:, dc, a * 128:(a + 1) * 128], Wa[:, dc, 640:1152], dc == 0, dc == 7,
                           r=["Wa"] + zk, w=["pb4"])
                    V(lambda e: e.tensor_copy(out=utm[:], in_=pbank[4][:]), r=["pb4"], w=["utm"])
                    for k in range(16):
                        for ri in range(2):
                            bank = pbank[6 + k // 8]
                            c0 = (k % 8) * 64 + ri * 32
                            MM(bank[:, c0:c0 + 32], Gt[:, ri, k, :], utm[:, 32 * k:32 * k + 32], True, True,
                               r=["Gt", "utm"], w=[f"pb{6 + k // 8}"])
                    for hh in range(2):
                        CP(Msb[:, hh * 512:(hh + 1) * 512], pbank[6 + hh][:], r=[f"pb{6 + hh}"], w=[f"Msb{hh}"], e="act")
                        V(lambda e: e.tensor_tensor(out=cmb1[:, hh * 512:(hh + 1) * 512], in0=Msb[:, hh * 512:(hh + 1) * 512],
                                                    in1=Bs1[:, 8 * hh:8 * hh + 8].rearrange("p a b c -> p (a b c)"), op=ALU.mult),
                          r=[f"Msb{hh}", "Bs1"], w=["cmb1"])
                        V(lambda e: e.tensor_tensor(out=cmb2[:, hh * 512:(hh + 1) * 512], in0=Msb[:, hh * 512:(hh + 1) * 512],
                                                    in1=Bs2[:, 8 * hh:8 * hh + 8].rearrange("p a b c -> p (a b c)"), op=ALU.mult),
                          r=[f"Msb{hh}", "Bs2"], w=["cmb2"], e="pool")
                    V(lambda e: e.tensor_reduce(out=Eblk[:, 0:16], in_=cmb1[:].rearrange("p (k x) -> p k x", k=16), axis=AX.X, op=ALU.add),
                      r=["cmb1"], w=["Eblk"])
                    V(lambda e: e.tensor_reduce(out=Eblk[:, 16:32], in_=cmb2[:].rearrange("p (k x) -> p k x", k=16), axis=AX.X, op=ALU.add),
                      r=["cmb2"], w=["Eblk"])
                    if b % 4 == 0:
                        V(lambda e: e.tensor_copy(out=Xtile[:, b // 4, :], in_=Xc[:]), r=["Xc"], w=["Xtile"])
                    V(lambda e: e.tensor_tensor(out=Xn[:, 0:16], in0=Xc[:, 0:16], in1=Lr, op=ALU.mult), r=["Xc"] + lk, w=["Xn"])
                    V(lambda e: e.tensor_tensor(out=ta[:], in0=Xc[:, 16:32], in1=Li, op=ALU.mult), r=["Xc"] + lk, w=["ta"])
                    V(lambda e: e.tensor_tensor(out=Xn[:, 0:16], in0=Xn[:, 0:16], in1=ta[:], op=ALU.subtract), r=["Xn", "ta"], w=["Xn"])
                    V(lambda e: e.tensor_tensor(out=Xn[:, 16:32], in0=Xc[:, 0:16], in1=Li, op=ALU.mult), r=["Xc"] + lk, w=["Xn"])
                    V(lambda e: e.tensor_tensor(out=ta[:], in0=Xc[:, 16:32], in1=Lr, op=ALU.mult), r=["Xc"] + lk, w=["ta"])
                    V(lambda e: e.tensor_tensor(out=Xn[:, 16:32], in0=Xn[:, 16:32], in1=ta[:], op=ALU.add), r=["Xn", "ta"], w=["Xn"])
                    V(lambda e: e.tensor_tensor(out=Xc[:], in0=Xn[:], in1=Eblk[:], op=ALU.add), r=["Xn", "Eblk"], w=["Xc"])
                if T > 0 and not DBG.get("nocompress"):
                    compress(T - 1, 32)
            if not DBG.get("nocompress"):
                compress(NT - 1, 31)

            if debug == "A":
                def doutt(name, shape, dt):
                    dbg_out[name] = nc.dram_tensor("dbg_" + name, list(shape), dt, kind="ExternalOutput").ap()
                    return dbg_out[name]
                DMA(doutt("ksT", [128, SEQ], BF16), ksT[:], r=["ksT"])
                DMA(doutt("vs", [128, 128 * 130], BF16), vsaug[:].rearrange("p a g d -> p (a g d)"), r=["vsaug"])
                DMA(doutt("kcT", [128, 1024], BF16), kcT[:], r=["kcT"])
                DMA(doutt("vcT", [128, 1024], BF16), vcT[:], r=["vcT"])
                DMA(doutt("Xtile", [128, NT * 32], F32), Xtile[:].rearrange("p a b -> p (a b)"), r=["Xtile"])
            S.barrier()


        with ExitStack() as sB:
            sbb = lambda name, shape, dt, st=sB: sb(name, shape, dt, st)
            mWM0, mTRI, mLMA = sbb("mWM0", [128, 128], BF16), sbb("mTRI", [128, 128], BF16), sbb("mLMA", [128, 128], BF16)
            blk64 = sbb("blk64", [128, 256], F32)
            cmpend = sbb("cmpend", [128, 8], F32)
            tpos = sbb("tpos", [128, 16], F32)
            tposm = sbb("tposm", [128, 16], F32)
            validW = sbb("validW", [128, 16, 5], F32)
            validA = sbb("validA", [128, 16], F32)
            vcaug = sbb("vcaug", [128, 8, 2, 65], BF16)
            qTz = sbb("qTz", [128, 2, 4, 512], BF16)
            kslocT = sbb("kslocT", [128, 640], BF16)
            kwT = sbb("kwT", [128, 1024], BF16)
            vslaug = sbb("vslaug", [128, 5, 2, 65], BF16)
            vwaug = sbb("vwaug", [128, 8, 2, 65], BF16)
            uT = sbb("uT", [128, 4, 512], BF16)
            utm = sbb("utmB", [128, 4, 512], BF16)
            gsig = sbb("gsig", [128, 4, 24], F32)
            aout = sbb("aout", [128, 4, 512], BF16)
            sout = sbb("sout", [128, 4, 512], BF16)
            tposBt = sbb("tposBt", [128, 512], F32)
            for (t_, d_, k_) in ((mWM0, mWM0_d, "mWM0"), (mTRI, mTRI_d, "mTRI"),
                                 (mLMA, mLMA_d, "mLMA"), (blk64, blk64_d, "blk64"), (cmpend, cmpend_d, "cmpend"),
                                 (tpos, tpos_d, "tpos"), (tposm, tposm_d, "tposm"), (validW, validW_d, "validW"),
                                 (validA, validA_d, "validA")):
                DMA(t_[:], d_, w=[k_])
            V(lambda e: e.memset(vcaug[:], 1.0), w=["vcaug"], e="pool")
            V(lambda e: e.memset(vslaug[:], 1.0), w=["vslaug"], e="pool")
            V(lambda e: e.memset(vwaug[:], 1.0), w=["vwaug"], e="pool")
            V(lambda e: e.memset(qTz[:], 0.0), w=["qTz"], e="pool")
            for m in range(8):
                pbb = pbank[7][:].bitcast(BF16)
                S.op("pe", lambda e: e.transpose(out=pbb[:, 0:128], in_=vcT[:, 128 * m:128 * m + 128], identity=identb[:]),
                     reads=["vcT", "identb"], writes=["pb7"])
                CP(vcaug[:, m, :, 0:64], pbb[:, 0:128].rearrange("p (g d) -> p g d", g=2), r=["pb7"], w=["vcaug"])

            cvin = [sbb(f"cvin{i}", [128, D], F32) for i in range(2)]
            cvout = [sbb(f"cvout{i}", [128, D], BF16) for i in range(2)]
            bg = []
            for ti, (src_t, dst_t) in enumerate(((utab, uvb[:, 0:D]), (vtab, uvb[:, D:2 * D]))):
                def op_in(ch, src_t=src_t):
                    i2 = ch % 2
                    return lambda: DMA(cvin[i2][:], src_t[128 * ch:128 * ch + 128, :], w=[f"cvin{i2}"])

                def op_cast(ch):
                    i2 = ch % 2
                    return lambda: CP(cvout[i2][:], cvin[i2][:], r=[f"cvin{i2}"], w=[f"cvout{i2}"], e="pool")

                def op_out(ch, dst_t=dst_t):
                    i2 = ch % 2
                    return lambda: DMA(dst_t[128 * ch:128 * ch + 128, :], cvout[i2][:], r=[f"cvout{i2}"], w=["tabbf"])
                bg.append(op_in(0))
                bg.append(op_in(1))
                for ch in range(128):
                    bg.append(op_cast(ch))
                    bg.append(op_out(ch))
                    if ch + 2 < 128:
                        bg.append(op_in(ch + 2))

            def bg_step(n=1):
                for _ in range(n):
                    if bg:
                        bg.pop(0)()

            wstB = [sbb(f"wstB{i}", [128, 8, 128], F32) for i in range(2)]
            wblB = [sbb(f"wblB{i}", [128, 8, 128], BF16) for i in range(3)]
            wctr = [0]

            def load_wblk(c0):
                i = wctr[0]
                wctr[0] += 1
                bl_ = wblB[i % 3]
                DMA(bl_[:], wBb[c0 // 128], r=["wBb"], w=[f"wblB{i % 3}"])
                return bl_, f"wblB{i % 3}"

            for jt in range(DBG.get("ntilesB", NOWN)):
                with ExitStack() as s1:
                    sb1 = lambda name, shape, dt: sb(name, shape, dt, s1)
                    xb = [sb1(f"xb{i}", [128, D], F32) for i in range(2)]
                    xsb = [sb1(f"xsb{i}", [128, D], BF16) for i in range(2)]
                    ssb = sb1("ssb", [128, 8], F32)
                    rsb = sb1("rsb", [128, 8], F32)
                    zT = sb1("zTB", [128, 8, 512], BF16)
                    rCt = sb1("rCt", [128, 512], F32)
                    rSt = sb1("rSt", [128, 512], F32)
                    rt1 = sb1("rt1", [128, 512], F32)
                    rt2 = sb1("rt2", [128, 512], F32)
                    DMA(tposBt[:], tposB_d[jt], w=["tposBt"])
                    for st in range(2):
                        DMA(rCt[:], ropeCl[jt, st], w=["rCt"])
                        DMA(rSt[:], ropeSl[jt, st], w=["rSt"])
                        for a in range(4):
                            i2 = a % 2
                            DMA(xb[i2][:], x_loc[jt, st * 512 + a * 128: st * 512 + a * 128 + 128, :], w=[f"xb{i2}"])
                            cidx = 4 * st + a
                            V(lambda e: e.activation(out=xsb[i2][:], in_=xb[i2][:], func=AF.Square, accum_out=ssb[:, cidx:cidx + 1]),
                              r=[f"xb{i2}"], w=[f"xsb{i2}", "ssb"], e="act")
                            V(lambda e: e.activation(out=rsb[:, cidx:cidx + 1], in_=ssb[:, cidx:cidx + 1], func=AF.Sqrt, bias=EPS, scale=1.0 / D),
                              r=["ssb"], w=["rsb"], e="act")
                            V(lambda e: e.reciprocal(out=rsb[:, cidx:cidx + 1], in_=rsb[:, cidx:cidx + 1]), r=["rsb"], w=["rsb"])
                            V(lambda e: e.tensor_scalar(out=xsb[i2][:], in0=xb[i2][:], scalar1=rsb[:, cidx:cidx + 1], scalar2=None, op0=ALU.mult),
                              r=[f"xb{i2}", "rsb"], w=[f"xsb{i2}"])
                            pbb = pbank[a % 2][:].bitcast(BF16)
                            for dc in range(8):
                                S.op("pe", lambda e: e.transpose(out=pbb[:, dc * 128:(dc + 1) * 128], in_=xsb[i2][:, dc * 128:(dc + 1) * 128],
                                                                 identity=identb[:]), reads=[f"xsb{i2}", "identb"], writes=[f"pb{a % 2}"])
                            CP(zT[:, :, a * 128:(a + 1) * 128], pbb.rearrange("p (c t) -> p c t", c=8), r=[f"pb{a % 2}"], w=["zTB"],
                               e=("act" if a % 2 else "dve"))

                        def fm_block(c0, bank):
                            wb_, wk_ = load_wblk(c0)
                            for dc in range(8):
                                MM(pbank[bank][:], wb_[:, dc, :], zT[:, dc, :], dc == 0, dc == 7, r=[wk_, "zTB"], w=[f"pb{bank}"])

                        def rope_evac(bA, bS, outs):
                            V(lambda e: e.tensor_tensor(out=rt1[:], in0=pbank[bA][:], in1=rCt[:], op=ALU.mult), r=[f"pb{bA}", "rCt"], w=["rt1"])
                            V(lambda e: e.tensor_tensor(out=rt2[:], in0=pbank[bS][:], in1=rSt[:], op=ALU.mult), r=[f"pb{bS}", "rSt"], w=["rt2"])
                            for (o_, ps_, k_, cs_) in outs:
                                V(lambda e: e.tensor_tensor(out=o_, in0=rt1[ps_, cs_], in1=rt2[ps_, cs_], op=ALU.add), r=["rt1", "rt2"], w=[k_], e="pool")

                        if st == 1:
                            for hl in range(4):
                                fm_block(128 * hl, 2)
                                fm_block(512 + 128 * hl, 3)
                                rope_evac(2, 3, [(qTz[0:64, 0, hl, :], slice(0, 64), "qTz", slice(0, 512)), (qTz[64:128, 1, hl, :], slice(64, 128), "qTz", slice(0, 512))])
                        fm_block(1024, 2)
                        fm_block(1152, 3)
                        if st == 0:
                            rope_evac(2, 3, [(kslocT[:, 0:128], slice(0, 128), "kslocT", slice(384, 512))])
                        else:
                            rope_evac(2, 3, [(kslocT[:, 128:640], slice(0, 128), "kslocT", slice(0, 512))])
                        fm_block(1280, 2)
                        fm_block(1408, 3)
                        rope_evac(2, 3, [(kwT[:, st * 512:(st + 1) * 512], slice(0, 128), "kwT", slice(0, 512))])
                        if st == 1:
                            for ub in range(4):
                                fm_block(1536 + 128 * ub, 2)
                                CP(uT[:, ub, :], pbank[2][:], r=["pb2"], w=["uT"], e="act")
                        chunks = [("vw", 2176)] + ([("vs", 2048)] if True else [])
                        if st == 1:
                            chunks += [("gt", 2304)] + [(f"u{i}", 2432 + 128 * i) for i in range(4)]
                        for (nm, c0) in chunks:
                            wb_, wk_ = load_wblk(c0)
                            for a in range(4):
                                if nm == "vs" and st == 0 and a != 3:
                                    continue
                                for dc in range(8):
                                    MM(pbank[4][:, a * 128:(a + 1) * 128], zT[:, dc, a * 128:(a + 1) * 128], wb_[:, dc, :], dc == 0, dc == 7,
                                       r=[wk_, "zTB"], w=["pb4"])
                            for a in range(4):
                                src = pbank[4][:, a * 128:(a + 1) * 128]
                                if nm == "vw":
                                    CP(vwaug[:, 4 * st + a, :, 0:64], src.rearrange("p (g d) -> p g d", g=2), r=["pb4"], w=["vwaug"], e="act")
                                elif nm == "vs":
                                    if st == 0 and a != 3:
                                        continue
                                    CP(vslaug[:, (0 if st == 0 else 1 + a), :, 0:64], src.rearrange("p (g d) -> p g d", g=2), r=["pb4"], w=["vslaug"], e="act")
                                elif nm == "gt":
                                    V(lambda e: e.activation(out=gsig[:, a, :], in_=src[:, 0:24], func=AF.Sigmoid), r=["pb4"], w=["gsig"], e="act")
                                else:
                                    ui = int(nm[1])
                                    CP(utm[:, a, 128 * ui:128 * ui + 128], src, r=["pb4"], w=["utmB"], e="act")
                S.barrier()

                if DBG.get("stopB1"):
                    continue
                with ExitStack() as s2:
                    sb2 = lambda name, shape, dt: sb(name, shape, dt, s2)
                    Fb = sb2("Fb", [128, 8192], BF16)
                    OV = sb2("OV", [128, 8, 256], BF16)
                    DMA(Fb[:], Fbase_d, w=["Fb"])
                    DMA(OV[:], OV_d, w=["OV"])
                    Eb = [sb2(f"Eb{i}", [128, 512], BF16) for i in range(3)]
                    cmall = sb2("cmall", [128, 8, 128], BF16)
                    negT4 = sb2("negT4", [128, 2, 512], BF16)
                    negsel = sb2("negsel", [128, 256], BF16)
                    impq = sb2("impq", [128, 256], F32)
                    w1_ = sb2("w1_", [128, 256], F32)
                    w2_ = sb2("w2_", [128, 256], F32)
                    w3_ = sb2("w3_", [128, 256], F32)
                    valid_ = sb2("valid_", [128, 256], F32)
                    local_ = sb2("local_", [128, 256], F32)
                    m8a = sb2("m8a", [128, 8], F32)
                    m8b = sb2("m8b", [128, 8], F32)
                    rz = sb2("rz", [128, 3, 4], F32)
                    coef = sb2("coef", [128, 3, 4], F32)
                    atmp = sb2("atmp", [128, 64], F32)
                    ectr = [0]

                    def next_E():
                        i = ectr[0] % 3
                        ectr[0] += 1
                        return Eb[i], f"Eb{i}"

                    sctr = [0]

                    def next_S():
                        i = (0, 1, 7)[sctr[0] % 3]
                        sctr[0] += 1
                        return pbank[i], f"pb{i}"

                    def acc_view(bank):
                        return pbank[bank][:].rearrange("p (h x) -> p h x", h=4)

                    for qb in range(4):
                        col = 4 * jt + qb
                        tsl = slice(128 * qb, 128 * qb + 128)
                        for m in range(8):
                            V(lambda e: e.tensor_scalar(out=cmall[:, m, :], in0=tposBt[:, tsl], scalar1=cmpend[:, m:m + 1], scalar2=None, op0=ALU.is_ge),
                              r=["tposBt", "cmpend"], w=["cmall"])
                        V(lambda e: e.tensor_scalar(out=valid_[:], in0=blk64[:], scalar1=tpos[:, col:col + 1], scalar2=None, op0=ALU.is_le),
                          r=["blk64", "tpos"], w=["valid_"])
                        V(lambda e: e.tensor_scalar(out=local_[:], in0=blk64[:], scalar1=tposm[:, col:col + 1], scalar2=None, op0=ALU.is_gt),
                          r=["blk64", "tposm"], w=["local_"])
                        V(lambda e: e.tensor_tensor(out=local_[:], in0=local_[:], in1=valid_[:], op=ALU.mult), r=["local_", "valid_"], w=["local_"])
                        for g in range(2):
                            qrhs = qTz[:, g, :, tsl]
                            def run_pass(items):
                                def issue_S(it):
                                    sbk, skey = next_S()
                                    it["s"](sbk, skey)
                                    return sbk, skey
                                pend = [issue_S(items[0])]
                                if len(items) > 1:
                                    pend.append(issue_S(items[1]))
                                for i, it in enumerate(items):
                                    if i + 2 < len(items):
                                        pend.append(issue_S(items[i + 2]))
                                    sbk, skey = pend.pop(0)
                                    E_, ek = next_E()
                                    V(lambda e: e.activation(out=E_[:], in_=sbk[:], func=AF.Exp, scale=0.125), r=[skey], w=[ek], e="act")
                                    if it.get("post"):
                                        it["post"](E_, ek)
                                    it["pv"](E_, ek)
                                    if it.get("bg"):
                                        bg_step(1)

                            def mk_cmp(m):
                                def s_(sbk, skey):
                                    MM(sbk[:], kcT[:, 128 * m:128 * m + 128], qrhs, True, True, r=["kcT", "qTz"], w=[skey])

                                def post(E_, ek):
                                    E3 = E_[:].rearrange("p (h t) -> p h t", h=4)
                                    V(lambda e: e.tensor_tensor(out=E3, in0=E3, in1=cmall[:, m, :].unsqueeze(1).to_broadcast([128, 4, 128]), op=ALU.mult),
                                      r=[ek, "cmall"], w=[ek])

                                def pv(E_, ek):
                                    for hl in range(4):
                                        MM(pbank[2][:, hl * 128:hl * 128 + 65], E_[:, hl * 128:(hl + 1) * 128], vcaug[:, m, g, :], m == 0 and hl == 0, m == 7,
                                           r=[ek, "vcaug"], w=["pb2"])
                                    for hl in range(4):
                                        bk = 3 + hl // 2
                                        MM(pbank[bk][:, (hl % 2) * 256:(hl % 2) * 256 + 256], E_[:, hl * 128:(hl + 1) * 128], OV[:, m, :], m == 0 and hl % 2 == 0, m == 7,
                                           r=[ek, "OV"], w=[f"pb{bk}"])
                                return dict(s=s_, post=post, pv=pv)

                            run_pass([mk_cmp(m) for m in range(8)])
                            V(lambda e: e.tensor_scalar(out=rz[:, 0, :], in0=acc_view(2)[:, :, 64], scalar1=1e-30, scalar2=None, op0=ALU.max),
                              r=["pb2"], w=["rz0"])
                            V(lambda e: e.reciprocal(out=rz[:, 0, :], in_=rz[:, 0, :]), r=["rz0"], w=["rz0"])
                            for hl in range(4):
                                bk = 3 + hl // 2
                                src = pbank[bk][:, (hl % 2) * 256:(hl % 2) * 256 + 256]
                                if hl == 0:
                                    V(lambda e: e.tensor_scalar(out=impq[:], in0=src, scalar1=rz[:, 0, 0:1], scalar2=None, op0=ALU.mult),
                                      r=[f"pb{bk}", "rz0"], w=["impq"])
                                else:
                                    V(lambda e: e.scalar_tensor_tensor(out=impq[:], in0=src, scalar=rz[:, 0, hl:hl + 1], in1=impq[:],
                                                                       op0=ALU.mult, op1=ALU.add), r=[f"pb{bk}", "rz0", "impq"], w=["impq"])
                            V(lambda e: e.tensor_scalar(out=w1_[:], in0=valid_[:], scalar1=-1.0, scalar2=1e30, op0=ALU.add, op1=ALU.mult), r=["valid_"], w=["w1_"])
                            V(lambda e: e.tensor_tensor(out=w2_[:], in0=impq[:], in1=valid_[:], op=ALU.mult), r=["impq", "valid_"], w=["w2_"])
                            V(lambda e: e.tensor_tensor(out=w2_[:], in0=w2_[:], in1=w1_[:], op=ALU.add), r=["w2_", "w1_"], w=["w2_"])
                            V(lambda e: e.tensor_scalar(out=w1_[:], in0=local_[:], scalar1=1e9, scalar2=None, op0=ALU.mult), r=["local_"], w=["w1_"])
                            V(lambda e: e.memset(w1_[:, 0:1], 1e9), r=[], w=["w1_"])
                            V(lambda e: e.tensor_tensor(out=w2_[:], in0=w2_[:], in1=w1_[:], op=ALU.max), r=["w2_", "w1_"], w=["w2_"])
                            V(lambda e: e.max(out=m8a[:], in_=w2_[:]), r=["w2_"], w=["m8a"])
                            V(lambda e: e.match_replace(out=w3_[:], in_to_replace=m8a[:], in_values=w2_[:], imm_value=-3e38), r=["w2_", "m8a"], w=["w3_"])
                            V(lambda e: e.max(out=m8b[:], in_=w3_[:]), r=["w3_"], w=["m8b"])
                            V(lambda e: e.tensor_scalar(out=w3_[:], in0=w2_[:], scalar1=m8b[:, 7:8], scalar2=None, op0=ALU.is_ge), r=["w2_", "m8b"], w=["w3_"])
                            V(lambda e: e.tensor_tensor(out=w3_[:], in0=w3_[:], in1=valid_[:], op=ALU.mult), r=["w3_", "valid_"], w=["w3_"])
                            V(lambda e: e.tensor_scalar(out=w1_[:], in0=local_[:], scalar1=-1.0, scalar2=-1.0, op0=ALU.add, op1=ALU.mult), r=["local_"], w=["w1_"])
                            V(lambda e: e.tensor_tensor(out=w3_[:], in0=w3_[:], in1=w1_[:], op=ALU.mult), r=["w3_", "w1_"], w=["w3_"])
                            V(lambda e: e.tensor_scalar(out=negsel[:], in0=w3_[:], scalar1=-1.0, scalar2=1e4, op0=ALU.add, op1=ALU.mult), r=["w3_"], w=["negsel"])
                            pbb7 = pbank[7][:].bitcast(BF16)
                            for hf in range(2):
                                S.op("pe", lambda e: e.transpose(out=pbb7[:, hf * 128:(hf + 1) * 128], in_=negsel[:, hf * 128:(hf + 1) * 128], identity=identb[:]),
                                     reads=["negsel", "identb"], writes=["pb7"])
                            for hf in range(2):
                                CP(negT4[:, hf, :].rearrange("p (h t) -> p h t", h=4),
                                   pbb7[:, hf * 128:(hf + 1) * 128].unsqueeze(1).to_broadcast([128, 4, 128]), r=["pb7"], w=["negT4"])
                            J = 4 * (8 * jt + 7) + qb + 1
                            J = min(J, DBG.get("maxJ", 1000))

                            def mk_win(c5):
                                c0 = 128 * qb + 128 * c5

                                def s_(sbk, skey):
                                    MM(sbk[:], kwT[:, c0:c0 + 128], qrhs, True, True, r=["kwT", "qTz"], w=[skey])

                                def post(E_, ek):
                                    if c5 in (0, 4):
                                        msk, mk = (mWM0, "mWM0") if c5 == 0 else (mTRI, "mTRI")
                                        E3 = E_[:].rearrange("p (h t) -> p h t", h=4)
                                        V(lambda e: e.tensor_tensor(out=E3, in0=E3, in1=msk[:].unsqueeze(1).to_broadcast([128, 4, 128]), op=ALU.mult), r=[ek, mk], w=[ek])
                                    V(lambda e: e.tensor_scalar(out=E_[:], in0=E_[:], scalar1=validW[:, col, c5:c5 + 1], scalar2=None, op0=ALU.mult),
                                      r=[ek, "validW"], w=[ek])

                                def pv(E_, ek):
                                    for hl in range(4):
                                        MM(pbank[6][:, hl * 128:hl * 128 + 65], E_[:, hl * 128:(hl + 1) * 128], vwaug[:, qb + c5, g, :], c5 == 0 and hl == 0, c5 == 4,
                                           r=[ek, "vwaug"], w=["pb6"])
                                return dict(s=s_, post=post, pv=pv)

                            def mk_loc(ci):
                                c0, msk, mk = ((128 * qb, mLMA, "mLMA"), (128 * qb + 128, mTRI, "mTRI"))[ci]

                                def s_(sbk, skey):
                                    MM(sbk[:], kslocT[:, c0:c0 + 128], qrhs, True, True, r=["kslocT", "qTz"], w=[skey])

                                def post(E_, ek):
                                    E3 = E_[:].rearrange("p (h t) -> p h t", h=4)
                                    V(lambda e: e.tensor_tensor(out=E3, in0=E3, in1=msk[:].unsqueeze(1).to_broadcast([128, 4, 128]), op=ALU.mult), r=[ek, mk], w=[ek])
                                    if ci == 0:
                                        V(lambda e: e.tensor_scalar(out=E_[:], in0=E_[:], scalar1=validA[:, col:col + 1], scalar2=None, op0=ALU.mult),
                                          r=[ek, "validA"], w=[ek])

                                def pv(E_, ek):
                                    for hl in range(4):
                                        MM(pbank[5][:, hl * 128:hl * 128 + 65], E_[:, hl * 128:(hl + 1) * 128], vslaug[:, qb + ci, g, :], ci == 0 and hl == 0, False,
                                           r=[ek, "vslaug"], w=["pb5"])
                                return dict(s=s_, post=post, pv=pv)

                            def mk_sel(j):
                                def s_(sbk, skey):
                                    MM(sbk[:], ksT[:, 128 * j:128 * j + 128], qrhs, True, False, r=["ksT", "qTz"], w=[skey])
                                    MM(sbk[:], Fb[:, 128 * (j % 64):128 * (j % 64) + 128], negT4[:, j // 64, :], False, True, r=["Fb", "negT4"], w=[skey])

                                def pv(E_, ek):
                                    for hl in range(4):
                                        MM(pbank[5][:, hl * 128:hl * 128 + 65], E_[:, hl * 128:(hl + 1) * 128], vsaug[:, j, g, :], False, j == J - 1,
                                           r=[ek, "vsaug"], w=["pb5"])
                                return dict(s=s_, post=None, pv=pv, bg=True)

                            run_pass([mk_win(c5) for c5 in range(5)] + [mk_loc(ci) for ci in range(2)] + [mk_sel(j) for j in range(J)])
                            for jb, bank in ((1, 5), (2, 6)):
                                V(lambda e: e.tensor_scalar(out=rz[:, jb, :], in0=acc_view(bank)[:, :, 64], scalar1=1e-30, scalar2=None, op0=ALU.max),
                                  r=[f"pb{bank}"], w=[f"rz{jb}"])
                                V(lambda e: e.reciprocal(out=rz[:, jb, :], in_=rz[:, jb, :]), r=[f"rz{jb}"], w=[f"rz{jb}"])
                            gv = gsig[:, qb, :].rearrange("p (h j) -> p j h", j=3)
                            V(lambda e: e.tensor_tensor(out=coef[:], in0=rz[:], in1=gv[:, :, 4 * g:4 * g + 4], op=ALU.mult),
                              r=["rz0", "rz1", "rz2", "gsig"], w=["coef"])
                            for hl in range(4):
                                h_ = 4 * g + hl
                                V(lambda e: e.tensor_scalar(out=atmp[:], in0=pbank[2][:, hl * 128:hl * 128 + 64], scalar1=coef[:, 0, hl:hl + 1], scalar2=None, op0=ALU.mult),
                                  r=["pb2", "coef"], w=["atmp"])
                                V(lambda e: e.scalar_tensor_tensor(out=atmp[:], in0=pbank[5][:, hl * 128:hl * 128 + 64], scalar=coef[:, 1, hl:hl + 1], in1=atmp[:],
                                                                   op0=ALU.mult, op1=ALU.add), r=["pb5", "coef", "atmp"], w=["atmp"])
                                V(lambda e: e.scalar_tensor_tensor(out=aout[:, qb, 64 * h_:64 * h_ + 64], in0=pbank[6][:, hl * 128:hl * 128 + 64], scalar=coef[:, 2, hl:hl + 1],
                                                                   in1=atmp[:], op0=ALU.mult, op1=ALU.add), r=["pb6", "coef", "atmp"], w=["aout"])
                S.barrier()
                if debug == "B2":
                    def doutt(name, shape, dt):
                        dbg_out[name] = nc.dram_tensor("dbg_" + name, list(shape), dt, kind="ExternalOutput").ap()
                        return dbg_out[name]
                    DMA(doutt(f"aout{jt}", [128, 2048], BF16), aout[:].rearrange("p a b -> p (a b)"), r=["aout"])
                    DMA(doutt(f"qTz{jt}", [128, 4096], BF16), qTz[:].rearrange("p a b c -> p (a b c)"), r=["qTz"])
                    DMA(doutt(f"kwT{jt}", [128, 1024], BF16), kwT[:], r=["kwT"])
                    DMA(doutt(f"kslocT{jt}", [128, 640], BF16), kslocT[:], r=["kslocT"])
                    DMA(doutt(f"gsig{jt}", [128, 96], F32), gsig[:].rearrange("p a b -> p (a b)"), r=["gsig"])
                    DMA(doutt(f"utm{jt}", [128, 2048], BF16), utm[:].rearrange("p a b -> p (a b)"), r=["utmB"])
                    S.barrier()

                if DBG.get("stopB2"):
                    continue
                with ExitStack() as s3:
                    sb3 = lambda name, shape, dt: sb(name, shape, dt, s3)
                    cosT = sb3("cosT", [128, 16, 128], F32)
                    sinT = sb3("sinT", [128, 16, 128], F32)
                    BbT = sb3("BbT", [128, 2, 16, 128], BF16)
                    Cp = sb3("Cp", [128, 2, 16, 32], BF16)
                    dsk = sb3("dsk", [128, 512], F32)
                    wglu = sb3("wglu", [128, 4, 1024], BF16)
                    rho = sb3("rho", [128, 16], F32)
                    z0 = sb3("z0", [128, 32], F32)
                    with ExitStack() as s3a:
                        sb3a = lambda name, shape, dt: sb(name, shape, dt, s3a)
                        ti_ = sb3a("ti_", [128, 1024], I32)
                        ta_ = sb3a("ta_", [128, 1024], F32)
                        tb_ = sb3a("tb_", [128, 1024], F32)
                        ang = sb3a("ang", [128, 1024], F32)
                        trw = sb3a("trw", [128, 128], F32)
                        Bexp = sb3a("Bexp", [128, 2, 16, 128], BF16)
                        cst = sb3a("cst", [128, 16, 16], F32)
                        ohs = sb3a("ohs", [128, NT], F32)
                        tmpX = sb3a("tmpX", [128, 32, NT], F32)
                        DMA(trw[:], trow, w=["trw"])
                        DMA(dsk[:], dskip_d, w=["dsk"])
                        DMA(ohs[:], onehot[:, jt, :], w=["ohs"])

                        def sincos3(out_ap, n, shift, key_out):
                            A_, B_, I_ = ta_[:, 0:n], tb_[:, 0:n], ti_[:, 0:n]
                            V(lambda e: e.tensor_scalar(out=A_, in0=ang[:, 0:n], scalar1=shift, scalar2=1.0 / (2 * PI), op0=ALU.add, op1=ALU.mult), r=["ang"], w=["ta_"])
                            V(lambda e: e.tensor_copy(out=I_, in_=A_), r=["ta_"], w=["ti_"])
                            V(lambda e: e.tensor_copy(out=B_, in_=I_), r=["ti_"], w=["tb_"])
                            V(lambda e: e.tensor_tensor(out=A_, in0=A_, in1=B_, op=ALU.subtract), r=["ta_", "tb_"], w=["ta_"])
                            V(lambda e: e.tensor_scalar(out=B_, in0=A_, scalar1=0.5, scalar2=None, op0=ALU.is_gt), r=["ta_"], w=["tb_"])
                            V(lambda e: e.tensor_tensor(out=A_, in0=A_, in1=B_, op=ALU.subtract), r=["ta_", "tb_"], w=["ta_"])
                            V(lambda e: e.tensor_scalar(out=B_, in0=A_, scalar1=-0.5, scalar2=None, op0=ALU.is_lt), r=["ta_"], w=["tb_"])
                            V(lambda e: e.tensor_tensor(out=A_, in0=A_, in1=B_, op=ALU.add), r=["ta_", "tb_"], w=["ta_"])
                            V(lambda e: e.activation(out=out_ap, in_=A_, func=AF.Sin, scale=2 * PI), r=["ta_"], w=[key_out], e="act")

                        for hh in range(2):
                            ks8 = slice(8 * hh, 8 * hh + 8)
                            V(lambda e: e.tensor_tensor(out=ang[:].rearrange("p (k t) -> p k t", k=8),
                                                        in0=thf[:, ks8].unsqueeze(2).to_broadcast([128, 8, 128]),
                                                        in1=trw[:].unsqueeze(1).to_broadcast([128, 8, 128]), op=ALU.mult), r=["thf", "trw"], w=["ang"])
                            sincos3(cosT[:, ks8, :].rearrange("p k t -> p (k t)"), 1024, PI / 2, "cosT")
                            sincos3(sinT[:, ks8, :].rearrange("p k t -> p (k t)"), 1024, 0.0, "sinT")
                        V(lambda e: e.activation(out=rho[:], in_=af[:], func=AF.Exp), r=["af"], w=["rho"], e="act")
                        V(lambda e: e.memset(Bexp[:], 0.0), w=["Bexp"], e="pool")
                        Bb5 = Bbar[:].rearrange("p r (a b) c -> p r a b c", b=4)
                        Be5 = Bexp[:].rearrange("p r (a b) x -> p r a b x", b=4)
                        for glo in range(2):
                            ps_ = slice(64 * glo, 64 * glo + 64)
                            for k4 in range(4):
                                V(lambda e: e.tensor_copy(out=Be5[ps_, :, :, k4, 32 * k4 + 16 * glo:32 * k4 + 16 * glo + 16], in_=Bb5[ps_, :, :, k4, :]),
                                  r=["Bbr", "Bbi", "Bexp"], w=["Bexp"])
                        for ri in range(2):
                            for k8 in range(2):
                                pbb = pbank[ri * 2 + k8][:].bitcast(BF16)
                                for kk in range(8):
                                    S.op("pe", lambda e: e.transpose(out=pbb[:, kk * 128:(kk + 1) * 128], in_=Bexp[:, ri, 8 * k8 + kk, :], identity=identb[:]),
                                         reads=["Bexp", "identb"], writes=[f"pb{ri * 2 + k8}"])
                                CP(BbT[:, ri, 8 * k8:8 * k8 + 8, :].rearrange("p k q -> p (k q)"), pbb, r=[f"pb{ri * 2 + k8}"], w=["BbT"])
                        V(lambda e: e.memset(Cp[:], 0.0), w=["Cp"], e="pool")
                        for ri, nm in ((0, "cre_f"), (1, "cim_f")):
                            DMA(cst[:], ssm_b[nm], w=["cst"])
                            for glo in range(2):
                                ps_ = slice(64 * glo, 64 * glo + 64)
                                V(lambda e: e.tensor_scalar(out=Cp[ps_, ri, :, 16 * glo:16 * glo + 16], in0=cst[ps_], scalar1=(1.0 if ri == 0 else -1.0),
                                                            scalar2=None, op0=ALU.mult), r=["cst", "Cp"], w=["Cp"])
                        for c4 in range(4):
                            st_ = wstB[c4 % 2]
                            stv = st_[:].rearrange("p a b -> p (a b)").rearrange("p (c n) -> p c n", c=4)
                            DMA(stv, wglu_d[:, 256 * c4:256 * c4 + 256].rearrange("(c p) n -> p c n", p=128), w=[f"wstB{c4 % 2}"])
                            CP(wglu[:, :, 256 * c4:256 * c4 + 256], stv, r=[f"wstB{c4 % 2}"], w=["wglu"])
                        V(lambda e: e.tensor_tensor(out=tmpX[:], in0=Xtile[:].rearrange("p t c -> p c t"),
                                                    in1=ohs[:].unsqueeze(1).to_broadcast([128, 32, NT]), op=ALU.mult), r=["Xtile", "ohs"], w=["tmpX"])
                        V(lambda e: e.tensor_reduce(out=z0[:], in_=tmpX[:], axis=AX.X, op=ALU.add), r=["tmpX"], w=["z0"])
                        S.barrier()
                    wr = sb3("wr", [128, 4, 128], F32)
                    wi = sb3("wi", [128, 4, 128], F32)
                    q1 = sb3("q1", [128, 4, 128], F32)
                    q2 = sb3("q2", [128, 4, 128], F32)
                    zr = sb3("zr", [128, 4, 128], F32)
                    zi = sb3("zi", [128, 4, 128], F32)
                    xr = sb3("xr", [128, 4, 128], F32)
                    xi = sb3("xi", [128, 4, 128], F32)
                    xrb = sb3("xrb", [128, 4, 128], BF16)
                    xib = sb3("xib", [128, 4, 128], BF16)
                    ypre = sb3("ypre", [128, 512], F32)
                    ygb = sb3("ygb", [128, 512], BF16)
                    ygT = sb3("ygT", [128, 4, 128], BF16)
                    sg = sb3("sg", [128, 512], F32)
                    fl4 = lambda t: t[:].rearrange("p k t -> p (k t)")
                    for qb in range(4):
                        tsl = slice(128 * qb, 128 * qb + 128)
                        for qq in range(4):
                            k4s = slice(4 * qq, 4 * qq + 4)
                            cq = cosT[:, k4s, :].rearrange("p k t -> p (k t)")
                            sq = sinT[:, k4s, :].rearrange("p k t -> p (k t)")
                            for ri in range(2):
                                for kk in range(4):
                                    MM(pbank[ri][:, kk * 128:(kk + 1) * 128], BbT[:, ri, 4 * qq + kk, :], uT[:, qq, tsl], True, True, r=["BbT", "uT"], w=[f"pb{ri}"])
                            V(lambda e: e.tensor_tensor(out=fl4(wr), in0=pbank[0][:], in1=cq, op=ALU.mult), r=["pb0", "cosT"], w=["wr"])
                            V(lambda e: e.tensor_tensor(out=fl4(q1), in0=pbank[1][:], in1=sq, op=ALU.mult), r=["pb1", "sinT"], w=["q1"])
                            V(lambda e: e.tensor_tensor(out=fl4(wi), in0=pbank[1][:], in1=cq, op=ALU.mult), r=["pb1", "cosT"], w=["wi"])
                            V(lambda e: e.tensor_tensor(out=fl4(q2), in0=pbank[0][:], in1=sq, op=ALU.mult), r=["pb0", "sinT"], w=["q2"])
                            V(lambda e: e.tensor_tensor(out=fl4(wr), in0=fl4(wr), in1=fl4(q1), op=ALU.add), r=["wr", "q1"], w=["wr"], e="pool")
                            V(lambda e: e.tensor_tensor(out=fl4(wi), in0=fl4(wi), in1=fl4(q2), op=ALU.subtract), r=["wi", "q2"], w=["wi"], e="pool")
                            for kk in range(4):
                                k = 4 * qq + kk
                                V(lambda e: e.tensor_tensor_scan(out=zr[:, kk, :], data0=rho[:, k:k + 1].to_broadcast([128, 128]), data1=wr[:, kk, :],
                                                                 initial=z0[:, k:k + 1], op0=ALU.mult, op1=ALU.add), r=["rho", "wr", "z0"], w=["zr"])
                                V(lambda e: e.tensor_tensor_scan(out=zi[:, kk, :], data0=rho[:, k:k + 1].to_broadcast([128, 128]), data1=wi[:, kk, :],
                                                                 initial=z0[:, 16 + k:17 + k], op0=ALU.mult, op1=ALU.add), r=["rho", "wi", "z0"], w=["zi"])
                            V(lambda e: e.tensor_tensor(out=fl4(xr), in0=fl4(zr), in1=cq, op=ALU.mult), r=["zr", "cosT"], w=["xr"])
                            V(lambda e: e.tensor_tensor(out=fl4(q1), in0=fl4(zi), in1=sq, op=ALU.mult), r=["zi", "sinT"], w=["q1"], e="pool")
                            V(lambda e: e.tensor_tensor(out=fl4(xr), in0=fl4(xr), in1=fl4(q1), op=ALU.subtract), r=["xr", "q1"], w=["xr"])
                            V(lambda e: e.tensor_tensor(out=fl4(xi), in0=fl4(zr), in1=sq, op=ALU.mult), r=["zr", "sinT"], w=["xi"], e="pool")
                            V(lambda e: e.tensor_tensor(out=fl4(q2), in0=fl4(zi), in1=cq, op=ALU.mult), r=["zi", "cosT"], w=["q2"])
                            V(lambda e: e.tensor_tensor(out=fl4(xi), in0=fl4(xi), in1=fl4(q2), op=ALU.add), r=["xi", "q2"], w=["xi"], e="pool")
                            CP(fl4(xrb), fl4(xr), r=["xr"], w=["xrb"], e="act")
                            CP(fl4(xib), fl4(xi), r=["xi"], w=["xib"], e="act")
                            V(lambda e: e.tensor_copy(out=z0[:, k4s], in_=xr[:, :, 127]), r=["xr", "z0"], w=["z0"])
                            V(lambda e: e.tensor_copy(out=z0[:, 16 + 4 * qq:20 + 4 * qq], in_=xi[:, :, 127]), r=["xi", "z0"], w=["z0"])
                            for kk in range(4):
                                k = 4 * qq + kk
                                MM(pbank[4][:, 32 * k:32 * k + 32], xrb[:, kk, :], Cp[:, 0, k, :], qq == 0 and kk == 0, False, r=["xrb", "Cp"], w=["pb4"])
                                MM(pbank[4][:, 32 * k:32 * k + 32], xib[:, kk, :], Cp[:, 1, k, :], False, True, r=["xib", "Cp"], w=["pb4"])
                        V(lambda e: e.tensor_tensor(out=sg[:], in0=utm[:, qb, :], in1=dsk[:], op=ALU.mult), r=["utmB", "dsk"], w=["sg"], e="pool")
                        V(lambda e: e.tensor_tensor(out=ypre[:], in0=pbank[4][:], in1=sg[:], op=ALU.add), r=["pb4", "sg"], w=["ypre"])
                        V(lambda e: e.activation(out=ygb[:], in_=ypre[:], func=AF.Gelu_apprx_tanh), r=["ypre"], w=["ygb"], e="act")
                        pbb5 = pbank[5][:].bitcast(BF16)
                        for cc in range(4):
                            S.op("pe", lambda e: e.transpose(out=pbb5[:, cc * 128:(cc + 1) * 128], in_=ygb[:, cc * 128:(cc + 1) * 128], identity=identb[:]),
                                 reads=["ygb", "identb"], writes=["pb5"])
                        CP(ygT[:].rearrange("p c t -> p (c t)"), pbb5[:, 0:512], r=["pb5"], w=["ygT"])
                        for half in range(2):
                            for cc in range(4):
                                MM(pbank[6 + half][:], ygT[:, cc, :], wglu[:, cc, half * 512:(half + 1) * 512], cc == 0, cc == 3, r=["ygT", "wglu"], w=[f"pb{6 + half}"])
                        V(lambda e: e.activation(out=sg[:], in_=pbank[7][:], func=AF.Sigmoid), r=["pb7"], w=["sg"], e="act")
                        V(lambda e: e.tensor_tensor(out=sout[:, qb, :], in0=pbank[6][:], in1=sg[:], op=ALU.mult), r=["pb6", "sg"], w=["sout"])
                S.barrier()
                with ExitStack() as s4:
                    sb4 = lambda name, shape, dt: sb(name, shape, dt, s4)
                    wout = sb4("wout", [128, 8, 1024], BF16)
                    mixT = sb4("mixT", [128, 8, 128], BF16)
                    xrow = [sb4(f"xrow{i}", [128, D], F32) for i in range(2)]
                    h1t = [sb4(f"h1t{i}", [128, D], F32) for i in range(2)]
                    for c8 in range(8):
                        st_ = wstB[c8 % 2]
                        DMA(st_[:], wout_d[:, 128 * c8:128 * c8 + 128].rearrange("(c p) n -> p c n", p=128), w=[f"wstB{c8 % 2}"])
                        CP(wout[:, :, 128 * c8:128 * c8 + 128], st_[:], r=[f"wstB{c8 % 2}"], w=["wout"], e=("act" if c8 % 2 else "dve"))
                    for qb in range(4):
                        i2 = qb % 2
                        DMA(xrow[i2][:], x_loc[jt, 512 + 128 * qb:512 + 128 * qb + 128, :], w=[f"xrow{i2}"])
                        pbb5 = pbank[5][:].bitcast(BF16)
                        for cc in range(8):
                            src = aout if cc < 4 else sout
                            kx = "aout" if cc < 4 else "sout"
                            S.op("pe", lambda e: e.transpose(out=pbb5[:, cc * 128:(cc + 1) * 128], in_=src[:, qb, (cc % 4) * 128:(cc % 4) * 128 + 128], identity=identb[:]),
                                 reads=[kx, "identb"], writes=["pb5"])
                        CP(mixT[:].rearrange("p c t -> p (c t)"), pbb5, r=["pb5"], w=["mixT"])
                        for half in range(2):
                            for cc in range(8):
                                MM(pbank[6 + half][:], mixT[:, cc, :], wout[:, cc, half * 512:(half + 1) * 512], cc == 0, cc == 7, r=["mixT", "wout"], w=[f"pb{6 + half}"])
                            V(lambda e: e.tensor_tensor(out=h1t[i2][:, half * 512:(half + 1) * 512], in0=pbank[6 + half][:], in1=xrow[i2][:, half * 512:(half + 1) * 512], op=ALU.add),
                              r=[f"pb{6 + half}", f"xrow{i2}"], w=[f"h1t{i2}"])
                        DMA(h1d[4 * jt + qb], h1t[i2][:], r=[f"h1t{i2}"], w=["h1d"])
                    if debug == "B4":
                        def doutt(name, shape, dt):
                            dbg_out[name] = nc.dram_tensor("dbg_" + name, list(shape), dt, kind="ExternalOutput").ap()
                            return dbg_out[name]
                        DMA(doutt(f"aout{jt}", [128, 2048], BF16), aout[:].rearrange("p a b -> p (a b)"), r=["aout"])
                        DMA(doutt(f"sout{jt}", [128, 2048], BF16), sout[:].rearrange("p a b -> p (a b)"), r=["sout"])
                        DMA(doutt(f"gsig{jt}", [128, 96], F32), gsig[:].rearrange("p a b -> p (a b)"), r=["gsig"])
                        DMA(doutt(f"utm{jt}", [128, 2048], BF16), utm[:].rearrange("p a b -> p (a b)"), r=["utmB"])
                        dh = doutt(f"h1{jt}", [4, 128, D], F32)
                        for qb in range(4):
                            DMA(xrow[0][:], h1d[4 * jt + qb], r=["h1d"], w=["xrow0"])
                            DMA(dh[qb], xrow[0][:], r=["xrow0"])
                S.barrier()

            while bg:
                bg_step(1)
        S.barrier()
        sctx.close()
        if not DBG.get("noC"):
          with ExitStack() as sC:
            sbc = lambda name, shape, dt: sb(name, shape, dt, sC)
            wq = sbc("wq", [128, 8, 2048], BF16)
            subT = sbc("subT", [128, 2, 128], BF16)
            gffn = sbc("gffn", [128, D], F32)
            gfin = sbc("gfin", [128, D], F32)
            wstC = [sbc(f"wstC{i}", [128, 8, 128], F32) for i in range(2)]
            DMA(gffn[:], gffn_d.partition_broadcast(128), w=["gffn"])
            DMA(gfin[:], fnorm.partition_broadcast(128), w=["gfin"])
            for c16 in range(16):
                st_ = wstC[c16 % 2]
                DMA(st_[:], wq_d[:, 128 * c16:128 * c16 + 128].rearrange("(c p) n -> p c n", p=128), w=[f"wstC{c16 % 2}"])
                CP(wq[:, :, 128 * c16:128 * c16 + 128], st_[:], r=[f"wstC{c16 % 2}"], w=["wq"], e=("act" if c16 % 2 else "dve"))
            for i_, d_ in enumerate((sub1T_d, sub2T_d)):
                DMA(wstC[0][:, 0, :], d_, w=["wstC0"])
                CP(subT[:, i_, :], wstC[0][:, 0, :], r=["wstC0"], w=["subT"])
            hb = [sbc(f"hb{i}", [128, D], F32) for i in range(2)]
            xn2 = [sbc(f"xn{i}", [128, D], F32) for i in range(2)]
            xnb = sbc("xnb", [128, D], BF16)
            xnT = sbc("xnT", [128, 8, 128], BF16)
            qTb = sbc("qTb", [128, 16, 128], BF16)
            sc = sbc("sc", [128, 16, 128], F32)
            scr = sbc("scr", [128, 128], F32)
            vtop = sbc("vtop", [128, 16, 16], F32)
            itop = sbc("itop", [128, 16, 16], U32)
            itf = sbc("itf", [128, 16, 16], F32)
            cand = sbc("cand", [128, 256], F32)
            cand2 = sbc("cand2", [128, 256], F32)
            cidx = sbc("cidx", [128, 256], F32)
            cjk = sbc("cjk", [128, 256], F32)
            top = sbc("top", [128, 8, 16], F32)
            eidf = sbc("eidf", [128, 128], F32)
            eidx2 = [sbc(f"eidx{i}", [128, 128], I32) for i in range(2)]
            gate2 = [sbc(f"gate{i}", [128, 8, 16], F32) for i in range(2)]
            ntop = sbc("ntop", [128, 8], F32)
            zs = sbc("zs", [128, 8], F32)
            hid = sbc("hid", [128, 128], F32)
            actw = sbc("actw", [128, 128], F32)
            ssc = sbc("ssc", [128, 2], F32)
            rsc = sbc("rsc", [128, 2], F32)
            gbuf = [sbc(f"gbuf{i}", [128, 2 * D], BF16) for i in range(NGB)]
            gjk = sbc("gjk", [128, D], F32)
            acc = sbc("acc", [128, D], F32)
            scb = [sbc(f"scb{i}", [128, D], BF16) for i in range(4)]
            otile = sbc("otile", [128, D], F32)
            gctr = [0]
            NQ = DBG.get("nqC", 16)

            def frontend(qi):
                p = qi % 2
                h_, hk_ = hb[p], f"hb{p}"
                xn, xk = xn2[p], f"xn{p}"
                eidx, ek_ = eidx2[p], f"eidx{p}"
                gate, gk = gate2[p], f"gate{p}"
                DMA(h_[:], h1d[qi], r=["h1d"], w=[hk_])
                V(lambda e: e.activation(out=xn[:], in_=h_[:], func=AF.Square, accum_out=ssc[:, 0:1]), r=[hk_], w=[xk, "ssc0"], e="act")
                V(lambda e: e.activation(out=rsc[:, 0:1], in_=ssc[:, 0:1], func=AF.Sqrt, bias=EPS, scale=1.0 / D), r=["ssc0"], w=["rsc0"], e="act")
                V(lambda e: e.reciprocal(out=rsc[:, 0:1], in_=rsc[:, 0:1]), r=["rsc0"], w=["rsc0"])
                yield
                V(lambda e: e.scalar_tensor_tensor(out=xn[:], in0=h_[:], scalar=rsc[:, 0:1], in1=gffn[:], op0=ALU.mult, op1=ALU.mult),
                  r=[hk_, "rsc0", "gffn"], w=[xk])
                CP(xnb[:], xn[:], r=[xk], w=["xnb"], e="act")
                yield
                pbb = pbank[0][:].bitcast(BF16)
                for dc in range(8):
                    S.op("pe", lambda e: e.transpose(out=pbb[:, dc * 128:(dc + 1) * 128], in_=xnb[:, dc * 128:(dc + 1) * 128], identity=identb[:]),
                         reads=["xnb", "identb"], writes=["pb0"])
                CP(xnT[:].rearrange("p c t -> p (c t)"), pbb, r=["pb0"], w=["xnT"])
                yield
                for b4 in range(4):
                    bank = 1 + b4 % 2
                    for bb in range(4):
                        blk = 4 * b4 + bb
                        for dc in range(8):
                            MM(pbank[bank][:, bb * 128:(bb + 1) * 128], wq[:, dc, blk * 128:(blk + 1) * 128], xnT[:, dc, :], dc == 0, dc == 7,
                               r=["wq", "xnT"], w=[f"pb{bank}"])
                        yield
                    CP(qTb[:, 4 * b4:4 * b4 + 4, :].rearrange("p a b -> p (a b)"), pbank[bank][:], r=[f"pb{bank}"], w=["qTb"], e=("act" if b4 % 2 else "dve"))
                    yield
                for b4 in range(4):
                    bank = 3 + b4 % 2
                    for bb in range(4):
                        blk = 4 * b4 + bb
                        MM(pbank[bank][:, bb * 128:(bb + 1) * 128], qTb[:, blk, :], subT[:, blk % 2, :], True, True, r=["qTb", "subT"], w=[f"pb{bank}"])
                    CP(sc[:, 4 * b4:4 * b4 + 4, :].rearrange("p a b -> p (a b)"), pbank[bank][:], r=[f"pb{bank}"], w=["sc"], e=("act" if b4 % 2 else "dve"))
                    yield
                for blk in range(16):
                    V(lambda e: e.max(out=vtop[:, blk, 0:8], in_=sc[:, blk, :]), r=["sc"], w=["vtop"])
                    V(lambda e: e.max_index(out=itop[:, blk, 0:8], in_max=vtop[:, blk, 0:8], in_values=sc[:, blk, :]), r=["sc", "vtop"], w=["itop"])
                    yield
                    V(lambda e: e.match_replace(out=scr[:], in_to_replace=vtop[:, blk, 0:8], in_values=sc[:, blk, :], imm_value=-1e30), r=["sc", "vtop"], w=["scr"])
                    V(lambda e: e.max(out=vtop[:, blk, 8:16], in_=scr[:]), r=["scr"], w=["vtop"])
                    yield
                    V(lambda e: e.max_index(out=itop[:, blk, 8:16], in_max=vtop[:, blk, 8:16], in_values=scr[:]), r=["scr", "vtop"], w=["itop"])
                    yield
                V(lambda e: e.tensor_copy(out=itf[:], in_=itop[:]), r=["itop"], w=["itf"])
                yield
                for hh in range(8):
                    c3 = cand[:].rearrange("p (a b) -> p a b", a=16)
                    i3 = cidx[:].rearrange("p (a b) -> p a b", a=16)
                    V(lambda e: e.tensor_tensor(out=c3, in0=vtop[:, 2 * hh, :].unsqueeze(2).to_broadcast([128, 16, 16]),
                                                in1=vtop[:, 2 * hh + 1, :].unsqueeze(1).to_broadcast([128, 16, 16]), op=ALU.add), r=["vtop"], w=["cand"])
                    V(lambda e: e.scalar_tensor_tensor(out=i3, in0=itf[:, 2 * hh, :].unsqueeze(2).to_broadcast([128, 16, 16]), scalar=128.0,
                                                       in1=itf[:, 2 * hh + 1, :].unsqueeze(1).to_broadcast([128, 16, 16]), op0=ALU.mult, op1=ALU.add),
                      r=["itf"], w=["cidx"])
                    yield
                    V(lambda e: e.max(out=top[:, hh, 0:8], in_=cand[:]), r=["cand"], w=["top"])
                    V(lambda e: e.match_replace(out=cand2[:], in_to_replace=top[:, hh, 0:8], in_values=cand[:], imm_value=-1e30), r=["cand", "top"], w=["cand2"])
                    V(lambda e: e.max(out=top[:, hh, 8:16], in_=cand2[:]), r=["cand2"], w=["top"])
                    yield
                    for kk in range(16):
                        V(lambda e: e.scalar_tensor_tensor(out=cjk[:], in0=cand[:], scalar=top[:, hh, kk:kk + 1], in1=cidx[:], op0=ALU.is_equal, op1=ALU.mult,
                                                           accum_out=eidf[:, 16 * hh + kk:16 * hh + kk + 1]), r=["cand", "cidx", "top"], w=[f"eidf{16 * hh + kk}"])
                        if kk % 2:
                            yield
                    V(lambda e: e.tensor_scalar(out=ntop[:, hh:hh + 1], in0=top[:, hh, 0:1], scalar1=-1.0, scalar2=None, op0=ALU.mult), r=["top"], w=["ntop"])
                    V(lambda e: e.activation(out=gate[:, hh, :], in_=top[:, hh, :], func=AF.Exp, bias=ntop[:, hh:hh + 1], scale=1.0, accum_out=zs[:, hh:hh + 1]),
                      r=["top", "ntop"], w=[gk, "zs"], e="act")
                    yield
                V(lambda e: e.reciprocal(out=zs[:], in_=zs[:]), r=["zs"], w=["zs"])
                V(lambda e: e.tensor_tensor(out=gate[:], in0=gate[:], in1=zs[:].unsqueeze(2).to_broadcast([128, 8, 16]), op=ALU.mult), r=[gk, "zs"], w=[gk])
                V(lambda e: e.tensor_scalar(out=eidf[:], in0=eidf[:], scalar1=16383.0, scalar2=0.0, op0=ALU.min, op1=ALU.max), r=[f"eidf{i}" for i in range(128)], w=["eidf"])
                V(lambda e: e.tensor_copy(out=eidx[:], in_=eidf[:]), r=["eidf"], w=[ek_])
                yield

            def drain(gen, n=None):
                if gen is None:
                    return
                k = 0
                for _ in gen:
                    k += 1
                    if n is not None and k >= n:
                        return

            drain(frontend(0))
            for qi in range(NQ):
                p = qi % 2
                h_, hk_ = hb[p], f"hb{p}"
                xn, xk = xn2[p], f"xn{p}"
                eidx, ek_ = eidx2[p], f"eidx{p}"
                gate, gk = gate2[p], f"gate{p}"
                fe_next = frontend(qi + 1) if qi + 1 < NQ else None
                gflat = gate[:].rearrange("p a b -> p (a b)")
                for grp in range(16):
                    gl = []
                    for hk in range(8 * grp, 8 * grp + 8):
                        gi = gctr[0] % NGB
                        gctr[0] += 1
                        gb = gbuf[gi]
                        gl.append((hk, gi, gb))
                        S.dma("pool", lambda q: q.indirect_dma_start(out=gb[:], out_offset=None, in_=uvb,
                                                                     in_offset=bass.IndirectOffsetOnAxis(ap=eidx[:, hk:hk + 1], axis=0)), reads=[ek_], writes=[f"gbuf{gi}"])
                        V(lambda e: e.scalar_tensor_tensor(out=gjk[:], in0=gb[:, 0:D], scalar=1.0, in1=xn[:], op0=ALU.mult, op1=ALU.mult, accum_out=hid[:, hk:hk + 1]),
                          r=[f"gbuf{gi}", xk], w=[f"hid{hk}"])
                        drain(fe_next, 2)
                    gs = slice(8 * grp, 8 * grp + 8)
                    V(lambda e: e.activation(out=actw[:, gs], in_=hid[:, gs], func=AF.Gelu_apprx_tanh), r=[f"hid{i}" for i in range(8 * grp, 8 * grp + 8)], w=[f"actw{grp}"], e="act")
                    V(lambda e: e.tensor_tensor(out=actw[:, gs], in0=actw[:, gs], in1=gflat[:, gs], op=ALU.mult), r=[f"actw{grp}", gk], w=[f"actw{grp}"])
                    for (hk, gi, gb) in gl:
                        si = hk % 4
                        V(lambda e: e.activation(out=scb[si][:], in_=gb[:, D:2 * D], func=AF.Copy, scale=actw[:, hk:hk + 1]), r=[f"gbuf{gi}", f"actw{grp}"], w=[f"scb{si}"], e="act")
                        for half in range(2):
                            MM(pbank[6 + half][:], identb[:], scb[si][:, half * 512:(half + 1) * 512], hk == 0, hk == 127, r=[f"scb{si}", "identb"], w=[f"pb{6 + half}"])
                        drain(fe_next, 1)
                drain(fe_next)
                for half in range(2):
                    V(lambda e: e.tensor_tensor(out=acc[:, half * 512:(half + 1) * 512], in0=pbank[6 + half][:], in1=h_[:, half * 512:(half + 1) * 512], op=ALU.add),
                      r=[f"pb{6 + half}", hk_], w=["acc"])
                V(lambda e: e.activation(out=otile[:], in_=acc[:], func=AF.Square, accum_out=ssc[:, 1:2]), r=["acc"], w=["otile", "ssc1"], e="act")
                V(lambda e: e.activation(out=rsc[:, 1:2], in_=ssc[:, 1:2], func=AF.Sqrt, bias=EPS, scale=1.0 / D), r=["ssc1"], w=["rsc1"], e="act")
                V(lambda e: e.reciprocal(out=rsc[:, 1:2], in_=rsc[:, 1:2]), r=["rsc1"], w=["rsc1"])
                V(lambda e: e.scalar_tensor_tensor(out=otile[:], in0=acc[:], scalar=rsc[:, 1:2], in1=gfin[:], op0=ALU.mult, op1=ALU.mult),
                  r=["acc", "rsc1", "gfin"], w=["otile"])
                DMA(y[qi // 4, 128 * (qi % 4):128 * (qi % 4) + 128, :], otile[:], r=["otile"])

        S.finish()
    return nc


def kernel(**inputs):
    com, per = _prep(inputs)
    nc = build_nc()
    in_maps = [dict(com, **per[c]) for c in range(NCORES)]
    res = run_bass_kernel_spmd(nc, in_maps, core_ids=list(range(NCORES)))
    out = np.zeros((NT, 512, D), np.float32)
    for c in range(NCORES):
        for j in range(NOWN):
            out[8 * j + c] = res.results[c]["y"][j]
    return out.reshape(1, SEQ, D)
```
